# Optimizing a Trainium2 kernel written in Bass

```python
import math
import jax
import jax.numpy as jnp
from jax import lax
import numpy as np

D_MODEL = 1024
BATCH = 4
SEQ = 8192
DEPTH = 4

CTX_LEN = 256
GRID_W = 64
EPS = 1e-6
ROPE_BASE = 10000.0
BLOCK = 128

A_HEADS = 4
A_QK = 32
A_V = 2 * A_QK
A_W = A_HEADS * A_V
B_HEADS = 6
B_KV = 2
B_HD = 64
WINDOW = 128
B_W = B_HEADS * B_HD
C_HEADS = 4
C_DK = 48
C_DV = 96
C_GATE_RANK = 16
C_GATE_NORM = 16.0
C_CHUNK = 64
C_W = C_HEADS * C_DV
MIX_W = A_W + B_W + C_W

A_QK_W = A_HEADS * 2 * A_QK
B_KV_W = B_KV * B_HD
C_QK_W = C_HEADS * C_DK
IN_SIZES = (A_QK_W, A_QK_W, A_W, B_W, B_KV_W, B_KV_W, C_QK_W, C_QK_W, C_W, C_W, 2 * C_GATE_RANK)
IN_W = 2 * A_QK_W + A_W + B_W + 2 * B_KV_W + 2 * C_QK_W + 2 * C_W + 2 * C_GATE_RANK

D_FF = 2816
N_EXPERTS = 8
TOP_K = 2
D_FF_EXPERT = 3584

kernel_name = 'hybrid_diffattn_swa_gla_moe_prefix_trunk'


def rms_norm(x, g):
    xf = x.astype(jnp.float32)
    y = xf * lax.rsqrt(jnp.mean(xf * xf, axis=-1, keepdims=True) + EPS)
    return (y * g.astype(jnp.float32)).astype(x.dtype)


def axial_rope_tables(rows, dim):
    row = jnp.repeat(jnp.arange(rows, dtype=jnp.float32), GRID_W)
    col = jnp.tile(jnp.arange(GRID_W, dtype=jnp.float32), rows)
    quarter = dim // 4
    inv = ROPE_BASE ** (-jnp.arange(quarter, dtype=jnp.float32) / quarter)
    ang_r = row[:, None] * inv
    ang_c = col[:, None] * inv
    return (jnp.cos(ang_r), jnp.sin(ang_r), jnp.cos(ang_c), jnp.sin(ang_c))


def _rotate(x, cos, sin):
    x1, x2 = jnp.split(x, 2, axis=-1)
    return jnp.concatenate([x1 * cos - x2 * sin, x2 * cos + x1 * sin], axis=-1)


def apply_axial_rope(x, tabs):
    cr, sr, cc, sc = (t.astype(x.dtype) for t in tabs)
    xr, xc = jnp.split(x, 2, axis=-1)
    return jnp.concatenate([_rotate(xr, cr, sr), _rotate(xc, cc, sc)], axis=-1)


def diff_attention(q_l, k_l, v_l, q_c, k_c, v_c, lam_p, norm_g, lam_init, tabs, want_ctx):
    bsz, n_lat, _ = q_l.shape
    n_ctx = q_c.shape[1]

    def heads_qk(t, n):
        return t.reshape(bsz, n, A_HEADS, 2, A_QK).transpose(0, 2, 3, 1, 4)

    def heads_v(t, n):
        return t.reshape(bsz, n, A_HEADS, A_V).transpose(0, 2, 1, 3)

    ql = apply_axial_rope(heads_qk(q_l, n_lat), tabs)
    kl = apply_axial_rope(heads_qk(k_l, n_lat), tabs)
    kc, vc = heads_qk(k_c, n_ctx), heads_v(v_c, n_ctx)
    k_all = jnp.concatenate([kc, kl], axis=3)
    v_all = jnp.concatenate([vc, heads_v(v_l, n_lat)], axis=2)
    lp = lam_p.astype(jnp.float32)
    lam = jnp.exp(jnp.sum(lp[0] * lp[1])) - jnp.exp(jnp.sum(lp[2] * lp[3])) + lam_init
    scale = A_QK ** -0.5

    def attend(qb, keys, vals):
        s = jnp.einsum('bhmqd,bhmkd->bhmqk', qb, keys).astype(jnp.float32) * scale
        p = jax.nn.softmax(s, axis=-1)
        p = p[:, :, 0] - lam * p[:, :, 1]
        return jnp.einsum('bhqk,bhkd->bhqd', p.astype(vals.dtype), vals)

    nb = n_lat // BLOCK
    q_blocks = jnp.moveaxis(ql.reshape(bsz, A_HEADS, 2, nb, BLOCK, A_QK), 3, 0)
    o_l = lax.map(lambda qb: attend(qb, k_all, v_all), q_blocks)
    o_l = jnp.moveaxis(o_l, 0, 2).reshape(bsz, A_HEADS, n_lat, A_V)

    def finish(o, n):
        o = rms_norm(o, norm_g) * (1.0 - lam_init)
        return o.transpose(0, 2, 1, 3).reshape(bsz, n, A_W)

    out_l = finish(o_l, n_lat)
    out_c = finish(attend(heads_qk(q_c, n_ctx), kc, vc), n_ctx) if want_ctx else None
    return out_l, out_c


def window_attention(q_l, k_l, v_l, q_c, k_c, v_c, sink, tabs, want_ctx):
    bsz, n_lat, _ = q_l.shape
    n_ctx = q_c.shape[1]
    rep = B_HEADS // B_KV

    def heads_q(t, n):
        return t.reshape(bsz, n, B_KV, rep, B_HD).transpose(0, 2, 3, 1, 4)

    def heads_kv(t, n):
        return t.reshape(bsz, n, B_KV, B_HD).transpose(0, 2, 1, 3)

    ql = apply_axial_rope(heads_q(q_l, n_lat), tabs)
    kl = apply_axial_rope(heads_kv(k_l, n_lat), tabs)
    vl = heads_kv(v_l, n_lat)
    kc, vc = heads_kv(k_c, n_ctx), heads_kv(v_c, n_ctx)
    pad = ((0, 0), (0, 0), (WINDOW, WINDOW), (0, 0))
    kp, vp = jnp.pad(kl, pad), jnp.pad(vl, pad)
    sink_l = sink.astype(jnp.float32).reshape(B_KV, rep, 1, 1)
    scale = B_HD ** -0.5
    span = BLOCK + 2 * WINDOW

    def attend(qb, keys, vals, mask):
        s = jnp.einsum('bgrqd,bgkd->bgrqk', qb, keys).astype(jnp.float32) * scale
        s = jnp.where(mask, s, -jnp.inf)
        s = jnp.concatenate([s, jnp.broadcast_to(sink_l, s.shape[:-1] + (1,))], axis=-1)
        p = jax.nn.softmax(s, axis=-1)[..., :-1]
        return jnp.einsum('bgrqk,bgkd->bgrqd', p.astype(vals.dtype), vals)

    def latent_block(args):
        qb, n = args
        start = n * BLOCK
        kw = lax.dynamic_slice_in_dim(kp, start, span, axis=2)
        vw = lax.dynamic_slice_in_dim(vp, start, span, axis=2)
        qpos = start + jnp.arange(BLOCK)
        kpos = start - WINDOW + jnp.arange(span)
        win = (jnp.abs(qpos[:, None] - kpos[None, :]) <= WINDOW) & (kpos >= 0) & (kpos < n_lat)
        mask = jnp.concatenate([jnp.ones((BLOCK, n_ctx), dtype=bool), win], axis=1)
        return attend(qb, jnp.concatenate([kc, kw], axis=2), jnp.concatenate([vc, vw], axis=2), mask)

    nb = n_lat // BLOCK
    q_blocks = jnp.moveaxis(ql.reshape(bsz, B_KV, rep, nb, BLOCK, B_HD), 3, 0)
    o = lax.map(latent_block, (q_blocks, jnp.arange(nb)))
    o = jnp.moveaxis(o, 0, 3).reshape(bsz, B_KV, rep, n_lat, B_HD)
    out_l = o.transpose(0, 3, 1, 2, 4).reshape(bsz, n_lat, B_W)
    out_c = None
    if want_ctx:
        oc = attend(heads_q(q_c, n_ctx), kc, vc, True)
        out_c = oc.transpose(0, 3, 1, 2, 4).reshape(bsz, n_ctx, B_W)
    return out_l, out_c


def gla_chunked(q, k, v, log_a, s0):
    bsz, nh, length, _ = q.shape
    dv = v.shape[-1]
    n = length // C_CHUNK

    def chunks(t):
        return jnp.moveaxis(t.reshape(bsz, nh, n, C_CHUNK, t.shape[-1]).astype(jnp.float32), 2, 0)

    qc, kc, vc, ac = chunks(q), chunks(k), chunks(v), chunks(log_a)
    b = jnp.cumsum(ac, axis=3)
    b_last = b[:, :, :, -1:, :]
    q_dec = qc * jnp.exp(b)
    k_inv = kc * jnp.exp(-b)
    k_end = kc * jnp.exp(b_last - b)
    causal = jnp.tril(jnp.ones((C_CHUNK, C_CHUNK), dtype=bool))
    att = jnp.where(causal, jnp.einsum('nbhcd,nbhsd->nbhcs', q_dec, k_inv), 0.0)
    o_intra = jnp.einsum('nbhcs,nbhse->nbhce', att, vc)
    upd = jnp.einsum('nbhcd,nbhce->nbhde', k_end, vc)
    decay = jnp.exp(b_last[:, :, :, 0, :])

    def step(state, xs):
        dec, u = xs
        return dec[..., None] * state + u, state

    s_final, s_in = lax.scan(step, s0, (decay, upd))
    o_inter = jnp.einsum('nbhcd,nbhde->nbhce', q_dec, s_in)
    o = jnp.moveaxis(o_intra + o_inter, 0, 2).reshape(bsz, nh, length, dv)
    return o.astype(v.dtype), s_final


def gla_mixer(parts_l, parts_c, w2, bg, norm_g, want_ctx):
    def prep(parts):
        q, k, v, r, g_low = parts
        bsz, n, _ = q.shape

        def heads(t):
            return t.reshape(bsz, n, C_HEADS, -1).transpose(0, 2, 1, 3)

        gates = [heads(jax.nn.log_sigmoid(
            (g_low[..., d * C_GATE_RANK:(d + 1) * C_GATE_RANK] @ w2[d] + bg[d]).astype(jnp.float32)) / C_GATE_NORM)
            for d in range(2)]
        return heads(q * C_DK ** -0.5), heads(k), heads(v), r, gates

    def flip(t):
        return jnp.flip(t, axis=2)

    def finish(o, r):
        bsz, _, n, _ = o.shape
        o = rms_norm(o, norm_g).transpose(0, 2, 1, 3).reshape(bsz, n, C_W)
        return o * jax.nn.silu(r)

    qc, kc, vc, rc, gc = prep(parts_c)
    s0 = jnp.zeros(qc.shape[:2] + (C_DK, C_DV), jnp.float32)
    o_cf, s_f = gla_chunked(qc, kc, vc, gc[0], s0)
    o_cb, s_b = gla_chunked(flip(qc), flip(kc), flip(vc), flip(gc[1]), s0)
    ql, kl, vl, rl, gl = prep(parts_l)
    o_lf, _ = gla_chunked(ql, kl, vl, gl[0], s_f)
    o_lb, _ = gla_chunked(flip(ql), flip(kl), flip(vl), flip(gl[1]), s_b)
    out_l = finish(o_lf + flip(o_lb), rl)
    out_c = finish(o_cf + flip(o_cb), rc) if want_ctx else None
    return out_l, out_c


def swiglu(h, wg, wu, wd):
    return (jax.nn.silu(h @ wg) * (h @ wu)) @ wd


def moe_ffn(h, router, wg, wu, wd):
    logits = (h @ router).astype(jnp.float32)
    top_v, top_i = lax.top_k(logits, TOP_K)
    top_w = jax.nn.softmax(top_v, axis=-1)
    combine = jnp.sum(jax.nn.one_hot(top_i, N_EXPERTS, dtype=jnp.float32) * top_w[..., None], axis=-2)
    out = jnp.zeros_like(h)
    for e in range(N_EXPERTS):
        out = out + combine[..., e:e + 1].astype(h.dtype) * swiglu(h, wg[e], wu[e], wd[e])
    return out


def setup_inputs(seed: int = 0) -> dict:
    key = jax.random.key(seed)
    ks = jax.random.split(key, 26)
    f32 = jnp.float32
    n_dense = (DEPTH + 1) // 2
    n_moe = DEPTH // 2

    def nrm(k, shape, fan_in, gain=1.0):
        return jax.random.normal(k, shape, f32) * (gain * fan_in ** -0.5)

    def gain(k, shape):
        return 1.0 + 0.05 * jax.random.normal(k, shape, f32)

    return {
        'x': jax.random.normal(ks[0], (BATCH, SEQ, D_MODEL), f32),
        'c': jax.random.normal(ks[1], (BATCH, D_MODEL), f32),
        'ctx': jax.random.normal(ks[2], (BATCH, CTX_LEN, D_MODEL), f32),
        'c_ctx': jax.random.normal(ks[3], (D_MODEL,), f32),
        'norm1_g': gain(ks[4], (DEPTH, D_MODEL)),
        'norm2_g': gain(ks[5], (DEPTH, D_MODEL)),
        'ada_w': nrm(ks[6], (DEPTH, D_MODEL, 6 * D_MODEL), D_MODEL, 0.5),
        'ada_b': 0.02 * jax.random.normal(ks[7], (DEPTH, 6 * D_MODEL), f32),
        'w_in': nrm(ks[8], (DEPTH, D_MODEL, IN_W), D_MODEL),
        'w_out': nrm(ks[9], (DEPTH, MIX_W, D_MODEL), MIX_W),
        'a_lambda': 0.1 * jax.random.normal(ks[10], (DEPTH, 4, A_QK), f32),
        'a_norm_g': gain(ks[11], (DEPTH, A_V)),
        'b_sink': jax.random.normal(ks[12], (DEPTH, B_HEADS), f32),
        'c_gate_w2': nrm(ks[13], (DEPTH, 2, C_GATE_RANK, C_HEADS * C_DK), C_GATE_RANK),
        'c_gate_b': 1.0 + 0.1 * jax.random.normal(ks[14], (DEPTH, 2, C_HEADS * C_DK), f32),
        'c_norm_g': gain(ks[15], (DEPTH, C_DV)),
        'ffn_w_gate': nrm(ks[16], (n_dense, D_MODEL, D_FF), D_MODEL),
        'ffn_w_up': nrm(ks[17], (n_dense, D_MODEL, D_FF), D_MODEL),
        'ffn_w_down': nrm(ks[18], (n_dense, D_FF, D_MODEL), D_FF),
        'moe_router': nrm(ks[19], (n_moe, D_MODEL, N_EXPERTS), D_MODEL),
        'moe_w_gate': nrm(ks[20], (n_moe, N_EXPERTS, D_MODEL, D_FF_EXPERT), D_MODEL),
        'moe_w_up': nrm(ks[21], (n_moe, N_EXPERTS, D_MODEL, D_FF_EXPERT), D_MODEL),
        'moe_w_down': nrm(ks[22], (n_moe, N_EXPERTS, D_FF_EXPERT, D_MODEL), D_FF_EXPERT),
        'final_g': gain(ks[23], (D_MODEL,)),
    }


def reference(x, c, ctx, c_ctx, norm1_g, norm2_g, ada_w, ada_b, w_in, w_out, a_lambda, a_norm_g,
              b_sink, c_gate_w2, c_gate_b, c_norm_g, ffn_w_gate, ffn_w_up, ffn_w_down,
              moe_router, moe_w_gate, moe_w_up, moe_w_down, final_g):
    n_lat = x.shape[1]
    n_ctx = ctx.shape[1]
    rows = n_lat // GRID_W
    tabs_a = axial_rope_tables(rows, A_QK)
    tabs_b = axial_rope_tables(rows, B_HD)
    split_at = [int(v) for v in np.cumsum(IN_SIZES)[:-1]]
    silu_c = jax.nn.silu(c)
    silu_cc = jax.nn.silu(c_ctx)
    xl, xc = x, ctx
    for layer in range(DEPTH):
        last = layer == DEPTH - 1
        lam_init = 0.8 - 0.6 * math.exp(-0.3 * layer)
        mod_l = (silu_c @ ada_w[layer] + ada_b[layer])[:, None, :]
        mod_c = (silu_cc @ ada_w[layer] + ada_b[layer])[None, None, :]
        sh1_l, sc1_l, g1_l, sh2_l, sc2_l, g2_l = jnp.split(mod_l, 6, axis=-1)
        sh1_c, sc1_c, g1_c, sh2_c, sc2_c, g2_c = jnp.split(mod_c, 6, axis=-1)

        h_l = rms_norm(xl, norm1_g[layer]) * (1.0 + sc1_l) + sh1_l
        h_c = rms_norm(xc, norm1_g[layer]) * (1.0 + sc1_c) + sh1_c
        proj = jnp.concatenate([h_c, h_l], axis=1) @ w_in[layer]
        pc = jnp.split(proj[:, :n_ctx], split_at, axis=-1)
        pl = jnp.split(proj[:, n_ctx:], split_at, axis=-1)
        a_l, a_c = diff_attention(pl[0], pl[1], pl[2], pc[0], pc[1], pc[2], a_lambda[layer],
                                  a_norm_g[layer], lam_init, tabs_a, not last)
        b_l, b_c = window_attention(pl[3], pl[4], pl[5], pc[3], pc[4], pc[5], b_sink[layer],
                                    tabs_b, not last)
        g_l, g_c = gla_mixer(pl[6:11], pc[6:11], c_gate_w2[layer], c_gate_b[layer],
                             c_norm_g[layer], not last)
        xl = xl + g1_l * (jnp.concatenate([a_l, b_l, g_l], axis=-1) @ w_out[layer])
        if not last:
            xc = xc + g1_c * (jnp.concatenate([a_c, b_c, g_c], axis=-1) @ w_out[layer])

        h2_l = rms_norm(xl, norm2_g[layer]) * (1.0 + sc2_l) + sh2_l
        if last:
            tok = h2_l
        else:
            h2_c = rms_norm(xc, norm2_g[layer]) * (1.0 + sc2_c) + sh2_c
            tok = jnp.concatenate([h2_c, h2_l], axis=1)
        j = layer // 2
        if layer % 2 == 0:
            y = swiglu(tok, ffn_w_gate[j], ffn_w_up[j], ffn_w_down[j])
        else:
            y = moe_ffn(tok, moe_router[j], moe_w_gate[j], moe_w_up[j], moe_w_down[j])
        xl = xl + g2_l * y[:, y.shape[1] - n_lat:]
        if not last:
            xc = xc + g2_c * y[:, :n_ctx]
    return rms_norm(xl, final_g)
```

```python
import math
from contextlib import ExitStack

import ml_dtypes
import numpy as np

import concourse.bass as bass
import concourse.mybir as mybir
from concourse.bass_utils import run_bass_kernel_spmd

F32 = mybir.dt.float32
BF16 = mybir.dt.bfloat16
AF = mybir.ActivationFunctionType
ALU = mybir.AluOpType
AX = mybir.AxisListType
ENGS = ("tensor", "vector", "scalar", "gpsimd", "sync")
NPBF = ml_dtypes.bfloat16

D = 1024
BATCH = 4
SEQ = 8192
CTX = 256
DEPTH = 4
TB = CTX + SEQ
NCORE = 8
TC = TB
EPS = 1e-6
D_FF = 2816
D_FFE = 3584
NEXP = 8


class Prog:
    def __init__(self, nc, stack, strict_same_engine=True):
        self.nc = nc
        self.sem_stack = stack
        self.stack = stack
        self.phase = 0
        self.ops = {e: [] for e in ENGS}
        self.sems = {}
        self.inc = {}
        self.cnt = {}
        self.seen = {e: {} for e in ENGS}
        self.buf = {}
        self.strict = strict_same_engine
        self.nps = 0
        for e in ENGS[:4]:
            self._mk(e, 1)

    def _mk(self, v, inc):
        self.sems[v] = self.sem_stack.enter_context(self.nc.semaphore("s_" + v.replace(":", "_")))
        self.inc[v] = inc
        self.cnt[v] = 0

    def sb(self, name, shape, dt):
        return self.stack.enter_context(self.nc.sbuf_tensor("%s_p%d" % (name, self.phase), list(shape), dt))

    def ps(self, name, shape, dt=F32):
        return self.stack.enter_context(self.nc.psum_tensor("%s_p%d" % (name, self.phase), list(shape), dt))

    def begin(self):
        self.phase += 1
        self.stack = ExitStack()
        self.stack.__enter__()

    def end(self):
        for eng in ENGS:
            self.finish(eng)
        self.emit()
        self.ops = {e: [] for e in ENGS}
        self.stack.__exit__(None, None, None)
        self.stack = None

    def _deps(self, reads, writes):
        deps = {}

        def add(vk):
            if vk is None:
                return
            v, k = vk
            if deps.get(v, 0) < k:
                deps[v] = k
        for b in reads:
            st = self.buf.get(b)
            if st:
                add(st[0])
        for b in writes:
            st = self.buf.get(b)
            if st:
                add(st[0])
                for r in st[1]:
                    add(r)
        return deps

    def op(self, eng, fn, reads=(), writes=(), slot=None):
        v = eng if slot is None else "dma:" + slot
        if v not in self.sems:
            self._mk(v, 16)
        deps = self._deps(reads, writes)
        for dv, k in deps.items():
            if dv == eng and slot is None:
                if eng == "tensor" or not self.strict or self.cnt[eng] + 1 - k >= 3:
                    continue
            if self.seen[eng].get(dv, 0) >= k:
                continue
            self.seen[eng][dv] = k
            self.ops[eng].append(("w", dv, k * self.inc[dv]))
        self.cnt[v] += 1
        k = self.cnt[v]
        self.ops[eng].append(("i", fn, v))
        for b in reads:
            st = self.buf.setdefault(b, [None, []])
            st[1].append((v, k))
        for b in writes:
            self.buf[b] = [(v, k), []]
        return (v, k)

    def pe(self, r, w, m, *a, **k):
        return self.op("tensor", (m, a, k), r, w)

    def dve(self, r, w, m, *a, **k):
        return self.op("vector", (m, a, k), r, w)

    def act(self, r, w, m, *a, **k):
        return self.op("scalar", (m, a, k), r, w)

    def pool(self, r, w, m, *a, **k):
        return self.op("gpsimd", (m, a, k), r, w)

    def load(self, out_ap, in_ap, key, r=()):
        return self.op("sync", ("dma_start", (), dict(out=out_ap, in_=in_ap)), r, [key], slot=key)

    def store(self, out_ap, in_ap, key, w=()):
        return self.op("gpsimd", ("dma_start", (), dict(out=out_ap, in_=in_ap)), [key], w, slot="st_" + key)

    def finish(self, eng="sync"):
        for v, c in self.cnt.items():
            if c and self.seen[eng].get(v, 0) < c:
                self.ops[eng].append(("w", v, c * self.inc[v]))
                self.seen[eng][v] = c

    def emit(self):
        nc = self.nc
        with nc.Block() as block:
            def run(engname):
                def body(e):
                    for o in self.ops[engname]:
                        if o[0] == "w":
                            e.wait_ge(self.sems[o[1]], o[2])
                        else:
                            getattr(e, o[1][0])(*o[1][1], **o[1][2]).then_inc(self.sems[o[2]], self.inc[o[2]])
                return body
            block.sync(run("sync"))
            block.tensor(run("tensor"))
            block.vector(run("vector"))
            block.scalar(run("scalar"))
            block.gpsimd(run("gpsimd"))


class Rot:
    def __init__(self, tiles, name):
        self.tiles = tiles
        self.name = name
        self.i = 0

    def next(self):
        j = self.i % len(self.tiles)
        self.i += 1
        return self.tiles[j], "%s%d" % (self.name, j)


def sb_rot(P, name, shape, dt, n):
    return Rot([P.sb("%s%d" % (name, j), shape, dt) for j in range(n)], name)


def ps_rot(P, name, n, shape=(128, 512), dt=F32):
    return Rot([P.ps("%s%d" % (name, j), shape, dt) for j in range(n)], name)


NWF = 2592
NWT = 1408
NW1 = NWF + NWT
ST = 384
NST = TC // ST
FEAT_ROWS = 1536


def phase_p1(P, io):
    xs = io["xs"]
    cT = io["cT"]
    ada_w = io["ada_w"]
    ada_b2 = io["ada_b2"]
    ng = io["ng"]
    w1 = io["w1"]
    w2f = io["w2f"]
    tabs = io["tabs"]
    idn = io["idn"]
    modrow = io["modrow"]
    feat = io["feat"]
    tokb = io["tokb"]
    tokf = io["tokf"]
    if True:
        P.begin()
        idf = P.sb("idf", [128, 128], F32)
        cTs = P.sb("cTs", [128, 8, 2], F32)
        scT = P.sb("scT", [128, 8, 2], F32)
        wfb = P.sb("wfb", [128, 8, NW1], BF16)
        w2s = P.sb("w2s", [33, 512], F32)
        ngs = P.sb("ngs", [2, 2, D], F32)
        modsb = P.sb("modsb", [2, 6 * D], F32)
        A1 = [P.sb("A1_%d" % i, [128, D], F32) for i in range(2)]
        B1 = [P.sb("B1_%d" % i, [128, D], F32) for i in range(2)]
        stage = sb_rot(P, "stage", [128, 2048], F32, 2)
        xrot = sb_rot(P, "xt", [128, D], F32, 2)
        hrot = sb_rot(P, "hx", [128, D], F32, 2)
        ssr = sb_rot(P, "ss", [128, 2], F32, 2)
        hT = sb_rot(P, "hT", [128, 8, ST], BF16, 2)
        glT = P.sb("glT", [33, ST], F32)
        tabr = sb_rot(P, "tab", [128, 4, ST], F32, 2)
        t1r = sb_rot(P, "t1", [128, ST], F32, 2)
        t2r = sb_rot(P, "t2", [128, ST], F32, 2)
        fo = sb_rot(P, "fo", [128, ST], BF16, 4)
        tbo = sb_rot(P, "tbo", [128, 1024], BF16, 2)
        tfo = sb_rot(P, "tfo", [128, 896], F32, 2)
        ez = sb_rot(P, "ez", [128, 512], F32, 2)
        pT = ps_rot(P, "pT", 2, (128, 4, 128))
        pg = ps_rot(P, "pg", 6)

        P.load(idf[:], idn, "idf")
        P.load(cTs[:], cT, "cTs")
        P.load(w2s[:], w2f, "w2s")
        P.load(modsb[:], ada_b2, "modsb")
        P.load(ngs[:], ng, "ngs")
        P.act(["cTs"], ["scT"], "activation", out=scT[:], in_=cTs[:], func=AF.Silu)
        P.pool([], ["glT"], "memset", glT[:], 1.0)
        for j in range(24):
            sg, sk = stage.next()
            P.load(sg[:].rearrange("p (c n) -> p c n", c=8), ada_w[:, :, j * 256:(j + 1) * 256], sk)
            pm, pk = pg.next()
            for c in range(8):
                P.pe(["scT", sk], [pk], "matmul", pm[0:2, 0:256], lhsT=scT[:, c, :], rhs=sg[:, c * 256:(c + 1) * 256],
                     start=(c == 0), stop=(c == 7))
            P.dve([pk, "modsb"], ["modsb"], "tensor_tensor", out=modsb[:, j * 256:(j + 1) * 256], in0=pm[0:2, 0:256],
                  in1=modsb[:, j * 256:(j + 1) * 256], op=ALU.add)
        for which in range(2):
            isc = 3 * which + 1
            P.dve(["modsb", "ngs"], ["modsb"], "scalar_tensor_tensor",
                  out=modsb[:, isc * D:(isc + 1) * D], in0=modsb[:, isc * D:(isc + 1) * D], scalar=1.0, in1=ngs[:, which, :],
                  op0=ALU.add, op1=ALU.mult)
        P.store(modrow, modsb[:].rearrange("p (a d) -> p a d", a=6), "modsb", ["MODROW"])
        for v in range(2):
            P.load(A1[v][:], modrow[v:v + 1, 1, :].partition_broadcast(128), "A1_%d" % v, ["MODROW"])
            P.load(B1[v][:], modrow[v:v + 1, 0, :].partition_broadcast(128), "B1_%d" % v, ["MODROW"])
        HW1 = NW1 // 2
        for c in range(16):
            sg, sk = stage.next()
            kc, hf = divmod(c, 2)
            P.load(sg[:, 0:HW1], w1[:, kc, hf * HW1:(hf + 1) * HW1], sk)
            (P.dve if c % 2 == 0 else P.pool)([sk], ["wfb"], "tensor_copy", out=wfb[:, kc, hf * HW1:(hf + 1) * HW1], in_=sg[:, 0:HW1])

        rope_pairs = [(0, 2, 0, 0), (1, 3, 0, 128), (4, 6, 0, 256), (5, 7, 0, 384),
                      (8, 11, 2, 512), (9, 12, 2, 640), (10, 13, 2, 768), (14, 15, 2, 896)]
        plain = [(16, 1024, 48 ** -0.5), (17, 1152, 48 ** -0.5), (18, 1280, 1.0), (19, 1408, 1.0)]
        for s in range(NST):
            hTt, hk = hT.next()
            tb_, tk = tabr.next()
            P.load(tb_[:], tabs[:, :, s * ST:(s + 1) * ST], tk)
            for t in range(3):
                g = 3 * s + t
                v = 1 if g < 2 else 0
                xt_, xk = xrot.next()
                P.load(xt_[:], xs[g * 128:(g + 1) * 128, :], xk)
                ss_, sk_ = ssr.next()
                hx_, hxk = hrot.next()
                P.act([xk], [hxk, sk_], "activation", out=hx_[:], in_=xt_[:], func=AF.Square, accum_out=ss_[:, 0:1])
                P.dve([sk_], [sk_], "tensor_scalar", out=ss_[:, 1:2], in0=ss_[:, 0:1], scalar1=1.0 / D, scalar2=EPS, op0=ALU.mult, op1=ALU.add)
                P.act([sk_], [sk_], "activation", out=ss_[:, 1:2], in_=ss_[:, 1:2], func=AF.Sqrt)
                P.dve([sk_], [sk_], "reciprocal", out=ss_[:, 1:2], in_=ss_[:, 1:2])
                P.dve([xk, sk_, "A1_%d" % v], [hxk], "scalar_tensor_tensor",
                      out=hx_[:], in0=xt_[:], scalar=ss_[:, 1:2], in1=A1[v][:], op0=ALU.mult, op1=ALU.mult)
                P.pool([hxk, "B1_%d" % v], [hxk], "tensor_tensor", out=hx_[:], in0=hx_[:], in1=B1[v][:], op=ALU.add)
                for hf in range(2):
                    pt_, ptk = pT.next()
                    for c in range(4):
                        P.pe([hxk, "idf"], [ptk], "transpose", out=pt_[:, c, :], in_=hx_[:, (4 * hf + c) * 128:(4 * hf + c + 1) * 128], identity=idf[:])
                    if hf == 0:
                        P.act([ptk], [hk], "activation", out=hTt[:, 0:4, t * 128:(t + 1) * 128], in_=pt_[:], func=AF.Copy)
                    else:
                        P.dve([ptk], [hk], "tensor_copy", out=hTt[:, 4:8, t * 128:(t + 1) * 128], in_=pt_[:])

            def fm(pd, pk_, f0, ncols, hTt, hk):
                for c in range(8):
                    P.pe(["wfb", hk], [pk_], "matmul", pd[0:ncols, 0:ST], lhsT=wfb[:, c, f0:f0 + ncols], rhs=hTt[:, c, :],
                         start=(c == 0), stop=(c == 7))

            for (fx, fp, ti, row0) in rope_pairs:
                px, pxk = pg.next()
                pp, ppk = pg.next()
                fm(px, pxk, fx * 128, 128, hTt, hk)
                fm(pp, ppk, fp * 128, 128, hTt, hk)
                a_, ak = t1r.next()
                b_, bk = t2r.next()
                P.dve([pxk, tk], [ak], "tensor_tensor", out=a_[:], in0=px[:, 0:ST], in1=tb_[:, ti, :], op=ALU.mult)
                P.dve([ppk, tk], [bk], "tensor_tensor", out=b_[:], in0=pp[:, 0:ST], in1=tb_[:, ti + 1, :], op=ALU.mult)
                o_, ok = fo.next()
                P.pool([ak, bk], [ok], "tensor_tensor", out=o_[:], in0=a_[:], in1=b_[:], op=ALU.add)
                P.store(feat[row0:row0 + 128, s * ST:(s + 1) * ST], o_[:], ok)
            for (f, row0, scl) in plain:
                px, pxk = pg.next()
                fm(px, pxk, f * 128, 128, hTt, hk)
                o_, ok = fo.next()
                P.act([pxk], [ok], "activation", out=o_[:], in_=px[:, 0:ST], func=AF.Copy, scale=scl)
                P.store(feat[row0:row0 + 128, s * ST:(s + 1) * ST], o_[:], ok)
            px, pxk = pg.next()
            fm(px, pxk, 2560, 32, hTt, hk)
            P.act([pxk], ["glT"], "activation", out=glT[0:32, :], in_=px[0:32, 0:ST], func=AF.Copy)
            for t in range(3):
                g = 3 * s + t
                tb2, tbk = tbo.next()
                tf2, tfk = tfo.next()
                for (c0, n) in ((0, 512), (512, 512), (1024, 384)):
                    px, pxk = pg.next()
                    for c in range(8):
                        P.pe(["wfb", hk], [pxk], "matmul", px[:, 0:n], lhsT=hTt[:, c, t * 128:(t + 1) * 128],
                             rhs=wfb[:, c, NWF + c0:NWF + c0 + n], start=(c == 0), stop=(c == 7))
                    if c0 < 1024:
                        P.dve([pxk], [tbk], "tensor_copy", out=tb2[:, c0:c0 + 512], in_=px[:, 0:512])
                    else:
                        P.act([pxk], [tfk], "activation", out=tf2[:, 0:384], in_=px[:, 0:384], func=AF.Silu)
                pz, pzk = pg.next()
                P.pe(["glT", "w2s"], [pzk], "matmul", pz[:, :], lhsT=glT[0:33, t * 128:(t + 1) * 128], rhs=w2s[:, :], start=True, stop=True)
                ez_, ezk = ez.next()
                P.act([pzk], [ezk], "activation", out=ez_[:], in_=pz[:], func=AF.Exp, scale=-1.0)
                P.act([ezk], [tfk], "activation", out=tf2[:, 384:896], in_=ez_[:], func=AF.Ln, bias=1.0)
                P.store(tokb[g * 128:(g + 1) * 128, :], tb2[:], tbk)
                P.store(tokf[g * 128:(g + 1) * 128, :], tf2[:], tfk)
        P.end()


A_Q0, A_K0, A_V0 = 0, 256, 512
B_Q0, B_K0, B_V0 = 768, 1152, 1280
C_Q0, C_K0, C_V0, C_R0, C_G0 = 1408, 1600, 1792, 2176, 2560


def _rope_perm(dim):
    q = dim // 4
    perm = np.zeros(dim, np.int64)
    sign = np.zeros(dim, np.float32)
    for d in range(dim):
        blk = d // q
        if blk % 2 == 0:
            perm[d] = d + q
            sign[d] = -1.0
        else:
            perm[d] = d - q
            sign[d] = 1.0
    return perm, sign


def w1_columns():
    cols = []
    pA, _ = _rope_perm(32)
    pB, _ = _rope_perm(64)

    def permuted(base, n, dim, perm):
        out = []
        for j in range(n):
            hd, d = divmod(j, dim)
            out.append(base + hd * dim + int(perm[d]))
        return out
    cols += list(range(A_Q0, A_Q0 + 256)) + permuted(A_Q0, 256, 32, pA)
    cols += list(range(A_K0, A_K0 + 256)) + permuted(A_K0, 256, 32, pA)
    cols += list(range(B_Q0, B_Q0 + 384)) + permuted(B_Q0, 384, 64, pB)
    cols += list(range(B_K0, B_K0 + 128)) + permuted(B_K0, 128, 64, pB)

    def padded(base):
        out = []
        for h in range(4):
            out += list(range(base + 48 * h, base + 48 * h + 48)) + [-1] * 16
        return out
    cols += padded(C_Q0) + padded(C_K0)
    cols += list(range(C_G0, C_G0 + 32))
    assert len(cols) == NWF
    cols += list(range(A_V0, A_V0 + 256)) + list(range(B_V0, B_V0 + 128)) + list(range(C_V0, C_V0 + 384))
    cols += padded(C_K0) + list(range(C_R0, C_R0 + 384))
    assert len(cols) == NW1
    return np.array(cols, np.int64)


def take_cols(w, cols):
    out = np.zeros((w.shape[0], len(cols)), w.dtype)
    m = cols >= 0
    out[:, m] = w[:, cols[m]]
    return out


def kmajor(w):
    K, N = w.shape
    return np.ascontiguousarray(w.reshape(K // 128, 128, N).transpose(1, 0, 2))


def rope_tables():
    out = np.zeros((4, 128, TB), np.float32)
    tok = np.arange(SEQ)
    row = (tok // 64).astype(np.float32)
    col = (tok % 64).astype(np.float32)
    for ti, dim in ((0, 32), (2, 64)):
        q = dim // 4
        inv = (10000.0 ** (-np.arange(q, dtype=np.float32) / q)).astype(np.float32)
        ang_r = row[:, None] * inv[None, :]
        ang_c = col[:, None] * inv[None, :]
        _, sign = _rope_perm(dim)
        cosd = np.zeros((dim, SEQ), np.float32)
        sind = np.zeros((dim, SEQ), np.float32)
        for d in range(dim):
            ang = ang_r if d < dim // 2 else ang_c
            cosd[d] = np.cos(ang[:, d % q])
            sind[d] = sign[d] * np.sin(ang[:, d % q])
        reps = 128 // dim
        out[ti, :, :CTX] = 1.0
        out[ti + 1, :, :CTX] = 0.0
        out[ti, :, CTX:] = np.tile(cosd, (reps, 1))
        out[ti + 1, :, CTX:] = np.tile(sind, (reps, 1))
    return out


def w2full(w2, bg):
    out = np.zeros((33, 512), np.float32)
    for d in range(2):
        for h in range(4):
            c0 = d * 256 + h * 64
            out[16 * d:16 * d + 16, c0:c0 + 48] = w2[d][:, 48 * h:48 * h + 48]
            out[32, c0:c0 + 48] = bg[d][48 * h:48 * h + 48]
    return out


NT = TB // 128


def phase_p2a(P, io):
    scale = 32 ** -0.5
    aqt = io["aqt"]
    akt = io["akt"]
    av = io["av"]
    lamb = io["lamb"]
    cst = io["cst"]
    mo = io["mo"]
    if True:
        P.begin()
        qT = P.sb("qT", [128, TB], BF16)
        kT = P.sb("kT", [128, TB], BF16)
        va = P.sb("va", [128, NT, 2, 65], BF16)
        lb = P.sb("lb", [128, 4, 32], F32)
        cs = P.sb("cs", [128, 66], F32)
        sm = P.sb("sm", [128, 8], F32)
        tmp32 = P.sb("tmp32", [128, 32], F32)
        gfin = P.sb("gfin", [128, 64], F32)
        pt = sb_rot(P, "pt", [128, 1024], BF16, 3)
        rec = sb_rot(P, "rec", [128, 2, 4], F32, 2)
        d1 = sb_rot(P, "d1", [128, 64], F32, 2)
        dd = sb_rot(P, "dd", [128, 64], F32, 2)
        jk = sb_rot(P, "jk", [128, 64], F32, 2)
        ssr = sb_rot(P, "ssa", [128, 2], F32, 2)
        mot = sb_rot(P, "mot", [128, 4, 128], F32, 2)
        psb = ps_rot(P, "psD", 2, (128, 1024))
        pob = ps_rot(P, "po", 4)
        osb = sb_rot(P, "osb", [128, 4, 65], F32, 4)
        P.load(qT[:], aqt, "qT")
        P.load(kT[:], akt, "kT")
        P.load(lb[:], lamb, "lb")
        P.load(cs[:], cst, "cs")
        P.pool([], ["va"], "memset", va[:], 1.0)
        for h in range(2):
            P.load(va[:, :, h, 0:64], av[:, h * 64:(h + 1) * 64].rearrange("(n p) d -> p n d", p=128), "va")
        for i in range(2):
            P.dve(["lb"], ["tmp32"], "tensor_tensor", out=tmp32[:], in0=lb[:, 2 * i, :], in1=lb[:, 2 * i + 1, :], op=ALU.mult)
            P.dve(["tmp32"], ["sm"], "reduce_sum", out=sm[:, i:i + 1], in_=tmp32[:], axis=AX.X)
        P.act(["sm"], ["sm"], "activation", out=sm[:, 0:2], in_=sm[:, 0:2], func=AF.Exp)
        P.dve(["sm"], ["sm"], "tensor_tensor", out=sm[:, 2:3], in0=sm[:, 0:1], in1=sm[:, 1:2], op=ALU.subtract)
        P.dve(["sm", "cs"], ["sm"], "tensor_tensor", out=sm[:, 2:3], in0=sm[:, 2:3], in1=cs[:, 64:65], op=ALU.add)
        P.dve(["sm"], ["sm"], "tensor_scalar", out=sm[:, 3:4], in0=sm[:, 2:3], scalar1=-1.0, scalar2=None, op0=ALU.mult)
        P.dve(["cs"], ["gfin"], "tensor_scalar", out=gfin[:], in0=cs[:, 0:64], scalar1=cs[:, 65:66], scalar2=None, op0=ALU.mult)

        groups = [(0, 2, 2)] + [(2 + 4 * g, 4, NT) for g in range(16)]
        for (t0, nq, nk) in groups:
            nqc = nq * 128
            mt, mk = mot.next()
            for hl in range(2):
                pos = []
                for m in range(2):
                    j = 2 * hl + m
                    kw = dict(tile_position=(96, 0)) if j == 3 else {}
                    for kp in range(nk // 2):
                        ps_, psk = psb.next()
                        for u in range(2):
                            kt = 2 * kp + u
                            P.pe(["kT", "qT"], [psk], "matmul", ps_[:, u * 512:u * 512 + nqc], lhsT=kT[32 * j:32 * j + 32, kt * 128:(kt + 1) * 128],
                                 rhs=qT[32 * j:32 * j + 32, t0 * 128:t0 * 128 + nqc], start=True, stop=True, **kw)
                        p_, pk_ = pt.next()
                        P.act([psk], [pk_], "activation", out=p_[:].rearrange("p (u n) -> p u n", u=2)[:, :, 0:nqc],
                              in_=ps_[:].rearrange("p (u n) -> p u n", u=2)[:, :, 0:nqc], func=AF.Exp, scale=scale)
                        for u in range(2):
                            kt = 2 * kp + u
                            for qb in range(nq):
                                P.pe([pk_, "va"], ["po%d" % qb], "matmul", pob.tiles[qb][:, 0:65], lhsT=p_[:, u * 512 + qb * 128:u * 512 + (qb + 1) * 128],
                                     rhs=va[:, kt, hl, :], start=(kt == 0), stop=(kt == nk - 1))
                    os_, osk = osb.next()
                    for qb in range(nq):
                        (P.act if qb % 2 == 0 else P.dve)(["po%d" % qb], [osk], *(("activation",) if qb % 2 == 0 else ("tensor_copy",)),
                                                          **(dict(out=os_[:, qb, :], in_=pob.tiles[qb][:, 0:65], func=AF.Copy) if qb % 2 == 0
                                                             else dict(out=os_[:, qb, :], in_=pob.tiles[qb][:, 0:65])))
                    pos.append((os_, osk))
                (po1, k1), (po2, k2) = pos
                rc, rck = rec.next()
                P.dve([k1], [rck], "reciprocal", out=rc[:, 0, 0:nq], in_=po1[:, 0:nq, 64])
                P.dve([k2], [rck], "reciprocal", out=rc[:, 1, 0:nq], in_=po2[:, 0:nq, 64])
                P.dve([rck, "sm"], [rck], "tensor_scalar", out=rc[:, 1, 0:nq], in0=rc[:, 1, 0:nq], scalar1=sm[:, 3:4], scalar2=None, op0=ALU.mult)
                for qb in range(nq):
                    a_, ak = d1.next()
                    P.dve([k1, rck], [ak], "tensor_scalar", out=a_[:], in0=po1[:, qb, 0:64], scalar1=rc[:, 0, qb:qb + 1], scalar2=None, op0=ALU.mult)
                    d_, dk = dd.next()
                    P.dve([k2, rck, ak], [dk], "scalar_tensor_tensor", out=d_[:], in0=po2[:, qb, 0:64], scalar=rc[:, 1, qb:qb + 1], in1=a_[:],
                          op0=ALU.mult, op1=ALU.add)
                    j_, jkk = jk.next()
                    s_, sk_ = ssr.next()
                    P.act([dk], [jkk, sk_], "activation", out=j_[:], in_=d_[:], func=AF.Square, accum_out=s_[:, 0:1])
                    P.dve([sk_], [sk_], "tensor_scalar", out=s_[:, 1:2], in0=s_[:, 0:1], scalar1=1.0 / 64, scalar2=EPS, op0=ALU.mult, op1=ALU.add)
                    P.act([sk_], [sk_], "activation", out=s_[:, 1:2], in_=s_[:, 1:2], func=AF.Sqrt)
                    P.dve([sk_], [sk_], "reciprocal", out=s_[:, 1:2], in_=s_[:, 1:2])
                    P.dve([dk, sk_, "gfin"], [mk], "scalar_tensor_tensor", out=mt[:, qb, hl * 64:(hl + 1) * 64], in0=d_[:], scalar=s_[:, 1:2],
                          in1=gfin[:], op0=ALU.mult, op1=ALU.mult)
            P.store(mo[t0 * 128:(t0 + nq) * 128, :].rearrange("(q p) c -> p q c", p=128), mt[:, 0:nq, :], mk)
        P.end()


def phase_p2b(P, io):
    scale = 64 ** -0.5
    bqt = io["bqt"]
    bkt = io["bkt"]
    bv = io["bv"]
    sink = io["sink"]
    masks = io["masks"]
    mo = io["mo"]
    if True:
        P.begin()
        qT = P.sb("qT", [64, 3, TB], BF16)
        kT = P.sb("kT", [64, TB], BF16)
        va = P.sb("va", [128, NT, 65], BF16)
        sk = P.sb("sk", [128, 3], F32)
        mk = P.sb("mk", [128, 2, 3, 128], F32)
        pe_ = sb_rot(P, "pe", [128, 3, 128], BF16, 10)
        den = sb_rot(P, "den", [128, 3], F32, 2)
        mot = sb_rot(P, "mot", [128, 3, 64], F32, 3)
        psb = ps_rot(P, "ps", 3)
        pob = ps_rot(P, "po", 2, (128, 3, 65))
        P.load(qT[:], bqt.rearrange("(h d) t -> d h t", d=64), "qT")
        P.load(kT[:], bkt, "kT")
        P.load(sk[:], sink, "sk")
        P.load(mk[:], masks, "mk")
        P.pool([], ["va"], "memset", va[:], 1.0)
        P.load(va[:, :, 0:64], bv.rearrange("(n p) d -> p n d", p=128), "va")
        P.act(["sk"], ["sk"], "activation", out=sk[:], in_=sk[:], func=AF.Exp)
        for n in range(NT):
            if n < 2:
                kts = [(0, None), (1, None)]
            else:
                kts = [(0, None), (1, None)]
                if n - 1 >= 2:
                    kts.append((n - 1, 0))
                kts.append((n, None))
                if n + 1 < NT:
                    kts.append((n + 1, 1))
            po, pok = pob.next()
            pts = []
            for i, (kt, msk) in enumerate(kts):
                ps_, psk = psb.next()
                P.pe(["kT", "qT"], [psk], "matmul", ps_[:, 0:384].rearrange("p (h q) -> p h q", h=3), lhsT=kT[:, kt * 128:(kt + 1) * 128],
                     rhs=qT[:, :, n * 128:(n + 1) * 128], start=True, stop=True)
                p_, pk_ = pe_.next()
                P.act([psk], [pk_], "activation", out=p_[:], in_=ps_[:, 0:384].rearrange("p (h q) -> p h q", h=3), func=AF.Exp, scale=scale)
                if msk is not None:
                    P.dve([pk_, "mk"], [pk_], "tensor_tensor", out=p_[:], in0=p_[:], in1=mk[:, msk, :, :], op=ALU.mult)
                pts.append((p_, pk_, kt))
            for h in range(3):
                for i, (p_, pk_, kt) in enumerate(pts):
                    P.pe([pk_, "va"], [pok], "matmul", po[:, h, :], lhsT=p_[:, h, :], rhs=va[:, kt, :], start=(i == 0), stop=(i == len(pts) - 1))
            dn, dnk = den.next()
            P.dve([pok, "sk"], [dnk], "tensor_tensor", out=dn[:], in0=po[:, :, 64], in1=sk[:], op=ALU.add)
            P.dve([dnk], [dnk], "reciprocal", out=dn[:], in_=dn[:])
            mt, mtk = mot.next()
            for h in range(3):
                P.dve([pok, dnk], [mtk], "tensor_scalar", out=mt[:, h, :], in0=po[:, h, 0:64], scalar1=dn[:, h:h + 1], scalar2=None, op0=ALU.mult)
            P.store(mo[n * 128:(n + 1) * 128, :], mt[:].rearrange("p h d -> p (h d)"), mtk)
        P.end()


def band_masks():
    j = np.arange(128)[:, None]
    i = np.arange(128)[None, :]
    m = np.zeros((128, 2, 3, 128), np.float32)
    m[:, 0, :, :] = (i <= j).astype(np.float32)[:, None, :]
    m[:, 1, :, :] = (j <= i).astype(np.float32)[:, None, :]
    return m


NCH = TB // 64


def gla_consts():
    s_ = np.arange(64)[:, None]
    t_ = np.arange(64)[None, :]
    tri = np.zeros((64, 2, 65), np.float32)
    trix = np.zeros((64, 2, 64), np.float32)
    mask = np.zeros((64, 2, 2, 64), np.float32)
    c = -1.0 / 16.0
    tri[:, 0, :64] = c * (s_ <= t_)
    tri[:, 1, :64] = c * (s_ >= t_)
    tri[:, :, 64] = c
    trix[:, 0, :] = c * (s_ > t_)
    trix[:, 1, :] = c * (s_ < t_)
    mask[:, 0, :, :] = (s_ <= t_).astype(np.float32)[:, None, :]
    mask[:, 1, :, :] = (s_ >= t_).astype(np.float32)[:, None, :]
    return tri, trix, mask


def phase_p2c(P, io):
    cqt = io["cqt"]
    ckt = io["ckt"]
    cktok = io["cktok"]
    cv = io["cv"]
    crs = io["crs"]
    sp = io["sp"]
    tri_d = io["tri_d"]
    trix_d = io["trix_d"]
    mask_d = io["mask_d"]
    gc_d = io["gc_d"]
    mo = io["mo"]
    if True:
        P.begin()
        qT = P.sb("qT", [64, 2, TB], BF16)
        kT = P.sb("kT", [64, 2, TB], BF16)
        OF = P.sb("OF", [64, NCH, 192], F32)
        tri = P.sb("tri_s", [64, 2, 65], F32)
        trix = P.sb("trix_s", [64, 2, 64], F32)
        mask = P.sb("mask_s", [64, 2, 2, 64], F32)
        gc = P.sb("gc_s", [64, 192], F32)
        S = [P.sb("S%d" % d, [64, 2, 96], F32) for d in range(2)]
        Sb = [P.sb("Sb%d" % d, [64, 2, 96], BF16) for d in range(2)]
        spr = sb_rot(P, "spc", [64, 2, 128], F32, 6)
        ktr = sb_rot(P, "ktk", [64, 128], BF16, 6)
        vr = sb_rot(P, "vv", [64, 192], BF16, 6)
        rr = sb_rot(P, "rs", [64, 192], F32, 4)
        E1 = sb_rot(P, "E1", [64, 2, 65], F32, 4)
        E2 = sb_rot(P, "E2", [64, 2, 64], F32, 4)
        E3 = sb_rot(P, "E3", [64, 128], F32, 4)
        qd = sb_rot(P, "qd", [64, 2, 64], BF16, 4)
        ki = sb_rot(P, "ki", [64, 2, 64], BF16, 4)
        ke = sb_rot(P, "ke", [64, 128], BF16, 4)
        att = sb_rot(P, "att", [64, 2, 64], BF16, 4)
        osum = sb_rot(P, "osum", [64, 192], F32, 2)
        jk = sb_rot(P, "jk", [64, 96], F32, 2)
        ssr = sb_rot(P, "ssc", [64, 4], F32, 2)
        yo = sb_rot(P, "yo", [64, 192], F32, 3)
        pb = ps_rot(P, "pb", 2)
        pbd = ps_rot(P, "pbd", 1)
        patt = ps_rot(P, "patt", 2)
        po = ps_rot(P, "po", 2)
        pu = ps_rot(P, "pu", 1)
        P.load(qT[:], cqt.rearrange("(h d) t -> d h t", d=64), "qT")
        P.load(kT[:], ckt.rearrange("(h d) t -> d h t", d=64), "kT")
        P.load(tri[:], tri_d, "tri")
        P.load(trix[:], trix_d, "trix")
        P.load(mask[:], mask_d, "mask")
        P.load(gc[:], gc_d, "gc")
        for d in range(2):
            P.pool([], ["S%d" % d], "memset", S[d][:], 0.0)
            P.pool([], ["Sb%d" % d], "memset", Sb[d][:], 0.0)
        fwd = [(c, 0) for c in range(NCH)]
        bwd = [(c, 1) for c in (3, 2, 1, 0)] + [(c, 1) for c in range(NCH - 1, 3, -1)]
        order = [x for pair in zip(fwd, bwd) for x in pair]
        seen_c = set()
        for (c, d) in order:
            second = c in seen_c
            seen_c.add(c)
            tk = slice(c * 64, (c + 1) * 64)
            sp_, spk = spr.next()
            P.load(sp_[:], sp[tk, :, :], spk)
            kt_, ktk = ktr.next()
            P.load(kt_[:], cktok[tk, :], ktk)
            v_, vk = vr.next()
            P.load(v_[:], cv[tk, :], vk)
            if second:
                r_, rk = rr.next()
                P.load(r_[:], crs[tk, :], rk)
            pb_, pbk = pb.next()
            for h in range(2):
                P.pe([spk, "tri"], [pbk], "matmul", pb_[0:64, h * 65:(h + 1) * 65], lhsT=sp_[:, d, 64 * h:64 * h + 64], rhs=tri[:, d, :], start=True, stop=True)
            pbd_, pbdk = pbd.next()
            P.pe([spk, "trix"], [pbdk], "matmul", pbd_[0:64, 0:128], lhsT=trix[:, d, :], rhs=sp_[:, d, :], start=True, stop=True)
            e1, e1k = E1.next()
            e2, e2k = E2.next()
            e3, e3k = E3.next()
            pbv = pb_[0:64, 0:130].rearrange("p (h n) -> p h n", h=2)
            P.act([pbk], [e1k], "activation", out=e1[:], in_=pbv, func=AF.Exp)
            P.act([pbk], [e2k], "activation", out=e2[:], in_=pbv[:, :, 0:64], func=AF.Exp, scale=-1.0)
            P.act([pbdk], [e3k], "activation", out=e3[:], in_=pbd_[0:64, 0:128], func=AF.Exp)
            qd_, qdk = qd.next()
            ki_, kik = ki.next()
            ke_, kek = ke.next()
            P.dve(["qT", e1k], [qdk], "tensor_tensor", out=qd_[:], in0=qT[:, :, tk], in1=e1[:, :, 0:64], op=ALU.mult)
            P.dve(["kT", e2k], [kik], "tensor_tensor", out=ki_[:], in0=kT[:, :, tk], in1=e2[:], op=ALU.mult)
            P.pool([ktk, e3k], [kek], "tensor_tensor", out=ke_[:], in0=kt_[:], in1=e3[:], op=ALU.mult)
            pa_, pak = patt.next()
            pav = pa_[0:64, 0:128].rearrange("p (h n) -> p h n", h=2)
            for h in range(2):
                P.pe([kik, qdk], [pak], "matmul", pa_[0:64, h * 64:(h + 1) * 64], lhsT=ki_[:, h, :], rhs=qd_[:, h, :], start=True, stop=True)
            at_, atk = att.next()
            P.dve([pak, "mask"], [atk], "tensor_tensor", out=at_[:], in0=pav, in1=mask[:, d, :, :], op=ALU.mult)
            po_, pok = po.next()
            for h in range(2):
                P.pe([atk, vk], [pok], "matmul", po_[0:64, 96 * h:96 * h + 96], lhsT=at_[:, h, :], rhs=v_[:, 96 * h:96 * h + 96], start=True, stop=False)
                P.pe([qdk, "Sb%d" % d], [pok], "matmul", po_[0:64, 96 * h:96 * h + 96], lhsT=qd_[:, h, :], rhs=Sb[d][:, h, :], start=False, stop=True)
            pu_, puk = pu.next()
            for h in range(2):
                P.pe([kek, vk], [puk], "matmul", pu_[0:64, 96 * h:96 * h + 96], lhsT=ke_[:, 64 * h:64 * h + 64], rhs=v_[:, 96 * h:96 * h + 96], start=True, stop=True)
            for h in range(2):
                P.dve(["S%d" % d, e1k, puk], ["S%d" % d], "scalar_tensor_tensor", out=S[d][:, h, :], in0=S[d][:, h, :], scalar=e1[:, h, 64:65],
                      in1=pu_[0:64, 96 * h:96 * h + 96], op0=ALU.mult, op1=ALU.add)
            P.pool(["S%d" % d], ["Sb%d" % d], "tensor_copy", out=Sb[d][:], in_=S[d][:])
            if not second:
                P.act([pok], ["OF%d" % c], "activation", out=OF[:, c, :], in_=po_[0:64, 0:192], func=AF.Copy)
            else:
                os_, osk = osum.next()
                P.dve([pok, "OF%d" % c], [osk], "tensor_tensor", out=os_[:], in0=po_[0:64, 0:192], in1=OF[:, c, :], op=ALU.add)
                s_, sk_ = ssr.next()
                for h in range(2):
                    j_, jkk = jk.next()
                    P.act([osk], [jkk, sk_], "activation", out=j_[:], in_=os_[:, 96 * h:96 * h + 96], func=AF.Square, accum_out=s_[:, h:h + 1])
                P.dve([sk_], [sk_], "tensor_scalar", out=s_[:, 2:4], in0=s_[:, 0:2], scalar1=1.0 / 96, scalar2=EPS, op0=ALU.mult, op1=ALU.add)
                P.act([sk_], [sk_], "activation", out=s_[:, 2:4], in_=s_[:, 2:4], func=AF.Sqrt)
                P.dve([sk_], [sk_], "reciprocal", out=s_[:, 2:4], in_=s_[:, 2:4])
                y_, yk = yo.next()
                for h in range(2):
                    P.dve([osk, sk_, "gc"], [yk], "scalar_tensor_tensor", out=y_[:, 96 * h:96 * h + 96], in0=os_[:, 96 * h:96 * h + 96],
                          scalar=s_[:, 2 + h:3 + h], in1=gc[:, 96 * h:96 * h + 96], op0=ALU.mult, op1=ALU.mult)
                P.pool([yk, rk], [yk], "tensor_tensor", out=y_[:], in0=y_[:], in1=r_[:], op=ALU.mult)
                P.store(mo[tk, :], y_[:], yk)
        P.end()


def phase_p3(P, io, E, FF, moe):
    FC = FF // 128
    NG = FF // 256
    xs = io["xs"]
    mo = io["mo"]
    modrow = io["modrow"]
    wout = io["wout"]
    router = io["router"]
    wg = io["wg"]
    wu = io["wu"]
    wd = io["wd"]
    idn = io["idn"]
    xo = io["xo"]
    if True:
        P.begin()
        idf = P.sb("idf", [128, 128], F32)
        woutb = P.sb("woutb", [128, 8, D], BF16)
        rts = P.sb("rts", [128, 8, 8], F32)
        G1 = [P.sb("G1_%d" % i, [128, D], F32) for i in range(2)]
        A2 = [P.sb("A2_%d" % i, [128, D], F32) for i in range(2)]
        B2 = [P.sb("B2_%d" % i, [128, D], F32) for i in range(2)]
        G2 = [P.sb("G2_%d" % i, [128, D], F32) for i in range(2)]
        stage = sb_rot(P, "stage", [128, 2048], F32, 3)
        xrot = sb_rot(P, "xt", [128, D], F32, 2)
        mrot = sb_rot(P, "mt", [128, D], F32, 2)
        xnew = sb_rot(P, "xn", [128, D], F32, 3)
        yacc = sb_rot(P, "ya", [128, D], F32, 3)
        hrot = sb_rot(P, "hx", [128, D], F32, 2)
        ssr = sb_rot(P, "ss", [128, 2], F32, 2)
        catT = sb_rot(P, "catT", [128, 8, 128], BF16, 2)
        h2T = sb_rot(P, "h2T", [128, 8, ST], BF16, 2)
        h2Tf = P.sb("h2Tf", [128, 8, ST], F32) if moe else None
        cw = sb_rot(P, "cw", [128, 3, 8], F32, 2)
        lg = sb_rot(P, "lg", [128, 8], F32, 2)
        rt = sb_rot(P, "rtmp", [128, 4, 8], F32, 2)
        rs_ = sb_rot(P, "rsc", [128, 4], F32, 2)
        wgb = sb_rot(P, "wgb", [128, 8, 256], BF16, 2)
        wub = sb_rot(P, "wub", [128, 8, 256], BF16, 2)
        wdb = sb_rot(P, "wdb", [128, 2, D], BF16, 2)
        sgr = sb_rot(P, "sg", [128, ST], F32, 2)
        aTr = sb_rot(P, "aT", [128, ST], BF16, 3)
        bank = [P.ps("bk%d" % i, [128, 512], F32) for i in range(8)]
        bk = ["bk%d" % i for i in range(8)]
        P.load(idf[:], idn, "idf")
        if moe:
            P.load(rts[:], router, "rts")
        for v in range(2):
            P.load(G1[v][:], modrow[v:v + 1, 2, :].partition_broadcast(128), "G1_%d" % v)
            P.load(B2[v][:], modrow[v:v + 1, 3, :].partition_broadcast(128), "B2_%d" % v)
            P.load(A2[v][:], modrow[v:v + 1, 4, :].partition_broadcast(128), "A2_%d" % v)
            P.load(G2[v][:], modrow[v:v + 1, 5, :].partition_broadcast(128), "G2_%d" % v)
        for c in range(8):
            sg, sk = stage.next()
            P.load(sg[:, 0:D], wout[:, c, :], sk)
            (P.dve if c % 2 == 0 else P.pool)([sk], ["woutb"], "tensor_copy", out=woutb[:, c, :], in_=sg[:, 0:D])
        ccast = 0
        for s in range(NST):
            h2, h2k = h2T.next()
            cw_, cwk = cw.next()
            xns = []
            yas = []
            for t in range(3):
                g = 3 * s + t
                v = 1 if g < 2 else 0
                mt, mtk = mrot.next()
                P.load(mt[:], mo[g * 128:(g + 1) * 128, :], mtk)
                xt_, xk = xrot.next()
                P.load(xt_[:], xs[g * 128:(g + 1) * 128, :], xk)
                ct, ctk = catT.next()
                for hf in range(2):
                    for c in range(4):
                        P.pe([mtk, "idf"], [bk[6 + hf]], "transpose", out=bank[6 + hf][:, c * 128:(c + 1) * 128],
                             in_=mt[:, (4 * hf + c) * 128:(4 * hf + c + 1) * 128], identity=idf[:])
                    (P.act if hf == 0 else P.dve)([bk[6 + hf]], [ctk], *(("activation",) if hf == 0 else ("tensor_copy",)),
                                                  **(dict(out=ct[:, 4 * hf:4 * hf + 4, :], in_=bank[6 + hf][:].rearrange("p (c n) -> p c n", c=4), func=AF.Copy)
                                                     if hf == 0 else dict(out=ct[:, 4 * hf:4 * hf + 4, :], in_=bank[6 + hf][:].rearrange("p (c n) -> p c n", c=4))))
                xn_, xnk = xnew.next()
                for hf in range(2):
                    for c in range(8):
                        P.pe([ctk, "woutb"], [bk[1 + hf]], "matmul", bank[1 + hf][:, :], lhsT=ct[:, c, :], rhs=woutb[:, c, hf * 512:(hf + 1) * 512],
                             start=(c == 0), stop=(c == 7))
                    P.dve([bk[1 + hf], "G1_%d" % v], [xnk], "tensor_tensor", out=xn_[:, hf * 512:(hf + 1) * 512], in0=bank[1 + hf][:, :],
                          in1=G1[v][:, hf * 512:(hf + 1) * 512], op=ALU.mult)
                P.pool([xnk, xk], [xnk], "tensor_tensor", out=xn_[:], in0=xn_[:], in1=xt_[:], op=ALU.add)
                xns.append((xn_, xnk, v))
                ss_, sk_ = ssr.next()
                hx_, hxk = hrot.next()
                P.act([xnk], [hxk, sk_], "activation", out=hx_[:], in_=xn_[:], func=AF.Square, accum_out=ss_[:, 0:1])
                P.dve([sk_], [sk_], "tensor_scalar", out=ss_[:, 1:2], in0=ss_[:, 0:1], scalar1=1.0 / D, scalar2=EPS, op0=ALU.mult, op1=ALU.add)
                P.act([sk_], [sk_], "activation", out=ss_[:, 1:2], in_=ss_[:, 1:2], func=AF.Sqrt)
                P.dve([sk_], [sk_], "reciprocal", out=ss_[:, 1:2], in_=ss_[:, 1:2])
                P.dve([xnk, sk_, "A2_%d" % v], [hxk], "scalar_tensor_tensor", out=hx_[:], in0=xn_[:], scalar=ss_[:, 1:2], in1=A2[v][:],
                      op0=ALU.mult, op1=ALU.mult)
                P.pool([hxk, "B2_%d" % v], [hxk], "tensor_tensor", out=hx_[:], in0=hx_[:], in1=B2[v][:], op=ALU.add)
                for hf in range(2):
                    for c in range(4):
                        P.pe([hxk, "idf"], [bk[6 + hf]], "transpose", out=bank[6 + hf][:, c * 128:(c + 1) * 128],
                             in_=hx_[:, (4 * hf + c) * 128:(4 * hf + c + 1) * 128], identity=idf[:])
                    src = bank[6 + hf][:].rearrange("p (c n) -> p c n", c=4)
                    if moe:
                        P.dve([bk[6 + hf]], ["h2Tf"], "tensor_copy", out=h2Tf[:, 4 * hf:4 * hf + 4, t * 128:(t + 1) * 128], in_=src)
                        P.pool(["h2Tf"], [h2k], "tensor_copy", out=h2[:, 4 * hf:4 * hf + 4, t * 128:(t + 1) * 128],
                               in_=h2Tf[:, 4 * hf:4 * hf + 4, t * 128:(t + 1) * 128])
                    else:
                        P.act([bk[6 + hf]], [h2k], "activation", out=h2[:, 4 * hf:4 * hf + 4, t * 128:(t + 1) * 128], in_=src, func=AF.Copy)
                if moe:
                    for c in range(8):
                        P.pe(["h2Tf", "rts"], [bk[3]], "matmul", bank[3][:, 0:8], lhsT=h2Tf[:, c, t * 128:(t + 1) * 128], rhs=rts[:, c, :],
                             start=(c == 0), stop=(c == 7))
                    l_, lk = lg.next()
                    r_, rk = rt.next()
                    q_, qk = rs_.next()
                    P.dve([bk[3]], [lk], "tensor_copy", out=l_[:], in_=bank[3][:, 0:8])
                    P.dve([lk], [qk], "reduce_max", out=q_[:, 0:1], in_=l_[:], axis=AX.X)
                    P.dve([lk, qk], [rk], "tensor_scalar", out=r_[:, 0, :], in0=l_[:], scalar1=q_[:, 0:1], scalar2=None, op0=ALU.is_equal)
                    P.dve([rk, lk], [rk], "scalar_tensor_tensor", out=r_[:, 1, :], in0=r_[:, 0, :], scalar=-1e30, in1=l_[:], op0=ALU.mult, op1=ALU.add)
                    P.dve([rk], [qk], "reduce_max", out=q_[:, 1:2], in_=r_[:, 1, :], axis=AX.X)
                    P.dve([lk, qk], [rk], "tensor_scalar", out=r_[:, 2, :], in0=l_[:], scalar1=q_[:, 1:2], scalar2=None, op0=ALU.is_ge)
                    P.dve([qk], [qk], "tensor_scalar", out=q_[:, 2:3], in0=q_[:, 0:1], scalar1=-1.0, scalar2=None, op0=ALU.mult)
                    P.act([lk, qk], [rk], "activation", out=r_[:, 3, :], in_=l_[:], func=AF.Exp, bias=q_[:, 2:3])
                    P.dve([rk], [rk], "tensor_tensor", out=r_[:, 3, :], in0=r_[:, 3, :], in1=r_[:, 2, :], op=ALU.mult)
                    P.dve([rk], [qk], "reduce_sum", out=q_[:, 3:4], in_=r_[:, 3, :], axis=AX.X)
                    P.dve([qk], [qk], "reciprocal", out=q_[:, 3:4], in_=q_[:, 3:4])
                    P.dve([rk, qk], [cwk], "tensor_scalar", out=cw_[:, t, :], in0=r_[:, 3, :], scalar1=q_[:, 3:4], scalar2=None, op0=ALU.mult)
            for t in range(3):
                ya_, yak = yacc.next()
                yas.append((ya_, yak))
            for e in range(E):
                for gi in range(NG):
                    tiles = []
                    for (src_, rot_, shp) in ((wg[e, :, :, gi * 256:(gi + 1) * 256], wgb, 8), (wu[e, :, :, gi * 256:(gi + 1) * 256], wub, 8),
                                              (wd[e, :, 2 * gi:2 * gi + 2, :], wdb, 2)):
                        sg, sk = stage.next()
                        P.load(sg[:].rearrange("p (c n) -> p c n", c=shp), src_, sk)
                        wb_, wbk = rot_.next()
                        (P.pool if ccast % 3 != 2 else P.dve)([sk], [wbk], "tensor_copy", out=wb_[:], in_=sg[:].rearrange("p (c n) -> p c n", c=shp))
                        ccast += 1
                        tiles.append((wb_, wbk))
                    (wg_, wgk), (wu_, wuk), (wd_, wdk) = tiles
                    for j in range(2):
                        for c in range(8):
                            P.pe([wgk, h2k], [bk[6]], "matmul", bank[6][:, 0:ST], lhsT=wg_[:, c, j * 128:(j + 1) * 128], rhs=h2[:, c, :],
                                 start=(c == 0), stop=(c == 7))
                        for c in range(8):
                            P.pe([wuk, h2k], [bk[7]], "matmul", bank[7][:, 0:ST], lhsT=wu_[:, c, j * 128:(j + 1) * 128], rhs=h2[:, c, :],
                                 start=(c == 0), stop=(c == 7))
                        sg_, sgk = sgr.next()
                        P.act([bk[6]], [sgk], "activation", out=sg_[:], in_=bank[6][:, 0:ST], func=AF.Silu)
                        a_, ak = aTr.next()
                        P.dve([sgk, bk[7]], [ak], "tensor_tensor", out=a_[:], in0=sg_[:], in1=bank[7][:, 0:ST], op=ALU.mult)
                        first = (gi == 0 and j == 0)
                        last = (gi == NG - 1 and j == 1)
                        for t in range(3):
                            for hf in range(2):
                                b_ = 2 * t + hf
                                P.pe([ak, wdk], [bk[b_]], "matmul", bank[b_][:, :], lhsT=a_[:, t * 128:(t + 1) * 128], rhs=wd_[:, j, hf * 512:(hf + 1) * 512],
                                     start=first, stop=last)
                for t in range(3):
                    ya_, yak = yas[t]
                    for hf in range(2):
                        b_ = 2 * t + hf
                        osl = ya_[:, hf * 512:(hf + 1) * 512]
                        if not moe:
                            P.act([bk[b_]], [yak], "activation", out=osl, in_=bank[b_][:, :], func=AF.Copy)
                        elif e == 0:
                            P.dve([bk[b_], cwk], [yak], "tensor_scalar", out=osl, in0=bank[b_][:, :], scalar1=cw_[:, t, e:e + 1], scalar2=None, op0=ALU.mult)
                        else:
                            P.dve([bk[b_], cwk, yak], [yak], "scalar_tensor_tensor", out=osl, in0=bank[b_][:, :], scalar=cw_[:, t, e:e + 1], in1=osl,
                                  op0=ALU.mult, op1=ALU.add)
            for t in range(3):
                g = 3 * s + t
                ya_, yak = yas[t]
                xn_, xnk, v = xns[t]
                P.dve([yak, "G2_%d" % v], [yak], "tensor_tensor", out=ya_[:], in0=ya_[:], in1=G2[v][:], op=ALU.mult)
                P.pool([yak, xnk], [yak], "tensor_tensor", out=ya_[:], in0=ya_[:], in1=xn_[:], op=ALU.add)
                P.store(xo[g * 128:(g + 1) * 128, :], ya_[:], yak)
        P.end()


def phase_p4(P, io):
    xs = io["xs"]
    fg = io["fg"]
    xo = io["xo"]
    if True:
        P.begin()
        g_ = P.sb("g_", [128, D], F32)
        xrot = sb_rot(P, "xt", [128, D], F32, 3)
        orot = sb_rot(P, "ot", [128, D], F32, 3)
        ssr = sb_rot(P, "ss", [128, 2], F32, 3)
        P.load(g_[:], fg[0:1, :].partition_broadcast(128), "g_")
        for g in range(2, TC // 128):
            xt_, xk = xrot.next()
            P.load(xt_[:], xs[g * 128:(g + 1) * 128, :], xk)
            o_, ok = orot.next()
            ss_, sk_ = ssr.next()
            P.act([xk], [ok, sk_], "activation", out=o_[:], in_=xt_[:], func=AF.Square, accum_out=ss_[:, 0:1])
            P.dve([sk_], [sk_], "tensor_scalar", out=ss_[:, 1:2], in0=ss_[:, 0:1], scalar1=1.0 / D, scalar2=EPS, op0=ALU.mult, op1=ALU.add)
            P.act([sk_], [sk_], "activation", out=ss_[:, 1:2], in_=ss_[:, 1:2], func=AF.Sqrt)
            P.dve([sk_], [sk_], "reciprocal", out=ss_[:, 1:2], in_=ss_[:, 1:2])
            P.dve([xk, sk_, "g_"], [ok], "scalar_tensor_tensor", out=o_[:], in0=xt_[:], scalar=ss_[:, 1:2], in1=g_[:], op0=ALU.mult, op1=ALU.mult)
            P.store(xo[(g - 2) * 128:(g - 1) * 128, :], o_[:], ok)
        P.end()


def build_fused(depth=DEPTH):
    nc = bass.Bass("TRN2", target_bir_lowering=False)

    def din(name, shape, dt=F32):
        return nc.dram_tensor(name, list(shape), dt, kind="ExternalInput").ap()

    def scratch(name, shape, dt=F32):
        return nc.dram_tensor(name, list(shape), dt).ap()

    xs = din("xs", [TB, D])
    cT = din("cT", [128, 8, 2])
    tabs = din("tabs", [128, 4, TB])
    idn = din("idn", [128, 128])
    ada_w = din("ada_w", [DEPTH, 128, 8, 6 * D])
    ada_b2 = din("ada_b2", [DEPTH, 2, 6 * D])
    ng = din("ng", [DEPTH, 2, 2, D])
    w1 = din("w1", [DEPTH, 128, 8, NW1])
    w2f = din("w2f", [DEPTH, 33, 512])
    lamb = din("lamb", [DEPTH, 128, 4, 32])
    cst = din("cst", [DEPTH, 128, 66])
    sink = din("sink", [DEPTH, 2, 128, 3])
    masks = din("masks", [128, 2, 3, 128])
    tri = din("tri", [64, 2, 65])
    trix = din("trix", [64, 2, 64])
    gmask = din("gmask", [64, 2, 2, 64])
    gc = din("gc", [DEPTH, 64, 192])
    wout = din("wout", [DEPTH, 128, 8, D])
    ffg = din("ffg", [2, 1, 128, 8, D_FF])
    ffu = din("ffu", [2, 1, 128, 8, D_FF])
    ffd = din("ffd", [2, 1, 128, D_FF // 128, D])
    mog = din("mog", [2, NEXP, 128, 8, D_FFE])
    mou = din("mou", [2, NEXP, 128, 8, D_FFE])
    mod_ = din("mod_", [2, NEXP, 128, D_FFE // 128, D])
    router = din("router", [2, 128, 8, 8])
    fg = din("fg", [1, D])
    out = nc.dram_tensor("out", [SEQ, D], F32, kind="ExternalOutput").ap()
    X = [scratch("X%d" % i, [TB, D]) for i in range(2)]
    MODROW = scratch("MODROW", [2, 6, D])
    FEAT = scratch("FEAT", [FEAT_ROWS, TB], BF16)
    TOKB = scratch("TOKB", [TB, 1024], BF16)
    TOKF = scratch("TOKF", [TB, 896])
    MO = scratch("MO", [TB, D])
    with ExitStack() as st:
        P = Prog(nc, st)
        xin = xs
        for L in range(depth):
            xout = X[L % 2]
            phase_p1(P, dict(xs=xin, cT=cT, ada_w=ada_w[L], ada_b2=ada_b2[L], ng=ng[L], w1=w1[L], w2f=w2f[L], tabs=tabs, idn=idn,
                             modrow=MODROW, feat=FEAT, tokb=TOKB, tokf=TOKF))
            for hh in range(2):
                phase_p2a(P, dict(aqt=FEAT[128 * hh:128 * hh + 128, :], akt=FEAT[256 + 128 * hh:256 + 128 * hh + 128, :],
                                  av=TOKB[:, 128 * hh:128 * hh + 128], lamb=lamb[L], cst=cst[L], mo=MO[:, 128 * hh:128 * hh + 128]))
                phase_p2b(P, dict(bqt=FEAT[512 + 192 * hh:512 + 192 * hh + 192, :], bkt=FEAT[896 + 64 * hh:896 + 64 * hh + 64, :],
                                  bv=TOKB[:, 256 + 64 * hh:256 + 64 * hh + 64], sink=sink[L, hh], masks=masks,
                                  mo=MO[:, 256 + 192 * hh:256 + 192 * hh + 192]))
                phase_p2c(P, dict(cqt=FEAT[1024 + 128 * hh:1024 + 128 * hh + 128, :], ckt=FEAT[1280 + 128 * hh:1280 + 128 * hh + 128, :],
                                  cktok=TOKB[:, 768 + 128 * hh:768 + 128 * hh + 128], cv=TOKB[:, 384 + 192 * hh:384 + 192 * hh + 192],
                                  crs=TOKF[:, 192 * hh:192 * hh + 192],
                                  sp=TOKF[:, 384:896].rearrange("t (d c) -> t d c", d=2)[:, :, 128 * hh:128 * hh + 128],
                                  tri_d=tri, trix_d=trix, mask_d=gmask, gc_d=gc[L], mo=MO[:, 640 + 192 * hh:640 + 192 * hh + 192]))
            j = L // 2
            if L % 2 == 0:
                phase_p3(P, dict(xs=xin, mo=MO, modrow=MODROW, wout=wout[L], router=router[0], wg=ffg[j], wu=ffu[j], wd=ffd[j],
                                 idn=idn, xo=xout), 1, D_FF, False)
            else:
                phase_p3(P, dict(xs=xin, mo=MO, modrow=MODROW, wout=wout[L], router=router[j], wg=mog[j], wu=mou[j], wd=mod_[j],
                                 idn=idn, xo=xout), NEXP, D_FFE, True)
            xin = xout
        phase_p4(P, dict(xs=xin, fg=fg, xo=out))
    return nc


_PROG = []


def _c(a):
    return np.ascontiguousarray(a)


def kernel(x, c, ctx, c_ctx, norm1_g, norm2_g, ada_w, ada_b, w_in, w_out, a_lambda, a_norm_g,
           b_sink, c_gate_w2, c_gate_b, c_norm_g, ffn_w_gate, ffn_w_up, ffn_w_down,
           moe_router, moe_w_gate, moe_w_up, moe_w_down, final_g):
    f = lambda a: np.asarray(a, np.float32)
    x, c, ctx, c_ctx = f(x), f(c), f(ctx), f(c_ctx)
    if not _PROG:
        _PROG.append(build_fused())
    nc = _PROG[0]
    cols = w1_columns()
    tabs = _c(rope_tables().transpose(1, 0, 2))
    tri, trix, gmask = gla_consts()
    shared = {
        "tabs": tabs, "idn": np.eye(128, dtype=np.float32),
        "ada_w": np.stack([kmajor(f(ada_w[L])) for L in range(DEPTH)]),
        "ada_b2": np.stack([np.stack([f(ada_b[L])] * 2) for L in range(DEPTH)]),
        "ng": np.stack([np.stack([np.stack([f(norm1_g[L]), f(norm2_g[L])])] * 2) for L in range(DEPTH)]),
        "w1": np.stack([kmajor(take_cols(f(w_in[L]), cols)) for L in range(DEPTH)]),
        "w2f": np.stack([w2full(f(c_gate_w2[L]), f(c_gate_b[L])) for L in range(DEPTH)]),
        "lamb": _c(np.broadcast_to(f(a_lambda)[:, None], (DEPTH, 128, 4, 32))),
        "masks": band_masks(), "tri": tri, "trix": trix, "gmask": gmask,
        "gc": _c(np.broadcast_to(np.tile(f(c_norm_g), (1, 2))[:, None, :], (DEPTH, 64, 192))),
        "wout": np.stack([kmajor(f(w_out[L])) for L in range(DEPTH)]),
        "ffg": np.stack([kmajor(f(ffn_w_gate[j]))[None] for j in range(2)]),
        "ffu": np.stack([kmajor(f(ffn_w_up[j]))[None] for j in range(2)]),
        "ffd": np.stack([kmajor(f(ffn_w_down[j]))[None] for j in range(2)]),
        "mog": np.stack([np.stack([kmajor(f(moe_w_gate[j][e])) for e in range(NEXP)]) for j in range(2)]),
        "mou": np.stack([np.stack([kmajor(f(moe_w_up[j][e])) for e in range(NEXP)]) for j in range(2)]),
        "mod_": np.stack([np.stack([kmajor(f(moe_w_down[j][e])) for e in range(NEXP)]) for j in range(2)]),
        "router": np.stack([kmajor(f(moe_router[j])) for j in range(2)]),
        "fg": _c(f(final_g)[None, :]),
    }
    cst = np.zeros((DEPTH, 128, 66), np.float32)
    for L in range(DEPTH):
        lam_init = 0.8 - 0.6 * math.exp(-0.3 * L)
        cst[L, :, :64] = f(a_norm_g[L])[None, :]
        cst[L, :, 64] = lam_init
        cst[L, :, 65] = 1.0 - lam_init
    shared["cst"] = cst
    shared["sink"] = _c(np.broadcast_to(f(b_sink).reshape(DEPTH, 2, 1, 3), (DEPTH, 2, 128, 3)))
    in_maps = []
    for i in range(NCORE):
        b = i // 2
        m = dict(shared)
        m["xs"] = _c(np.concatenate([ctx[b], x[b]], 0))
        m["cT"] = _c(np.stack([c[b].reshape(8, 128).T, c_ctx.reshape(8, 128).T], -1))
        in_maps.append(m)
    res = run_bass_kernel_spmd(nc, in_maps, core_ids=list(range(NCORE)))
    out = np.stack([np.asarray(res.results[2 * b]["out"]) for b in range(BATCH)], 0)
    return np.ascontiguousarray(out.astype(np.float32))
```

```python
import math
from contextlib import ExitStack

import ml_dtypes
import numpy as np

import concourse.bass as bass
import concourse.mybir as mybir
from concourse.bass_utils import run_bass_kernel_spmd

F32 = mybir.dt.float32
BF16 = mybir.dt.bfloat16
AF = mybir.ActivationFunctionType
ALU = mybir.AluOpType
AX = mybir.AxisListType
ENGS = ("tensor", "vector", "scalar", "gpsimd", "sync")
NPBF = ml_dtypes.bfloat16

D = 1024
BATCH = 4
SEQ = 8192
CTX = 256
DEPTH = 4
TB = CTX + SEQ
NCORE = 8
TC = TB
EPS = 1e-6
D_FF = 2816
D_FFE = 3584
NEXP = 8


class Prog:
    def __init__(self, nc, stack, strict_same_engine=True):
        self.nc = nc
        self.sem_stack = stack
        self.stack = stack
        self.phase = 0
        self.ops = {e: [] for e in ENGS}
        self.sems = {}
        self.inc = {}
        self.cnt = {}
        self.seen = {e: {} for e in ENGS}
        self.buf = {}
        self.strict = strict_same_engine
        self.nps = 0
        for e in ENGS[:4]:
            self._mk(e, 1)

    def _mk(self, v, inc):
        self.sems[v] = self.sem_stack.enter_context(self.nc.semaphore("s_" + v.replace(":", "_")))
        self.inc[v] = inc
        self.cnt[v] = 0

    def sb(self, name, shape, dt):
        return self.stack.enter_context(self.nc.sbuf_tensor("%s_p%d" % (name, self.phase), list(shape), dt))

    def ps(self, name, shape, dt=F32):
        return self.stack.enter_context(self.nc.psum_tensor("%s_p%d" % (name, self.phase), list(shape), dt))

    def begin(self):
        self.phase += 1
        self.stack = ExitStack()
        self.stack.__enter__()

    def end(self):
        for eng in ENGS:
            self.finish(eng)
        self.emit()
        self.ops = {e: [] for e in ENGS}
        self.stack.__exit__(None, None, None)
        self.stack = None

    def _deps(self, reads, writes):
        deps = {}

        def add(vk):
            if vk is None:
                return
            v, k = vk
            if deps.get(v, 0) < k:
                deps[v] = k
        for b in reads:
            st = self.buf.get(b)
            if st:
                add(st[0])
        for b in writes:
            st = self.buf.get(b)
            if st:
                add(st[0])
                for r in st[1]:
                    add(r)
        return deps

    def op(self, eng, fn, reads=(), writes=(), slot=None):
        v = eng if slot is None else "dma:" + slot
        if v not in self.sems:
            self._mk(v, 16)
        deps = self._deps(reads, writes)
        for dv, k in deps.items():
            if dv == eng and slot is None:
                if eng == "tensor" or not self.strict or self.cnt[eng] + 1 - k >= 3:
                    continue
            if self.seen[eng].get(dv, 0) >= k:
                continue
            self.seen[eng][dv] = k
            self.ops[eng].append(("w", dv, k * self.inc[dv]))
        self.cnt[v] += 1
        k = self.cnt[v]
        self.ops[eng].append(("i", fn, v))
        for b in reads:
            st = self.buf.setdefault(b, [None, []])
            st[1].append((v, k))
        for b in writes:
            self.buf[b] = [(v, k), []]
        return (v, k)

    def pe(self, r, w, m, *a, **k):
        return self.op("tensor", (m, a, k), r, w)

    def dve(self, r, w, m, *a, **k):
        return self.op("vector", (m, a, k), r, w)

    def act(self, r, w, m, *a, **k):
        return self.op("scalar", (m, a, k), r, w)

    def pool(self, r, w, m, *a, **k):
        return self.op("gpsimd", (m, a, k), r, w)

    def load(self, out_ap, in_ap, key, r=()):
        return self.op("sync", ("dma_start", (), dict(out=out_ap, in_=in_ap)), r, [key], slot=key)

    def store(self, out_ap, in_ap, key, w=()):
        return self.op("gpsimd", ("dma_start", (), dict(out=out_ap, in_=in_ap)), [key], w, slot="st_" + key)

    def finish(self, eng="sync"):
        for v, c in self.cnt.items():
            if c and self.seen[eng].get(v, 0) < c:
                self.ops[eng].append(("w", v, c * self.inc[v]))
                self.seen[eng][v] = c

    def emit(self):
        nc = self.nc
        with nc.Block() as block:
            def run(engname):
                def body(e):
                    for o in self.ops[engname]:
                        if o[0] == "w":
                            e.wait_ge(self.sems[o[1]], o[2])
                        else:
                            getattr(e, o[1][0])(*o[1][1], **o[1][2]).then_inc(self.sems[o[2]], self.inc[o[2]])
                return body
            block.sync(run("sync"))
            block.tensor(run("tensor"))
            block.vector(run("vector"))
            block.scalar(run("scalar"))
            block.gpsimd(run("gpsimd"))


class Rot:
    def __init__(self, tiles, name):
        self.tiles = tiles
        self.name = name
        self.i = 0

    def next(self):
        j = self.i % len(self.tiles)
        self.i += 1
        return self.tiles[j], "%s%d" % (self.name, j)


def sb_rot(P, name, shape, dt, n):
    return Rot([P.sb("%s%d" % (name, j), shape, dt) for j in range(n)], name)


def ps_rot(P, name, n, shape=(128, 512), dt=F32):
    return Rot([P.ps("%s%d" % (name, j), shape, dt) for j in range(n)], name)


NWF = 2592
NWT = 1408
NW1 = NWF + NWT
ST = 384
NST = TC // ST
FEAT_ROWS = 1536


def phase_p1(P, io):
    xs = io["xs"]
    cT = io["cT"]
    ada_w = io["ada_w"]
    ada_b2 = io["ada_b2"]
    ng = io["ng"]
    w1 = io["w1"]
    w2f = io["w2f"]
    tabs = io["tabs"]
    idn = io["idn"]
    modrow = io["modrow"]
    feat = io["feat"]
    tokb = io["tokb"]
    tokf = io["tokf"]
    if True:
        P.begin()
        idf = P.sb("idf", [128, 128], F32)
        cTs = P.sb("cTs", [128, 8, 2], F32)
        scT = P.sb("scT", [128, 8, 2], F32)
        wfb = P.sb("wfb", [128, 8, NW1], BF16)
        w2s = P.sb("w2s", [33, 512], F32)
        ngs = P.sb("ngs", [2, 2, D], F32)
        modsb = P.sb("modsb", [2, 6 * D], F32)
        A1 = [P.sb("A1_%d" % i, [128, D], F32) for i in range(2)]
        B1 = [P.sb("B1_%d" % i, [128, D], F32) for i in range(2)]
        stage = sb_rot(P, "stage", [128, 2048], F32, 2)
        xrot = sb_rot(P, "xt", [128, D], F32, 2)
        hrot = sb_rot(P, "hx", [128, D], F32, 2)
        ssr = sb_rot(P, "ss", [128, 2], F32, 2)
        hT = sb_rot(P, "hT", [128, 8, ST], BF16, 2)
        glT = P.sb("glT", [33, ST], F32)
        tabr = sb_rot(P, "tab", [128, 4, ST], F32, 2)
        t1r = sb_rot(P, "t1", [128, ST], F32, 2)
        t2r = sb_rot(P, "t2", [128, ST], F32, 2)
        fo = sb_rot(P, "fo", [128, ST], BF16, 4)
        tbo = sb_rot(P, "tbo", [128, 1024], BF16, 2)
        tfo = sb_rot(P, "tfo", [128, 896], F32, 2)
        ez = sb_rot(P, "ez", [128, 512], F32, 2)
        pT = ps_rot(P, "pT", 2, (128, 4, 128))
        pg = ps_rot(P, "pg", 6)

        P.load(idf[:], idn, "idf")
        P.load(cTs[:], cT, "cTs")
        P.load(w2s[:], w2f, "w2s")
        P.load(modsb[:], ada_b2, "modsb")
        P.load(ngs[:], ng, "ngs")
        P.act(["cTs"], ["scT"], "activation", out=scT[:], in_=cTs[:], func=AF.Silu)
        P.pool([], ["glT"], "memset", glT[:], 1.0)
        for j in range(24):
            sg, sk = stage.next()
            P.load(sg[:].rearrange("p (c n) -> p c n", c=8), ada_w[:, :, j * 256:(j + 1) * 256], sk)
            pm, pk = pg.next()
            for c in range(8):
                P.pe(["scT", sk], [pk], "matmul", pm[0:2, 0:256], lhsT=scT[:, c, :], rhs=sg[:, c * 256:(c + 1) * 256],
                     start=(c == 0), stop=(c == 7))
            P.dve([pk, "modsb"], ["modsb"], "tensor_tensor", out=modsb[:, j * 256:(j + 1) * 256], in0=pm[0:2, 0:256],
                  in1=modsb[:, j * 256:(j + 1) * 256], op=ALU.add)
        for which in range(2):
            isc = 3 * which + 1
            P.dve(["modsb", "ngs"], ["modsb"], "scalar_tensor_tensor",
                  out=modsb[:, isc * D:(isc + 1) * D], in0=modsb[:, isc * D:(isc + 1) * D], scalar=1.0, in1=ngs[:, which, :],
                  op0=ALU.add, op1=ALU.mult)
        P.store(modrow, modsb[:].rearrange("p (a d) -> p a d", a=6), "modsb", ["MODROW"])
        for v in range(2):
            P.load(A1[v][:], modrow[v:v + 1, 1, :].partition_broadcast(128), "A1_%d" % v, ["MODROW"])
            P.load(B1[v][:], modrow[v:v + 1, 0, :].partition_broadcast(128), "B1_%d" % v, ["MODROW"])
        HW1 = NW1 // 2
        for c in range(16):
            sg, sk = stage.next()
            kc, hf = divmod(c, 2)
            P.load(sg[:, 0:HW1], w1[:, kc, hf * HW1:(hf + 1) * HW1], sk)
            (P.dve if c % 2 == 0 else P.pool)([sk], ["wfb"], "tensor_copy", out=wfb[:, kc, hf * HW1:(hf + 1) * HW1], in_=sg[:, 0:HW1])

        rope_pairs = [(0, 2, 0, 0), (1, 3, 0, 128), (4, 6, 0, 256), (5, 7, 0, 384),
                      (8, 11, 2, 512), (9, 12, 2, 640), (10, 13, 2, 768), (14, 15, 2, 896)]
        plain = [(16, 1024, 48 ** -0.5), (17, 1152, 48 ** -0.5), (18, 1280, 1.0), (19, 1408, 1.0)]
        for s in range(NST):
            hTt, hk = hT.next()
            tb_, tk = tabr.next()
            P.load(tb_[:], tabs[:, :, s * ST:(s + 1) * ST], tk)
            for t in range(3):
                g = 3 * s + t
                v = 1 if g < 2 else 0
                xt_, xk = xrot.next()
                P.load(xt_[:], xs[g * 128:(g + 1) * 128, :], xk)
                ss_, sk_ = ssr.next()
                hx_, hxk = hrot.next()
                P.act([xk], [hxk, sk_], "activation", out=hx_[:], in_=xt_[:], func=AF.Square, accum_out=ss_[:, 0:1])
                P.dve([sk_], [sk_], "tensor_scalar", out=ss_[:, 1:2], in0=ss_[:, 0:1], scalar1=1.0 / D, scalar2=EPS, op0=ALU.mult, op1=ALU.add)
                P.act([sk_], [sk_], "activation", out=ss_[:, 1:2], in_=ss_[:, 1:2], func=AF.Sqrt)
                P.dve([sk_], [sk_], "reciprocal", out=ss_[:, 1:2], in_=ss_[:, 1:2])
                P.dve([xk, sk_, "A1_%d" % v], [hxk], "scalar_tensor_tensor",
                      out=hx_[:], in0=xt_[:], scalar=ss_[:, 1:2], in1=A1[v][:], op0=ALU.mult, op1=ALU.mult)
                P.pool([hxk, "B1_%d" % v], [hxk], "tensor_tensor", out=hx_[:], in0=hx_[:], in1=B1[v][:], op=ALU.add)
                for hf in range(2):
                    pt_, ptk = pT.next()
                    for c in range(4):
                        P.pe([hxk, "idf"], [ptk], "transpose", out=pt_[:, c, :], in_=hx_[:, (4 * hf + c) * 128:(4 * hf + c + 1) * 128], identity=idf[:])
                    if hf == 0:
                        P.act([ptk], [hk], "activation", out=hTt[:, 0:4, t * 128:(t + 1) * 128], in_=pt_[:], func=AF.Copy)
                    else:
                        P.dve([ptk], [hk], "tensor_copy", out=hTt[:, 4:8, t * 128:(t + 1) * 128], in_=pt_[:])

            def fm(pd, pk_, f0, ncols, hTt, hk):
                for c in range(8):
                    P.pe(["wfb", hk], [pk_], "matmul", pd[0:ncols, 0:ST], lhsT=wfb[:, c, f0:f0 + ncols], rhs=hTt[:, c, :],
                         start=(c == 0), stop=(c == 7))

            for (fx, fp, ti, row0) in rope_pairs:
                px, pxk = pg.next()
                pp, ppk = pg.next()
                fm(px, pxk, fx * 128, 128, hTt, hk)
                fm(pp, ppk, fp * 128, 128, hTt, hk)
                a_, ak = t1r.next()
                b_, bk = t2r.next()
                P.dve([pxk, tk], [ak], "tensor_tensor", out=a_[:], in0=px[:, 0:ST], in1=tb_[:, ti, :], op=ALU.mult)
                P.dve([ppk, tk], [bk], "tensor_tensor", out=b_[:], in0=pp[:, 0:ST], in1=tb_[:, ti + 1, :], op=ALU.mult)
                o_, ok = fo.next()
                P.pool([ak, bk], [ok], "tensor_tensor", out=o_[:], in0=a_[:], in1=b_[:], op=ALU.add)
                P.store(feat[row0:row0 + 128, s * ST:(s + 1) * ST], o_[:], ok)
            for (f, row0, scl) in plain:
                px, pxk = pg.next()
                fm(px, pxk, f * 128, 128, hTt, hk)
                o_, ok = fo.next()
                P.act([pxk], [ok], "activation", out=o_[:], in_=px[:, 0:ST], func=AF.Copy, scale=scl)
                P.store(feat[row0:row0 + 128, s * ST:(s + 1) * ST], o_[:], ok)
            px, pxk = pg.next()
            fm(px, pxk, 2560, 32, hTt, hk)
            P.act([pxk], ["glT"], "activation", out=glT[0:32, :], in_=px[0:32, 0:ST], func=AF.Copy)
            for t in range(3):
                g = 3 * s + t
                tb2, tbk = tbo.next()
                tf2, tfk = tfo.next()
                for (c0, n) in ((0, 512), (512, 512), (1024, 384)):
                    px, pxk = pg.next()
                    for c in range(8):
                        P.pe(["wfb", hk], [pxk], "matmul", px[:, 0:n], lhsT=hTt[:, c, t * 128:(t + 1) * 128],
                             rhs=wfb[:, c, NWF + c0:NWF + c0 + n], start=(c == 0), stop=(c == 7))
                    if c0 < 1024:
                        P.dve([pxk], [tbk], "tensor_copy", out=tb2[:, c0:c0 + 512], in_=px[:, 0:512])
                    else:
                        P.act([pxk], [tfk], "activation", out=tf2[:, 0:384], in_=px[:, 0:384], func=AF.Silu)
                pz, pzk = pg.next()
                P.pe(["glT", "w2s"], [pzk], "matmul", pz[:, :], lhsT=glT[0:33, t * 128:(t + 1) * 128], rhs=w2s[:, :], start=True, stop=True)
                ez_, ezk = ez.next()
                P.act([pzk], [ezk], "activation", out=ez_[:], in_=pz[:], func=AF.Exp, scale=-1.0)
                P.act([ezk], [tfk], "activation", out=tf2[:, 384:896], in_=ez_[:], func=AF.Ln, bias=1.0)
                P.store(tokb[g * 128:(g + 1) * 128, :], tb2[:], tbk)
                P.store(tokf[g * 128:(g + 1) * 128, :], tf2[:], tfk)
        P.end()


A_Q0, A_K0, A_V0 = 0, 256, 512
B_Q0, B_K0, B_V0 = 768, 1152, 1280
C_Q0, C_K0, C_V0, C_R0, C_G0 = 1408, 1600, 1792, 2176, 2560


def _rope_perm(dim):
    q = dim // 4
    perm = np.zeros(dim, np.int64)
    sign = np.zeros(dim, np.float32)
    for d in range(dim):
        blk = d // q
        if blk % 2 == 0:
            perm[d] = d + q
            sign[d] = -1.0
        else:
            perm[d] = d - q
            sign[d] = 1.0
    return perm, sign


def w1_columns():
    cols = []
    pA, _ = _rope_perm(32)
    pB, _ = _rope_perm(64)

    def permuted(base, n, dim, perm):
        out = []
        for j in range(n):
            hd, d = divmod(j, dim)
            out.append(base + hd * dim + int(perm[d]))
        return out
    cols += list(range(A_Q0, A_Q0 + 256)) + permuted(A_Q0, 256, 32, pA)
    cols += list(range(A_K0, A_K0 + 256)) + permuted(A_K0, 256, 32, pA)
    cols += list(range(B_Q0, B_Q0 + 384)) + permuted(B_Q0, 384, 64, pB)
    cols += list(range(B_K0, B_K0 + 128)) + permuted(B_K0, 128, 64, pB)

    def padded(base):
        out = []
        for h in range(4):
            out += list(range(base + 48 * h, base + 48 * h + 48)) + [-1] * 16
        return out
    cols += padded(C_Q0) + padded(C_K0)
    cols += list(range(C_G0, C_G0 + 32))
    assert len(cols) == NWF
    cols += list(range(A_V0, A_V0 + 256)) + list(range(B_V0, B_V0 + 128)) + list(range(C_V0, C_V0 + 384))
    cols += padded(C_K0) + list(range(C_R0, C_R0 + 384))
    assert len(cols) == NW1
    return np.array(cols, np.int64)


def take_cols(w, cols):
    out = np.zeros((w.shape[0], len(cols)), w.dtype)
    m = cols >= 0
    out[:, m] = w[:, cols[m]]
    return out


def kmajor(w):
    K, N = w.shape
    return np.ascontiguousarray(w.reshape(K // 128, 128, N).transpose(1, 0, 2))


def rope_tables():
    out = np.zeros((4, 128, TB), np.float32)
    tok = np.arange(SEQ)
    row = (tok // 64).astype(np.float32)
    col = (tok % 64).astype(np.float32)
    for ti, dim in ((0, 32), (2, 64)):
        q = dim // 4
        inv = (10000.0 ** (-np.arange(q, dtype=np.float32) / q)).astype(np.float32)
        ang_r = row[:, None] * inv[None, :]
        ang_c = col[:, None] * inv[None, :]
        _, sign = _rope_perm(dim)
        cosd = np.zeros((dim, SEQ), np.float32)
        sind = np.zeros((dim, SEQ), np.float32)
        for d in range(dim):
            ang = ang_r if d < dim // 2 else ang_c
            cosd[d] = np.cos(ang[:, d % q])
            sind[d] = sign[d] * np.sin(ang[:, d % q])
        reps = 128 // dim
        out[ti, :, :CTX] = 1.0
        out[ti + 1, :, :CTX] = 0.0
        out[ti, :, CTX:] = np.tile(cosd, (reps, 1))
        out[ti + 1, :, CTX:] = np.tile(sind, (reps, 1))
    return out


def w2full(w2, bg):
    out = np.zeros((33, 512), np.float32)
    for d in range(2):
        for h in range(4):
            c0 = d * 256 + h * 64
            out[16 * d:16 * d + 16, c0:c0 + 48] = w2[d][:, 48 * h:48 * h + 48]
            out[32, c0:c0 + 48] = bg[d][48 * h:48 * h + 48]
    return out


NT = TB // 128


def phase_p2a(P, io):
    scale = 32 ** -0.5
    aqt = io["aqt"]
    akt = io["akt"]
    av = io["av"]
    lamb = io["lamb"]
    cst = io["cst"]
    idn = io["idn"]
    mo = io["mo"]
    if True:
        P.begin()
        qT = P.sb("qT", [128, TB], BF16)
        kT = P.sb("kT", [128, TB], BF16)
        va = P.sb("va", [128, NT, 2, 65], BF16)
        idf = P.sb("idf", [128, 128], F32)
        lb = P.sb("lb", [128, 4, 32], F32)
        cs = P.sb("cs", [128, 66], F32)
        sm = P.sb("sm", [128, 8], F32)
        tmp32 = P.sb("tmp32", [128, 32], F32)
        gfin = P.sb("gfin", [128, 64], F32)
        pt = sb_rot(P, "pt", [128, 1024], BF16, 3)
        oT = sb_rot(P, "oT", [65, 512], F32, 2)
        rec = sb_rot(P, "rec", [128, 2, 4], F32, 2)
        d1 = sb_rot(P, "d1", [128, 64], F32, 2)
        dd = sb_rot(P, "dd", [128, 64], F32, 2)
        jk = sb_rot(P, "jk", [128, 64], F32, 2)
        ssr = sb_rot(P, "ssa", [128, 2], F32, 2)
        mot = sb_rot(P, "mot", [128, 4, 128], F32, 2)
        psb = ps_rot(P, "psD", 2, (128, 1024))
        pob = ps_rot(P, "poT", 2)
        ptr = ps_rot(P, "ptr", 1)
        osb = sb_rot(P, "osb", [128, 4, 65], F32, 4)
        P.load(qT[:], aqt, "qT")
        P.load(kT[:], akt, "kT")
        P.load(lb[:], lamb, "lb")
        P.load(cs[:], cst, "cs")
        P.load(idf[:], idn, "idf")
        P.pool([], ["va"], "memset", va[:], 1.0)
        for h in range(2):
            P.load(va[:, :, h, 0:64], av[:, h * 64:(h + 1) * 64].rearrange("(n p) d -> p n d", p=128), "va")
        for i in range(2):
            P.dve(["lb"], ["tmp32"], "tensor_tensor", out=tmp32[:], in0=lb[:, 2 * i, :], in1=lb[:, 2 * i + 1, :], op=ALU.mult)
            P.dve(["tmp32"], ["sm"], "reduce_sum", out=sm[:, i:i + 1], in_=tmp32[:], axis=AX.X)
        P.act(["sm"], ["sm"], "activation", out=sm[:, 0:2], in_=sm[:, 0:2], func=AF.Exp)
        P.dve(["sm"], ["sm"], "tensor_tensor", out=sm[:, 2:3], in0=sm[:, 0:1], in1=sm[:, 1:2], op=ALU.subtract)
        P.dve(["sm", "cs"], ["sm"], "tensor_tensor", out=sm[:, 2:3], in0=sm[:, 2:3], in1=cs[:, 64:65], op=ALU.add)
        P.dve(["sm"], ["sm"], "tensor_scalar", out=sm[:, 3:4], in0=sm[:, 2:3], scalar1=-1.0, scalar2=None, op0=ALU.mult)
        P.dve(["cs"], ["gfin"], "tensor_scalar", out=gfin[:], in0=cs[:, 0:64], scalar1=cs[:, 65:66], scalar2=None, op0=ALU.mult)

        groups = [(0, 2, 2)] + [(2 + 4 * g, 4, NT) for g in range(16)]
        steps = []
        for (t0, nq, nk) in groups:
            for hl in range(2):
                for m in range(2):
                    for kp in range(nk // 2):
                        steps.append((t0, nq, nk, hl, m, kp))

        def emit_scores(st_):
            t0, nq, nk, hl, m, kp = st_
            nqc = nq * 128
            j = 2 * hl + m
            kw = dict(tile_position=(96, 0)) if j == 3 else {}
            ps_, psk = psb.next()
            for u in range(2):
                kt = 2 * kp + u
                P.pe(["kT", "qT"], [psk], "matmul", ps_[:, u * 512:u * 512 + nqc], lhsT=kT[32 * j:32 * j + 32, kt * 128:(kt + 1) * 128],
                     rhs=qT[32 * j:32 * j + 32, t0 * 128:t0 * 128 + nqc], start=True, stop=True, **kw)
            return ps_, psk

        nxt = emit_scores(steps[0])
        pos = []
        po = pok = mt = mk = None
        for si, st_ in enumerate(steps):
            t0, nq, nk, hl, m, kp = st_
            nqc = nq * 128
            ps_, psk = nxt
            if si + 1 < len(steps):
                nxt = emit_scores(steps[si + 1])
            if kp == 0:
                po, pok = pob.next()
                if hl == 0 and m == 0:
                    mt, mk = mot.next()
                if m == 0:
                    pos = []
            p_, pk_ = pt.next()
            P.act([psk], [pk_], "activation", out=p_[:].rearrange("p (u n) -> p u n", u=2)[:, :, 0:nqc],
                  in_=ps_[:].rearrange("p (u n) -> p u n", u=2)[:, :, 0:nqc], func=AF.Exp, scale=scale)
            for u in range(2):
                kt = 2 * kp + u
                P.pe([pk_, "va"], [pok], "matmul", po[0:65, 0:nqc], lhsT=va[:, kt, hl, :], rhs=p_[:, u * 512:u * 512 + nqc],
                     start=(kt == 0), stop=(kt == nk - 1))
            if kp != nk // 2 - 1:
                continue
            o_, ok_ = oT.next()
            P.act([pok], [ok_], "activation", out=o_[:, 0:nqc], in_=po[0:65, 0:nqc], func=AF.Copy)
            tr, trk = ptr.next()
            for qb in range(nq):
                P.pe([ok_, "idf"], [trk], "transpose", out=tr[:, qb * 65:(qb + 1) * 65], in_=o_[:, qb * 128:(qb + 1) * 128], identity=idf[0:65, 0:65])
            os_, osk = osb.next()
            P.dve([trk], [osk], "tensor_copy", out=os_[:, 0:nq, :], in_=tr[:, 0:nq * 65].rearrange("p (q d) -> p q d", d=65))
            pos.append((os_, osk))
            if m == 0:
                continue
            if True:
                (po1, k1), (po2, k2) = pos
                rc, rck = rec.next()
                P.dve([k1], [rck], "reciprocal", out=rc[:, 0, 0:nq], in_=po1[:, 0:nq, 64])
                P.dve([k2], [rck], "reciprocal", out=rc[:, 1, 0:nq], in_=po2[:, 0:nq, 64])
                P.dve([rck, "sm"], [rck], "tensor_scalar", out=rc[:, 1, 0:nq], in0=rc[:, 1, 0:nq], scalar1=sm[:, 3:4], scalar2=None, op0=ALU.mult)
                for qb in range(nq):
                    a_, ak = d1.next()
                    P.dve([k1, rck], [ak], "tensor_scalar", out=a_[:], in0=po1[:, qb, 0:64], scalar1=rc[:, 0, qb:qb + 1], scalar2=None, op0=ALU.mult)
                    d_, dk = dd.next()
                    P.dve([k2, rck, ak], [dk], "scalar_tensor_tensor", out=d_[:], in0=po2[:, qb, 0:64], scalar=rc[:, 1, qb:qb + 1], in1=a_[:],
                          op0=ALU.mult, op1=ALU.add)
                    j_, jkk = jk.next()
                    s_, sk_ = ssr.next()
                    P.act([dk], [jkk, sk_], "activation", out=j_[:], in_=d_[:], func=AF.Square, accum_out=s_[:, 0:1])
                    P.dve([sk_], [sk_], "tensor_scalar", out=s_[:, 1:2], in0=s_[:, 0:1], scalar1=1.0 / 64, scalar2=EPS, op0=ALU.mult, op1=ALU.add)
                    P.act([sk_], [sk_], "activation", out=s_[:, 1:2], in_=s_[:, 1:2], func=AF.Sqrt)
                    P.dve([sk_], [sk_], "reciprocal", out=s_[:, 1:2], in_=s_[:, 1:2])
                    P.dve([dk, sk_, "gfin"], [mk], "scalar_tensor_tensor", out=mt[:, qb, hl * 64:(hl + 1) * 64], in0=d_[:], scalar=s_[:, 1:2],
                          in1=gfin[:], op0=ALU.mult, op1=ALU.mult)
            if hl == 1:
                P.store(mo[t0 * 128:(t0 + nq) * 128, :].rearrange("(q p) c -> p q c", p=128), mt[:, 0:nq, :], mk)
        P.end()


def phase_p2b(P, io):
    scale = 64 ** -0.5
    bqt = io["bqt"]
    bkt = io["bkt"]
    bv = io["bv"]
    sink = io["sink"]
    masks = io["masks"]
    mo = io["mo"]
    if True:
        P.begin()
        qT = P.sb("qT", [64, 3, TB], BF16)
        kT = P.sb("kT", [64, TB], BF16)
        va = P.sb("va", [128, NT, 65], BF16)
        sk = P.sb("sk", [128, 3], F32)
        mk = P.sb("mk", [128, 2, 3, 128], F32)
        pe_ = sb_rot(P, "pe", [128, 3, 128], BF16, 10)
        den = sb_rot(P, "den", [128, 3], F32, 2)
        mot = sb_rot(P, "mot", [128, 3, 64], F32, 3)
        psb = ps_rot(P, "ps", 3)
        pob = ps_rot(P, "po", 2, (128, 3, 65))
        P.load(qT[:], bqt.rearrange("(h d) t -> d h t", d=64), "qT")
        P.load(kT[:], bkt, "kT")
        P.load(sk[:], sink, "sk")
        P.load(mk[:], masks, "mk")
        P.pool([], ["va"], "memset", va[:], 1.0)
        P.load(va[:, :, 0:64], bv.rearrange("(n p) d -> p n d", p=128), "va")
        P.act(["sk"], ["sk"], "activation", out=sk[:], in_=sk[:], func=AF.Exp)
        for n in range(NT):
            if n < 2:
                kts = [(0, None), (1, None)]
            else:
                kts = [(0, None), (1, None)]
                if n - 1 >= 2:
                    kts.append((n - 1, 0))
                kts.append((n, None))
                if n + 1 < NT:
                    kts.append((n + 1, 1))
            po, pok = pob.next()
            pts = []
            for i, (kt, msk) in enumerate(kts):
                ps_, psk = psb.next()
                P.pe(["kT", "qT"], [psk], "matmul", ps_[:, 0:384].rearrange("p (h q) -> p h q", h=3), lhsT=kT[:, kt * 128:(kt + 1) * 128],
                     rhs=qT[:, :, n * 128:(n + 1) * 128], start=True, stop=True)
                p_, pk_ = pe_.next()
                P.act([psk], [pk_], "activation", out=p_[:], in_=ps_[:, 0:384].rearrange("p (h q) -> p h q", h=3), func=AF.Exp, scale=scale)
                if msk is not None:
                    P.dve([pk_, "mk"], [pk_], "tensor_tensor", out=p_[:], in0=p_[:], in1=mk[:, msk, :, :], op=ALU.mult)
                pts.append((p_, pk_, kt))
            for h in range(3):
                for i, (p_, pk_, kt) in enumerate(pts):
                    P.pe([pk_, "va"], [pok], "matmul", po[:, h, :], lhsT=p_[:, h, :], rhs=va[:, kt, :], start=(i == 0), stop=(i == len(pts) - 1))
            dn, dnk = den.next()
            P.dve([pok, "sk"], [dnk], "tensor_tensor", out=dn[:], in0=po[:, :, 64], in1=sk[:], op=ALU.add)
            P.dve([dnk], [dnk], "reciprocal", out=dn[:], in_=dn[:])
            mt, mtk = mot.next()
            for h in range(3):
                P.dve([pok, dnk], [mtk], "tensor_scalar", out=mt[:, h, :], in0=po[:, h, 0:64], scalar1=dn[:, h:h + 1], scalar2=None, op0=ALU.mult)
            P.store(mo[n * 128:(n + 1) * 128, :], mt[:].rearrange("p h d -> p (h d)"), mtk)
        P.end()


def band_masks():
    j = np.arange(128)[:, None]
    i = np.arange(128)[None, :]
    m = np.zeros((128, 2, 3, 128), np.float32)
    m[:, 0, :, :] = (i <= j).astype(np.float32)[:, None, :]
    m[:, 1, :, :] = (j <= i).astype(np.float32)[:, None, :]
    return m


NCH = TB // 64


def gla_consts():
    s_ = np.arange(64)[:, None]
    t_ = np.arange(64)[None, :]
    tri = np.zeros((64, 2, 65), np.float32)
    trix = np.zeros((64, 2, 64), np.float32)
    mask = np.zeros((64, 2, 2, 64), np.float32)
    c = -1.0 / 16.0
    tri[:, 0, :64] = c * (s_ <= t_)
    tri[:, 1, :64] = c * (s_ >= t_)
    tri[:, :, 64] = c
    trix[:, 0, :] = c * (s_ > t_)
    trix[:, 1, :] = c * (s_ < t_)
    mask[:, 0, :, :] = (s_ <= t_).astype(np.float32)[:, None, :]
    mask[:, 1, :, :] = (s_ >= t_).astype(np.float32)[:, None, :]
    return tri, trix, mask


def phase_p2c(P, io):
    cqt = io["cqt"]
    ckt = io["ckt"]
    cktok = io["cktok"]
    cv = io["cv"]
    crs = io["crs"]
    sp = io["sp"]
    tri_d = io["tri_d"]
    trix_d = io["trix_d"]
    mask_d = io["mask_d"]
    gc_d = io["gc_d"]
    mo = io["mo"]
    if True:
        P.begin()
        qT = P.sb("qT", [64, 2, TB], BF16)
        kT = P.sb("kT", [64, 2, TB], BF16)
        OF = P.sb("OF", [64, NCH, 192], F32)
        tri = P.sb("tri_s", [64, 2, 65], F32)
        trix = P.sb("trix_s", [64, 2, 64], F32)
        mask = P.sb("mask_s", [64, 2, 2, 64], F32)
        gc = P.sb("gc_s", [64, 192], F32)
        S = [P.sb("S%d" % d, [64, 2, 96], F32) for d in range(2)]
        Sb = [P.sb("Sb%d" % d, [64, 2, 96], BF16) for d in range(2)]
        spr = sb_rot(P, "spc", [64, 2, 128], F32, 6)
        ktr = sb_rot(P, "ktk", [64, 128], BF16, 6)
        vr = sb_rot(P, "vv", [64, 192], BF16, 6)
        rr = sb_rot(P, "rs", [64, 192], F32, 4)
        E1 = sb_rot(P, "E1", [64, 2, 65], F32, 4)
        E2 = sb_rot(P, "E2", [64, 2, 64], F32, 4)
        E3 = sb_rot(P, "E3", [64, 128], F32, 4)
        qd = sb_rot(P, "qd", [64, 2, 64], BF16, 4)
        ki = sb_rot(P, "ki", [64, 2, 64], BF16, 4)
        ke = sb_rot(P, "ke", [64, 128], BF16, 4)
        att = sb_rot(P, "att", [64, 2, 64], BF16, 4)
        osum = sb_rot(P, "osum", [64, 192], F32, 2)
        jk = sb_rot(P, "jk", [64, 96], F32, 2)
        ssr = sb_rot(P, "ssc", [64, 4], F32, 2)
        yo = sb_rot(P, "yo", [64, 192], F32, 3)
        pb = ps_rot(P, "pb", 2)
        pbd = ps_rot(P, "pbd", 1)
        patt = ps_rot(P, "patt", 2)
        po = ps_rot(P, "po", 2)
        pu = ps_rot(P, "pu", 1)
        P.load(qT[:], cqt.rearrange("(h d) t -> d h t", d=64), "qT")
        P.load(kT[:], ckt.rearrange("(h d) t -> d h t", d=64), "kT")
        P.load(tri[:], tri_d, "tri")
        P.load(trix[:], trix_d, "trix")
        P.load(mask[:], mask_d, "mask")
        P.load(gc[:], gc_d, "gc")
        for d in range(2):
            P.pool([], ["S%d" % d], "memset", S[d][:], 0.0)
            P.pool([], ["Sb%d" % d], "memset", Sb[d][:], 0.0)
        fwd = [(c, 0) for c in range(NCH)]
        bwd = [(c, 1) for c in (3, 2, 1, 0)] + [(c, 1) for c in range(NCH - 1, 3, -1)]
        order = [x for pair in zip(fwd, bwd) for x in pair]
        seen_c = set()
        for (c, d) in order:
            second = c in seen_c
            seen_c.add(c)
            tk = slice(c * 64, (c + 1) * 64)
            sp_, spk = spr.next()
            P.load(sp_[:], sp[tk, :, :], spk)
            kt_, ktk = ktr.next()
            P.load(kt_[:], cktok[tk, :], ktk)
            v_, vk = vr.next()
            P.load(v_[:], cv[tk, :], vk)
            if second:
                r_, rk = rr.next()
                P.load(r_[:], crs[tk, :], rk)
            pb_, pbk = pb.next()
            for h in range(2):
                P.pe([spk, "tri"], [pbk], "matmul", pb_[0:64, h * 65:(h + 1) * 65], lhsT=sp_[:, d, 64 * h:64 * h + 64], rhs=tri[:, d, :], start=True, stop=True)
            pbd_, pbdk = pbd.next()
            P.pe([spk, "trix"], [pbdk], "matmul", pbd_[0:64, 0:128], lhsT=trix[:, d, :], rhs=sp_[:, d, :], start=True, stop=True)
            e1, e1k = E1.next()
            e2, e2k = E2.next()
            e3, e3k = E3.next()
            pbv = pb_[0:64, 0:130].rearrange("p (h n) -> p h n", h=2)
            P.act([pbk], [e1k], "activation", out=e1[:], in_=pbv, func=AF.Exp)
            P.act([pbk], [e2k], "activation", out=e2[:], in_=pbv[:, :, 0:64], func=AF.Exp, scale=-1.0)
            P.act([pbdk], [e3k], "activation", out=e3[:], in_=pbd_[0:64, 0:128], func=AF.Exp)
            qd_, qdk = qd.next()
            ki_, kik = ki.next()
            ke_, kek = ke.next()
            P.dve(["qT", e1k], [qdk], "tensor_tensor", out=qd_[:], in0=qT[:, :, tk], in1=e1[:, :, 0:64], op=ALU.mult)
            P.dve(["kT", e2k], [kik], "tensor_tensor", out=ki_[:], in0=kT[:, :, tk], in1=e2[:], op=ALU.mult)
            P.pool([ktk, e3k], [kek], "tensor_tensor", out=ke_[:], in0=kt_[:], in1=e3[:], op=ALU.mult)
            pa_, pak = patt.next()
            pav = pa_[0:64, 0:128].rearrange("p (h n) -> p h n", h=2)
            for h in range(2):
                P.pe([kik, qdk], [pak], "matmul", pa_[0:64, h * 64:(h + 1) * 64], lhsT=ki_[:, h, :], rhs=qd_[:, h, :], start=True, stop=True)
            at_, atk = att.next()
            P.dve([pak, "mask"], [atk], "tensor_tensor", out=at_[:], in0=pav, in1=mask[:, d, :, :], op=ALU.mult)
            po_, pok = po.next()
            for h in range(2):
                P.pe([atk, vk], [pok], "matmul", po_[0:64, 96 * h:96 * h + 96], lhsT=at_[:, h, :], rhs=v_[:, 96 * h:96 * h + 96], start=True, stop=False)
                P.pe([qdk, "Sb%d" % d], [pok], "matmul", po_[0:64, 96 * h:96 * h + 96], lhsT=qd_[:, h, :], rhs=Sb[d][:, h, :], start=False, stop=True)
            pu_, puk = pu.next()
            for h in range(2):
                P.pe([kek, vk], [puk], "matmul", pu_[0:64, 96 * h:96 * h + 96], lhsT=ke_[:, 64 * h:64 * h + 64], rhs=v_[:, 96 * h:96 * h + 96], start=True, stop=True)
            for h in range(2):
                P.dve(["S%d" % d, e1k, puk], ["S%d" % d], "scalar_tensor_tensor", out=S[d][:, h, :], in0=S[d][:, h, :], scalar=e1[:, h, 64:65],
                      in1=pu_[0:64, 96 * h:96 * h + 96], op0=ALU.mult, op1=ALU.add)
            P.pool(["S%d" % d], ["Sb%d" % d], "tensor_copy", out=Sb[d][:], in_=S[d][:])
            if not second:
                P.act([pok], ["OF%d" % c], "activation", out=OF[:, c, :], in_=po_[0:64, 0:192], func=AF.Copy)
            else:
                os_, osk = osum.next()
                P.dve([pok, "OF%d" % c], [osk], "tensor_tensor", out=os_[:], in0=po_[0:64, 0:192], in1=OF[:, c, :], op=ALU.add)
                s_, sk_ = ssr.next()
                for h in range(2):
                    j_, jkk = jk.next()
                    P.act([osk], [jkk, sk_], "activation", out=j_[:], in_=os_[:, 96 * h:96 * h + 96], func=AF.Square, accum_out=s_[:, h:h + 1])
                P.dve([sk_], [sk_], "tensor_scalar", out=s_[:, 2:4], in0=s_[:, 0:2], scalar1=1.0 / 96, scalar2=EPS, op0=ALU.mult, op1=ALU.add)
                P.act([sk_], [sk_], "activation", out=s_[:, 2:4], in_=s_[:, 2:4], func=AF.Sqrt)
                P.dve([sk_], [sk_], "reciprocal", out=s_[:, 2:4], in_=s_[:, 2:4])
                y_, yk = yo.next()
                for h in range(2):
                    P.dve([osk, sk_, "gc"], [yk], "scalar_tensor_tensor", out=y_[:, 96 * h:96 * h + 96], in0=os_[:, 96 * h:96 * h + 96],
                          scalar=s_[:, 2 + h:3 + h], in1=gc[:, 96 * h:96 * h + 96], op0=ALU.mult, op1=ALU.mult)
                P.pool([yk, rk], [yk], "tensor_tensor", out=y_[:], in0=y_[:], in1=r_[:], op=ALU.mult)
                P.store(mo[tk, :], y_[:], yk)
        P.end()


def phase_p3(P, io, E, FF, moe):
    FC = FF // 128
    NG = FF // 256
    xs = io["xs"]
    mo = io["mo"]
    modrow = io["modrow"]
    wout = io["wout"]
    router = io["router"]
    wg = io["wg"]
    wu = io["wu"]
    wd = io["wd"]
    idn = io["idn"]
    xo = io["xo"]
    wgd, wud, wdd = io["wgd"], io["wud"], io["wdd"]
    if True:
        P.begin()
        idf = P.sb("idf", [128, 128], F32)
        woutb = P.sb("woutb", [128, 8, D], BF16)
        rts = P.sb("rts", [128, 8, 8], F32)
        G1 = [P.sb("G1_%d" % i, [128, D], F32) for i in range(2)]
        A2 = [P.sb("A2_%d" % i, [128, D], F32) for i in range(2)]
        B2 = [P.sb("B2_%d" % i, [128, D], F32) for i in range(2)]
        G2 = [P.sb("G2_%d" % i, [128, D], F32) for i in range(2)]
        stage = sb_rot(P, "stage", [128, 2048], F32, 3)
        xrot = sb_rot(P, "xt", [128, D], F32, 2)
        mrot = sb_rot(P, "mt", [128, D], F32, 2)
        xnew = sb_rot(P, "xn", [128, D], F32, 3)
        yacc = sb_rot(P, "ya", [128, D], F32, 3)
        hrot = sb_rot(P, "hx", [128, D], F32, 2)
        ssr = sb_rot(P, "ss", [128, 2], F32, 2)
        catT = sb_rot(P, "catT", [128, 8, 128], BF16, 2)
        h2T = sb_rot(P, "h2T", [128, 8, ST], BF16, 2)
        h2Tf = P.sb("h2Tf", [128, 8, ST], F32) if moe else None
        cw = sb_rot(P, "cw", [128, 3, 8], F32, 2)
        lg = sb_rot(P, "lg", [128, 8], F32, 2)
        rt = sb_rot(P, "rtmp", [128, 4, 8], F32, 2)
        rs_ = sb_rot(P, "rsc", [128, 4], F32, 2)
        wgb = sb_rot(P, "wgb", [128, 8, 256], BF16, 2)
        wub = sb_rot(P, "wub", [128, 8, 256], BF16, 2)
        wdb = sb_rot(P, "wdb", [128, 2, D], BF16, 2)
        sgr = sb_rot(P, "sg", [128, ST], F32, 2)
        aTr = sb_rot(P, "aT", [128, ST], BF16, 3)
        bank = [P.ps("bk%d" % i, [128, 512], F32) for i in range(8)]
        bk = ["bk%d" % i for i in range(8)]
        P.load(idf[:], idn, "idf")
        if moe:
            P.load(rts[:], router, "rts")
        for v in range(2):
            P.load(G1[v][:], modrow[v:v + 1, 2, :].partition_broadcast(128), "G1_%d" % v)
            P.load(B2[v][:], modrow[v:v + 1, 3, :].partition_broadcast(128), "B2_%d" % v)
            P.load(A2[v][:], modrow[v:v + 1, 4, :].partition_broadcast(128), "A2_%d" % v)
            P.load(G2[v][:], modrow[v:v + 1, 5, :].partition_broadcast(128), "G2_%d" % v)
        for c in range(8):
            sg, sk = stage.next()
            P.load(sg[:, 0:D], wout[:, c, :], sk)
            (P.dve if c % 2 == 0 else P.pool)([sk], ["woutb"], "tensor_copy", out=woutb[:, c, :], in_=sg[:, 0:D])
        ccast = 0
        for e in range(E):
            for gi in range(NG):
                for (src_, dst_, rot_, shp) in ((wg[e, :, :, gi * 256:(gi + 1) * 256], wgd[e, :, :, gi * 256:(gi + 1) * 256], wgb, 8),
                                                (wu[e, :, :, gi * 256:(gi + 1) * 256], wud[e, :, :, gi * 256:(gi + 1) * 256], wub, 8),
                                                (wd[e, :, 2 * gi:2 * gi + 2, :], wdd[e, :, 2 * gi:2 * gi + 2, :], wdb, 2)):
                    sg, sk = stage.next()
                    P.load(sg[:].rearrange("p (c n) -> p c n", c=shp), src_, sk)
                    wb_, wbk = rot_.next()
                    if ccast % 3 == 0:
                        P.dve([sk], [wbk], "tensor_copy", out=wb_[:], in_=sg[:].rearrange("p (c n) -> p c n", c=shp))
                    elif ccast % 3 == 1:
                        P.pool([sk], [wbk], "tensor_copy", out=wb_[:], in_=sg[:].rearrange("p (c n) -> p c n", c=shp))
                    else:
                        P.act([sk], [wbk], "activation", out=wb_[:], in_=sg[:].rearrange("p (c n) -> p c n", c=shp), func=AF.Copy)
                    ccast += 1
                    P.store(dst_, wb_[:], wbk)
        for eng in ENGS:
            P.finish(eng)
        for s in range(NST):
            h2, h2k = h2T.next()
            cw_, cwk = cw.next()
            xns = []
            yas = []
            for t in range(3):
                g = 3 * s + t
                v = 1 if g < 2 else 0
                mt, mtk = mrot.next()
                P.load(mt[:], mo[g * 128:(g + 1) * 128, :], mtk)
                xt_, xk = xrot.next()
                P.load(xt_[:], xs[g * 128:(g + 1) * 128, :], xk)
                ct, ctk = catT.next()
                for hf in range(2):
                    for c in range(4):
                        P.pe([mtk, "idf"], [bk[6 + hf]], "transpose", out=bank[6 + hf][:, c * 128:(c + 1) * 128],
                             in_=mt[:, (4 * hf + c) * 128:(4 * hf + c + 1) * 128], identity=idf[:])
                    (P.act if hf == 0 else P.dve)([bk[6 + hf]], [ctk], *(("activation",) if hf == 0 else ("tensor_copy",)),
                                                  **(dict(out=ct[:, 4 * hf:4 * hf + 4, :], in_=bank[6 + hf][:].rearrange("p (c n) -> p c n", c=4), func=AF.Copy)
                                                     if hf == 0 else dict(out=ct[:, 4 * hf:4 * hf + 4, :], in_=bank[6 + hf][:].rearrange("p (c n) -> p c n", c=4))))
                xn_, xnk = xnew.next()
                for hf in range(2):
                    for c in range(8):
                        P.pe([ctk, "woutb"], [bk[1 + hf]], "matmul", bank[1 + hf][:, :], lhsT=ct[:, c, :], rhs=woutb[:, c, hf * 512:(hf + 1) * 512],
                             start=(c == 0), stop=(c == 7))
                    P.dve([bk[1 + hf], "G1_%d" % v], [xnk], "tensor_tensor", out=xn_[:, hf * 512:(hf + 1) * 512], in0=bank[1 + hf][:, :],
                          in1=G1[v][:, hf * 512:(hf + 1) * 512], op=ALU.mult)
                P.pool([xnk, xk], [xnk], "tensor_tensor", out=xn_[:], in0=xn_[:], in1=xt_[:], op=ALU.add)
                xns.append((xn_, xnk, v))
                ss_, sk_ = ssr.next()
                hx_, hxk = hrot.next()
                P.act([xnk], [hxk, sk_], "activation", out=hx_[:], in_=xn_[:], func=AF.Square, accum_out=ss_[:, 0:1])
                P.dve([sk_], [sk_], "tensor_scalar", out=ss_[:, 1:2], in0=ss_[:, 0:1], scalar1=1.0 / D, scalar2=EPS, op0=ALU.mult, op1=ALU.add)
                P.act([sk_], [sk_], "activation", out=ss_[:, 1:2], in_=ss_[:, 1:2], func=AF.Sqrt)
                P.dve([sk_], [sk_], "reciprocal", out=ss_[:, 1:2], in_=ss_[:, 1:2])
                P.dve([xnk, sk_, "A2_%d" % v], [hxk], "scalar_tensor_tensor", out=hx_[:], in0=xn_[:], scalar=ss_[:, 1:2], in1=A2[v][:],
                      op0=ALU.mult, op1=ALU.mult)
                P.pool([hxk, "B2_%d" % v], [hxk], "tensor_tensor", out=hx_[:], in0=hx_[:], in1=B2[v][:], op=ALU.add)
                for hf in range(2):
                    for c in range(4):
                        P.pe([hxk, "idf"], [bk[6 + hf]], "transpose", out=bank[6 + hf][:, c * 128:(c + 1) * 128],
                             in_=hx_[:, (4 * hf + c) * 128:(4 * hf + c + 1) * 128], identity=idf[:])
                    src = bank[6 + hf][:].rearrange("p (c n) -> p c n", c=4)
                    if moe:
                        P.dve([bk[6 + hf]], ["h2Tf"], "tensor_copy", out=h2Tf[:, 4 * hf:4 * hf + 4, t * 128:(t + 1) * 128], in_=src)
                        P.pool(["h2Tf"], [h2k], "tensor_copy", out=h2[:, 4 * hf:4 * hf + 4, t * 128:(t + 1) * 128],
                               in_=h2Tf[:, 4 * hf:4 * hf + 4, t * 128:(t + 1) * 128])
                    else:
                        P.act([bk[6 + hf]], [h2k], "activation", out=h2[:, 4 * hf:4 * hf + 4, t * 128:(t + 1) * 128], in_=src, func=AF.Copy)
                if moe:
                    for c in range(8):
                        P.pe(["h2Tf", "rts"], [bk[3]], "matmul", bank[3][:, 0:8], lhsT=h2Tf[:, c, t * 128:(t + 1) * 128], rhs=rts[:, c, :],
                             start=(c == 0), stop=(c == 7))
                    l_, lk = lg.next()
                    r_, rk = rt.next()
                    q_, qk = rs_.next()
                    P.dve([bk[3]], [lk], "tensor_copy", out=l_[:], in_=bank[3][:, 0:8])
                    P.dve([lk], [qk], "reduce_max", out=q_[:, 0:1], in_=l_[:], axis=AX.X)
                    P.dve([lk, qk], [rk], "tensor_scalar", out=r_[:, 0, :], in0=l_[:], scalar1=q_[:, 0:1], scalar2=None, op0=ALU.is_equal)
                    P.dve([rk, lk], [rk], "scalar_tensor_tensor", out=r_[:, 1, :], in0=r_[:, 0, :], scalar=-1e30, in1=l_[:], op0=ALU.mult, op1=ALU.add)
                    P.dve([rk], [qk], "reduce_max", out=q_[:, 1:2], in_=r_[:, 1, :], axis=AX.X)
                    P.dve([lk, qk], [rk], "tensor_scalar", out=r_[:, 2, :], in0=l_[:], scalar1=q_[:, 1:2], scalar2=None, op0=ALU.is_ge)
                    P.dve([qk], [qk], "tensor_scalar", out=q_[:, 2:3], in0=q_[:, 0:1], scalar1=-1.0, scalar2=None, op0=ALU.mult)
                    P.act([lk, qk], [rk], "activation", out=r_[:, 3, :], in_=l_[:], func=AF.Exp, bias=q_[:, 2:3])
                    P.dve([rk], [rk], "tensor_tensor", out=r_[:, 3, :], in0=r_[:, 3, :], in1=r_[:, 2, :], op=ALU.mult)
                    P.dve([rk], [qk], "reduce_sum", out=q_[:, 3:4], in_=r_[:, 3, :], axis=AX.X)
                    P.dve([qk], [qk], "reciprocal", out=q_[:, 3:4], in_=q_[:, 3:4])
                    P.dve([rk, qk], [cwk], "tensor_scalar", out=cw_[:, t, :], in0=r_[:, 3, :], scalar1=q_[:, 3:4], scalar2=None, op0=ALU.mult)
            for t in range(3):
                ya_, yak = yacc.next()
                yas.append((ya_, yak))
            for e in range(E):
                for gi in range(NG):
                    tiles = []
                    for (src_, rot_) in ((wgd[e, :, :, gi * 256:(gi + 1) * 256], wgb), (wud[e, :, :, gi * 256:(gi + 1) * 256], wub),
                                         (wdd[e, :, 2 * gi:2 * gi + 2, :], wdb)):
                        wb_, wbk = rot_.next()
                        P.load(wb_[:], src_, wbk)
                        tiles.append((wb_, wbk))
                    (wg_, wgk), (wu_, wuk), (wd_, wdk) = tiles
                    for j in range(2):
                        for c in range(8):
                            P.pe([wgk, h2k], [bk[6]], "matmul", bank[6][:, 0:ST], lhsT=wg_[:, c, j * 128:(j + 1) * 128], rhs=h2[:, c, :],
                                 start=(c == 0), stop=(c == 7))
                        for c in range(8):
                            P.pe([wuk, h2k], [bk[7]], "matmul", bank[7][:, 0:ST], lhsT=wu_[:, c, j * 128:(j + 1) * 128], rhs=h2[:, c, :],
                                 start=(c == 0), stop=(c == 7))
                        sg_, sgk = sgr.next()
                        P.act([bk[6]], [sgk], "activation", out=sg_[:], in_=bank[6][:, 0:ST], func=AF.Silu)
                        a_, ak = aTr.next()
                        P.dve([sgk, bk[7]], [ak], "tensor_tensor", out=a_[:], in0=sg_[:], in1=bank[7][:, 0:ST], op=ALU.mult)
                        first = (gi == 0 and j == 0)
                        last = (gi == NG - 1 and j == 1)
                        for t in range(3):
                            for hf in range(2):
                                b_ = 2 * t + hf
                                P.pe([ak, wdk], [bk[b_]], "matmul", bank[b_][:, :], lhsT=a_[:, t * 128:(t + 1) * 128], rhs=wd_[:, j, hf * 512:(hf + 1) * 512],
                                     start=first, stop=last)
                for t in range(3):
                    ya_, yak = yas[t]
                    for hf in range(2):
                        b_ = 2 * t + hf
                        osl = ya_[:, hf * 512:(hf + 1) * 512]
                        if not moe:
                            P.act([bk[b_]], [yak], "activation", out=osl, in_=bank[b_][:, :], func=AF.Copy)
                        elif e == 0:
                            P.dve([bk[b_], cwk], [yak], "tensor_scalar", out=osl, in0=bank[b_][:, :], scalar1=cw_[:, t, e:e + 1], scalar2=None, op0=ALU.mult)
                        else:
                            P.dve([bk[b_], cwk, yak], [yak], "scalar_tensor_tensor", out=osl, in0=bank[b_][:, :], scalar=cw_[:, t, e:e + 1], in1=osl,
                                  op0=ALU.mult, op1=ALU.add)
            for t in range(3):
                g = 3 * s + t
                ya_, yak = yas[t]
                xn_, xnk, v = xns[t]
                P.dve([yak, "G2_%d" % v], [yak], "tensor_tensor", out=ya_[:], in0=ya_[:], in1=G2[v][:], op=ALU.mult)
                P.pool([yak, xnk], [yak], "tensor_tensor", out=ya_[:], in0=ya_[:], in1=xn_[:], op=ALU.add)
                P.store(xo[g * 128:(g + 1) * 128, :], ya_[:], yak)
        P.end()


def phase_p4(P, io):
    xs = io["xs"]
    fg = io["fg"]
    xo = io["xo"]
    if True:
        P.begin()
        g_ = P.sb("g_", [128, D], F32)
        xrot = sb_rot(P, "xt", [128, D], F32, 3)
        orot = sb_rot(P, "ot", [128, D], F32, 3)
        ssr = sb_rot(P, "ss", [128, 2], F32, 3)
        P.load(g_[:], fg[0:1, :].partition_broadcast(128), "g_")
        for g in range(2, TC // 128):
            xt_, xk = xrot.next()
            P.load(xt_[:], xs[g * 128:(g + 1) * 128, :], xk)
            o_, ok = orot.next()
            ss_, sk_ = ssr.next()
            P.act([xk], [ok, sk_], "activation", out=o_[:], in_=xt_[:], func=AF.Square, accum_out=ss_[:, 0:1])
            P.dve([sk_], [sk_], "tensor_scalar", out=ss_[:, 1:2], in0=ss_[:, 0:1], scalar1=1.0 / D, scalar2=EPS, op0=ALU.mult, op1=ALU.add)
            P.act([sk_], [sk_], "activation", out=ss_[:, 1:2], in_=ss_[:, 1:2], func=AF.Sqrt)
            P.dve([sk_], [sk_], "reciprocal", out=ss_[:, 1:2], in_=ss_[:, 1:2])
            P.dve([xk, sk_, "g_"], [ok], "scalar_tensor_tensor", out=o_[:], in0=xt_[:], scalar=ss_[:, 1:2], in1=g_[:], op0=ALU.mult, op1=ALU.mult)
            P.store(xo[(g - 2) * 128:(g - 1) * 128, :], o_[:], ok)
        P.end()


def build_fused(depth=DEPTH):
    nc = bass.Bass("TRN2", target_bir_lowering=False)

    def din(name, shape, dt=F32):
        return nc.dram_tensor(name, list(shape), dt, kind="ExternalInput").ap()

    def scratch(name, shape, dt=F32):
        return nc.dram_tensor(name, list(shape), dt).ap()

    xs = din("xs", [TB, D])
    cT = din("cT", [128, 8, 2])
    tabs = din("tabs", [128, 4, TB])
    idn = din("idn", [128, 128])
    ada_w = din("ada_w", [DEPTH, 128, 8, 6 * D])
    ada_b2 = din("ada_b2", [DEPTH, 2, 6 * D])
    ng = din("ng", [DEPTH, 2, 2, D])
    w1 = din("w1", [DEPTH, 128, 8, NW1])
    w2f = din("w2f", [DEPTH, 33, 512])
    lamb = din("lamb", [DEPTH, 128, 4, 32])
    cst = din("cst", [DEPTH, 128, 66])
    sink = din("sink", [DEPTH, 2, 128, 3])
    masks = din("masks", [128, 2, 3, 128])
    tri = din("tri", [64, 2, 65])
    trix = din("trix", [64, 2, 64])
    gmask = din("gmask", [64, 2, 2, 64])
    gc = din("gc", [DEPTH, 64, 192])
    wout = din("wout", [DEPTH, 128, 8, D])
    ffg = din("ffg", [2, 1, 128, 8, D_FF])
    ffu = din("ffu", [2, 1, 128, 8, D_FF])
    ffd = din("ffd", [2, 1, 128, D_FF // 128, D])
    mog = din("mog", [2, NEXP, 128, 8, D_FFE])
    mou = din("mou", [2, NEXP, 128, 8, D_FFE])
    mod_ = din("mod_", [2, NEXP, 128, D_FFE // 128, D])
    router = din("router", [2, 128, 8, 8])
    fg = din("fg", [1, D])
    out = nc.dram_tensor("out", [SEQ, D], F32, kind="ExternalOutput").ap()
    X = [scratch("X%d" % i, [TB, D]) for i in range(2)]
    MODROW = scratch("MODROW", [2, 6, D])
    FEAT = scratch("FEAT", [FEAT_ROWS, TB], BF16)
    TOKB = scratch("TOKB", [TB, 1024], BF16)
    TOKF = scratch("TOKF", [TB, 896])
    MO = scratch("MO", [TB, D])
    WGD = scratch("WGD", [NEXP, 128, 8, D_FFE], BF16)
    WUD = scratch("WUD", [NEXP, 128, 8, D_FFE], BF16)
    WDD = scratch("WDD", [NEXP, 128, D_FFE // 128, D], BF16)
    FGD = scratch("FGD", [1, 128, 8, D_FF], BF16)
    FUD = scratch("FUD", [1, 128, 8, D_FF], BF16)
    FDD = scratch("FDD", [1, 128, D_FF // 128, D], BF16)
    with ExitStack() as st:
        P = Prog(nc, st)
        xin = xs
        for L in range(depth):
            xout = X[L % 2]
            phase_p1(P, dict(xs=xin, cT=cT, ada_w=ada_w[L], ada_b2=ada_b2[L], ng=ng[L], w1=w1[L], w2f=w2f[L], tabs=tabs, idn=idn,
                             modrow=MODROW, feat=FEAT, tokb=TOKB, tokf=TOKF))
            for hh in range(2):
                phase_p2a(P, dict(aqt=FEAT[128 * hh:128 * hh + 128, :], akt=FEAT[256 + 128 * hh:256 + 128 * hh + 128, :],
                                  av=TOKB[:, 128 * hh:128 * hh + 128], lamb=lamb[L], cst=cst[L], idn=idn, mo=MO[:, 128 * hh:128 * hh + 128]))
                phase_p2b(P, dict(bqt=FEAT[512 + 192 * hh:512 + 192 * hh + 192, :], bkt=FEAT[896 + 64 * hh:896 + 64 * hh + 64, :],
                                  bv=TOKB[:, 256 + 64 * hh:256 + 64 * hh + 64], sink=sink[L, hh], masks=masks,
                                  mo=MO[:, 256 + 192 * hh:256 + 192 * hh + 192]))
                phase_p2c(P, dict(cqt=FEAT[1024 + 128 * hh:1024 + 128 * hh + 128, :], ckt=FEAT[1280 + 128 * hh:1280 + 128 * hh + 128, :],
                                  cktok=TOKB[:, 768 + 128 * hh:768 + 128 * hh + 128], cv=TOKB[:, 384 + 192 * hh:384 + 192 * hh + 192],
                                  crs=TOKF[:, 192 * hh:192 * hh + 192],
                                  sp=TOKF[:, 384:896].rearrange("t (d c) -> t d c", d=2)[:, :, 128 * hh:128 * hh + 128],
                                  tri_d=tri, trix_d=trix, mask_d=gmask, gc_d=gc[L], mo=MO[:, 640 + 192 * hh:640 + 192 * hh + 192]))
            j = L // 2
            if L % 2 == 0:
                phase_p3(P, dict(xs=xin, mo=MO, modrow=MODROW, wout=wout[L], router=router[0], wg=ffg[j], wu=ffu[j], wd=ffd[j],
                                 idn=idn, xo=xout, wgd=FGD, wud=FUD, wdd=FDD), 1, D_FF, False)
            else:
                phase_p3(P, dict(xs=xin, mo=MO, modrow=MODROW, wout=wout[L], router=router[j], wg=mog[j], wu=mou[j], wd=mod_[j],
                                 idn=idn, xo=xout, wgd=WGD, wud=WUD, wdd=WDD), NEXP, D_FFE, True)
            xin = xout
        phase_p4(P, dict(xs=xin, fg=fg, xo=out))
    return nc


_PROG = []


def _c(a):
    return np.ascontiguousarray(a)


def kernel(x, c, ctx, c_ctx, norm1_g, norm2_g, ada_w, ada_b, w_in, w_out, a_lambda, a_norm_g,
           b_sink, c_gate_w2, c_gate_b, c_norm_g, ffn_w_gate, ffn_w_up, ffn_w_down,
           moe_router, moe_w_gate, moe_w_up, moe_w_down, final_g):
    f = lambda a: np.asarray(a, np.float32)
    x, c, ctx, c_ctx = f(x), f(c), f(ctx), f(c_ctx)
    if not _PROG:
        _PROG.append(build_fused())
    nc = _PROG[0]
    cols = w1_columns()
    tabs = _c(rope_tables().transpose(1, 0, 2))
    tri, trix, gmask = gla_consts()
    shared = {
        "tabs": tabs, "idn": np.eye(128, dtype=np.float32),
        "ada_w": np.stack([kmajor(f(ada_w[L])) for L in range(DEPTH)]),
        "ada_b2": np.stack([np.stack([f(ada_b[L])] * 2) for L in range(DEPTH)]),
        "ng": np.stack([np.stack([np.stack([f(norm1_g[L]), f(norm2_g[L])])] * 2) for L in range(DEPTH)]),
        "w1": np.stack([kmajor(take_cols(f(w_in[L]), cols)) for L in range(DEPTH)]),
        "w2f": np.stack([w2full(f(c_gate_w2[L]), f(c_gate_b[L])) for L in range(DEPTH)]),
        "lamb": _c(np.broadcast_to(f(a_lambda)[:, None], (DEPTH, 128, 4, 32))),
        "masks": band_masks(), "tri": tri, "trix": trix, "gmask": gmask,
        "gc": _c(np.broadcast_to(np.tile(f(c_norm_g), (1, 2))[:, None, :], (DEPTH, 64, 192))),
        "wout": np.stack([kmajor(f(w_out[L])) for L in range(DEPTH)]),
        "ffg": np.stack([kmajor(f(ffn_w_gate[j]))[None] for j in range(2)]),
        "ffu": np.stack([kmajor(f(ffn_w_up[j]))[None] for j in range(2)]),
        "ffd": np.stack([kmajor(f(ffn_w_down[j]))[None] for j in range(2)]),
        "mog": np.stack([np.stack([kmajor(f(moe_w_gate[j][e])) for e in range(NEXP)]) for j in range(2)]),
        "mou": np.stack([np.stack([kmajor(f(moe_w_up[j][e])) for e in range(NEXP)]) for j in range(2)]),
        "mod_": np.stack([np.stack([kmajor(f(moe_w_down[j][e])) for e in range(NEXP)]) for j in range(2)]),
        "router": np.stack([kmajor(f(moe_router[j])) for j in range(2)]),
        "fg": _c(f(final_g)[None, :]),
    }
    cst = np.zeros((DEPTH, 128, 66), np.float32)
    for L in range(DEPTH):
        lam_init = 0.8 - 0.6 * math.exp(-0.3 * L)
        cst[L, :, :64] = f(a_norm_g[L])[None, :]
        cst[L, :, 64] = lam_init
        cst[L, :, 65] = 1.0 - lam_init
    shared["cst"] = cst
    shared["sink"] = _c(np.broadcast_to(f(b_sink).reshape(DEPTH, 2, 1, 3), (DEPTH, 2, 128, 3)))
    in_maps = []
    for i in range(NCORE):
        b = i // 2
        m = dict(shared)
        m["xs"] = _c(np.concatenate([ctx[b], x[b]], 0))
        m["cT"] = _c(np.stack([c[b].reshape(8, 128).T, c_ctx.reshape(8, 128).T], -1))
        in_maps.append(m)
    res = run_bass_kernel_spmd(nc, in_maps, core_ids=list(range(NCORE)))
    out = np.stack([np.asarray(res.results[2 * b]["out"]) for b in range(BATCH)], 0)
    return np.ascontiguousarray(out.astype(np.float32))
```

```python
import math
from contextlib import ExitStack

import ml_dtypes
import numpy as np

import concourse.bass as bass
import concourse.mybir as mybir
from concourse.bass_utils import run_bass_kernel_spmd

F32 = mybir.dt.float32
BF16 = mybir.dt.bfloat16
AF = mybir.ActivationFunctionType
ALU = mybir.AluOpType
AX = mybir.AxisListType
ENGS = ("tensor", "vector", "scalar", "gpsimd", "sync")
NPBF = ml_dtypes.bfloat16

D = 1024
BATCH = 4
SEQ = 8192
CTX = 256
DEPTH = 4
TB = CTX + SEQ
NCORE = 8
TC = TB
EPS = 1e-6
D_FF = 2816
D_FFE = 3584
NEXP = 8


class Prog:
    def __init__(self, nc, stack, strict_same_engine=True):
        self.nc = nc
        self.sem_stack = stack
        self.stack = stack
        self.phase = 0
        self.ops = {e: [] for e in ENGS}
        self.sems = {}
        self.inc = {}
        self.cnt = {}
        self.seen = {e: {} for e in ENGS}
        self.buf = {}
        self.strict = strict_same_engine
        self.nps = 0
        for e in ENGS[:4]:
            self._mk(e, 1)

    def _mk(self, v, inc):
        self.sems[v] = self.sem_stack.enter_context(self.nc.semaphore("s_" + v.replace(":", "_")))
        self.inc[v] = inc
        self.cnt[v] = 0

    def sb(self, name, shape, dt):
        return self.stack.enter_context(self.nc.sbuf_tensor("%s_p%d" % (name, self.phase), list(shape), dt))

    def ps(self, name, shape, dt=F32):
        return self.stack.enter_context(self.nc.psum_tensor("%s_p%d" % (name, self.phase), list(shape), dt))

    def begin(self):
        self.phase += 1
        self.stack = ExitStack()
        self.stack.__enter__()

    def end(self):
        for eng in ENGS:
            self.finish(eng)
        self.emit()
        self.ops = {e: [] for e in ENGS}
        self.stack.__exit__(None, None, None)
        self.stack = None

    def _deps(self, reads, writes):
        deps = {}

        def add(vk):
            if vk is None:
                return
            v, k = vk
            if deps.get(v, 0) < k:
                deps[v] = k
        for b in reads:
            st = self.buf.get(b)
            if st:
                add(st[0])
        for b in writes:
            st = self.buf.get(b)
            if st:
                add(st[0])
                for r in st[1]:
                    add(r)
        return deps

    def op(self, eng, fn, reads=(), writes=(), slot=None):
        v = eng if slot is None else "dma:" + slot
        if v not in self.sems:
            self._mk(v, 16)
        deps = self._deps(reads, writes)
        for dv, k in deps.items():
            if dv == eng and slot is None:
                if eng == "tensor" or not self.strict or self.cnt[eng] + 1 - k >= 3:
                    continue
            if self.seen[eng].get(dv, 0) >= k:
                continue
            self.seen[eng][dv] = k
            self.ops[eng].append(("w", dv, k * self.inc[dv]))
        self.cnt[v] += 1
        k = self.cnt[v]
        self.ops[eng].append(("i", fn, v))
        for b in reads:
            st = self.buf.setdefault(b, [None, []])
            st[1].append((v, k))
        for b in writes:
            self.buf[b] = [(v, k), []]
        return (v, k)

    def pe(self, r, w, m, *a, **k):
        return self.op("tensor", (m, a, k), r, w)

    def dve(self, r, w, m, *a, **k):
        return self.op("vector", (m, a, k), r, w)

    def act(self, r, w, m, *a, **k):
        return self.op("scalar", (m, a, k), r, w)

    def pool(self, r, w, m, *a, **k):
        return self.op("gpsimd", (m, a, k), r, w)

    def load(self, out_ap, in_ap, key, r=()):
        return self.op("sync", ("dma_start", (), dict(out=out_ap, in_=in_ap)), r, [key], slot=key)

    def store(self, out_ap, in_ap, key, w=()):
        return self.op("gpsimd", ("dma_start", (), dict(out=out_ap, in_=in_ap)), [key], w, slot="st_" + key)

    def finish(self, eng="sync"):
        for v, c in self.cnt.items():
            if c and self.seen[eng].get(v, 0) < c:
                self.ops[eng].append(("w", v, c * self.inc[v]))
                self.seen[eng][v] = c

    def emit(self):
        nc = self.nc
        with nc.Block() as block:
            def run(engname):
                def body(e):
                    for o in self.ops[engname]:
                        if o[0] == "w":
                            e.wait_ge(self.sems[o[1]], o[2])
                        else:
                            getattr(e, o[1][0])(*o[1][1], **o[1][2]).then_inc(self.sems[o[2]], self.inc[o[2]])
                return body
            block.sync(run("sync"))
            block.tensor(run("tensor"))
            block.vector(run("vector"))
            block.scalar(run("scalar"))
            block.gpsimd(run("gpsimd"))


class Rot:
    def __init__(self, tiles, name):
        self.tiles = tiles
        self.name = name
        self.i = 0

    def next(self):
        j = self.i % len(self.tiles)
        self.i += 1
        return self.tiles[j], "%s%d" % (self.name, j)


def sb_rot(P, name, shape, dt, n):
    return Rot([P.sb("%s%d" % (name, j), shape, dt) for j in range(n)], name)


def ps_rot(P, name, n, shape=(128, 512), dt=F32):
    return Rot([P.ps("%s%d" % (name, j), shape, dt) for j in range(n)], name)


NWF = 2592
NWT = 1408
NW1 = NWF + NWT
ST = 384
NST = TC // ST
FEAT_ROWS = 1536


def phase_p1(P, io):
    xs = io["xs"]
    cT = io["cT"]
    ada_w = io["ada_w"]
    ada_b2 = io["ada_b2"]
    ng = io["ng"]
    w1 = io["w1"]
    w2f = io["w2f"]
    tabs = io["tabs"]
    idn = io["idn"]
    modrow = io["modrow"]
    feat = io["feat"]
    tokb = io["tokb"]
    tokf = io["tokf"]
    if True:
        P.begin()
        idf = P.sb("idf", [128, 128], F32)
        cTs = P.sb("cTs", [128, 8, 2], F32)
        scT = P.sb("scT", [128, 8, 2], F32)
        wfb = P.sb("wfb", [128, 8, NW1], BF16)
        w2s = P.sb("w2s", [33, 512], F32)
        ngs = P.sb("ngs", [2, 2, D], F32)
        modsb = P.sb("modsb", [2, 6 * D], F32)
        A1 = [P.sb("A1_%d" % i, [128, D], F32) for i in range(2)]
        B1 = [P.sb("B1_%d" % i, [128, D], F32) for i in range(2)]
        stage = sb_rot(P, "stage", [128, 2048], F32, 2)
        xrot = sb_rot(P, "xt", [128, D], F32, 2)
        hrot = sb_rot(P, "hx", [128, D], F32, 2)
        ssr = sb_rot(P, "ss", [128, 2], F32, 2)
        hT = sb_rot(P, "hT", [128, 8, ST], BF16, 2)
        glT = P.sb("glT", [33, ST], F32)
        tabr = sb_rot(P, "tab", [128, 4, ST], F32, 2)
        t1r = sb_rot(P, "t1", [128, ST], F32, 2)
        t2r = sb_rot(P, "t2", [128, ST], F32, 2)
        fo = sb_rot(P, "fo", [128, ST], BF16, 4)
        tbo = sb_rot(P, "tbo", [128, 1024], BF16, 2)
        tfo = sb_rot(P, "tfo", [128, 896], F32, 2)
        ez = sb_rot(P, "ez", [128, 512], F32, 2)
        pT = ps_rot(P, "pT", 2, (128, 4, 128))
        pg = ps_rot(P, "pg", 6)

        P.load(idf[:], idn, "idf")
        P.load(cTs[:], cT, "cTs")
        P.load(w2s[:], w2f, "w2s")
        P.load(modsb[:], ada_b2, "modsb")
        P.load(ngs[:], ng, "ngs")
        P.act(["cTs"], ["scT"], "activation", out=scT[:], in_=cTs[:], func=AF.Silu)
        P.pool([], ["glT"], "memset", glT[:], 1.0)
        for j in range(24):
            sg, sk = stage.next()
            P.load(sg[:].rearrange("p (c n) -> p c n", c=8), ada_w[:, :, j * 256:(j + 1) * 256], sk)
            pm, pk = pg.next()
            for c in range(8):
                P.pe(["scT", sk], [pk], "matmul", pm[0:2, 0:256], lhsT=scT[:, c, :], rhs=sg[:, c * 256:(c + 1) * 256],
                     start=(c == 0), stop=(c == 7))
            P.dve([pk, "modsb"], ["modsb"], "tensor_tensor", out=modsb[:, j * 256:(j + 1) * 256], in0=pm[0:2, 0:256],
                  in1=modsb[:, j * 256:(j + 1) * 256], op=ALU.add)
        for which in range(2):
            isc = 3 * which + 1
            P.dve(["modsb", "ngs"], ["modsb"], "scalar_tensor_tensor",
                  out=modsb[:, isc * D:(isc + 1) * D], in0=modsb[:, isc * D:(isc + 1) * D], scalar=1.0, in1=ngs[:, which, :],
                  op0=ALU.add, op1=ALU.mult)
        P.store(modrow, modsb[:].rearrange("p (a d) -> p a d", a=6), "modsb", ["MODROW"])
        for v in range(2):
            P.load(A1[v][:], modrow[v:v + 1, 1, :].partition_broadcast(128), "A1_%d" % v, ["MODROW"])
            P.load(B1[v][:], modrow[v:v + 1, 0, :].partition_broadcast(128), "B1_%d" % v, ["MODROW"])
        HW1 = NW1 // 2
        for c in range(16):
            sg, sk = stage.next()
            kc, hf = divmod(c, 2)
            P.load(sg[:, 0:HW1], w1[:, kc, hf * HW1:(hf + 1) * HW1], sk)
            (P.dve if c % 2 == 0 else P.pool)([sk], ["wfb"], "tensor_copy", out=wfb[:, kc, hf * HW1:(hf + 1) * HW1], in_=sg[:, 0:HW1])

        rope_pairs = [(0, 2, 0, 0), (1, 3, 0, 128), (4, 6, 0, 256), (5, 7, 0, 384),
                      (8, 11, 2, 512), (9, 12, 2, 640), (10, 13, 2, 768), (14, 15, 2, 896)]
        plain = [(16, 1024, 48 ** -0.5), (17, 1152, 48 ** -0.5), (18, 1280, 1.0), (19, 1408, 1.0)]
        for s in range(NST):
            hTt, hk = hT.next()
            tb_, tk = tabr.next()
            P.load(tb_[:], tabs[:, :, s * ST:(s + 1) * ST], tk)
            for t in range(3):
                g = 3 * s + t
                v = 1 if g < 2 else 0
                xt_, xk = xrot.next()
                P.load(xt_[:], xs[g * 128:(g + 1) * 128, :], xk)
                ss_, sk_ = ssr.next()
                hx_, hxk = hrot.next()
                P.act([xk], [hxk, sk_], "activation", out=hx_[:], in_=xt_[:], func=AF.Square, accum_out=ss_[:, 0:1])
                P.dve([sk_], [sk_], "tensor_scalar", out=ss_[:, 1:2], in0=ss_[:, 0:1], scalar1=1.0 / D, scalar2=EPS, op0=ALU.mult, op1=ALU.add)
                P.act([sk_], [sk_], "activation", out=ss_[:, 1:2], in_=ss_[:, 1:2], func=AF.Sqrt)
                P.dve([sk_], [sk_], "reciprocal", out=ss_[:, 1:2], in_=ss_[:, 1:2])
                P.dve([xk, sk_, "A1_%d" % v], [hxk], "scalar_tensor_tensor",
                      out=hx_[:], in0=xt_[:], scalar=ss_[:, 1:2], in1=A1[v][:], op0=ALU.mult, op1=ALU.mult)
                P.pool([hxk, "B1_%d" % v], [hxk], "tensor_tensor", out=hx_[:], in0=hx_[:], in1=B1[v][:], op=ALU.add)
                for hf in range(2):
                    pt_, ptk = pT.next()
                    for c in range(4):
                        P.pe([hxk, "idf"], [ptk], "transpose", out=pt_[:, c, :], in_=hx_[:, (4 * hf + c) * 128:(4 * hf + c + 1) * 128], identity=idf[:])
                    if hf == 0:
                        P.act([ptk], [hk], "activation", out=hTt[:, 0:4, t * 128:(t + 1) * 128], in_=pt_[:], func=AF.Copy)
                    else:
                        P.dve([ptk], [hk], "tensor_copy", out=hTt[:, 4:8, t * 128:(t + 1) * 128], in_=pt_[:])

            def fm(pd, pk_, f0, ncols, hTt, hk):
                for c in range(8):
                    P.pe(["wfb", hk], [pk_], "matmul", pd[0:ncols, 0:ST], lhsT=wfb[:, c, f0:f0 + ncols], rhs=hTt[:, c, :],
                         start=(c == 0), stop=(c == 7))

            for (fx, fp, ti, row0) in rope_pairs:
                px, pxk = pg.next()
                pp, ppk = pg.next()
                fm(px, pxk, fx * 128, 128, hTt, hk)
                fm(pp, ppk, fp * 128, 128, hTt, hk)
                a_, ak = t1r.next()
                b_, bk = t2r.next()
                P.dve([pxk, tk], [ak], "tensor_tensor", out=a_[:], in0=px[:, 0:ST], in1=tb_[:, ti, :], op=ALU.mult)
                P.dve([ppk, tk], [bk], "tensor_tensor", out=b_[:], in0=pp[:, 0:ST], in1=tb_[:, ti + 1, :], op=ALU.mult)
                o_, ok = fo.next()
                P.pool([ak, bk], [ok], "tensor_tensor", out=o_[:], in0=a_[:], in1=b_[:], op=ALU.add)
                P.store(feat[row0:row0 + 128, s * ST:(s + 1) * ST], o_[:], ok)
            for (f, row0, scl) in plain:
                px, pxk = pg.next()
                fm(px, pxk, f * 128, 128, hTt, hk)
                o_, ok = fo.next()
                P.act([pxk], [ok], "activation", out=o_[:], in_=px[:, 0:ST], func=AF.Copy, scale=scl)
                P.store(feat[row0:row0 + 128, s * ST:(s + 1) * ST], o_[:], ok)
            px, pxk = pg.next()
            fm(px, pxk, 2560, 32, hTt, hk)
            P.act([pxk], ["glT"], "activation", out=glT[0:32, :], in_=px[0:32, 0:ST], func=AF.Copy)
            for t in range(3):
                g = 3 * s + t
                tb2, tbk = tbo.next()
                tf2, tfk = tfo.next()
                for (c0, n) in ((0, 512), (512, 512), (1024, 384)):
                    px, pxk = pg.next()
                    for c in range(8):
                        P.pe(["wfb", hk], [pxk], "matmul", px[:, 0:n], lhsT=hTt[:, c, t * 128:(t + 1) * 128],
                             rhs=wfb[:, c, NWF + c0:NWF + c0 + n], start=(c == 0), stop=(c == 7))
                    if c0 < 1024:
                        P.dve([pxk], [tbk], "tensor_copy", out=tb2[:, c0:c0 + 512], in_=px[:, 0:512])
                    else:
                        P.act([pxk], [tfk], "activation", out=tf2[:, 0:384], in_=px[:, 0:384], func=AF.Silu)
                pz, pzk = pg.next()
                P.pe(["glT", "w2s"], [pzk], "matmul", pz[:, :], lhsT=glT[0:33, t * 128:(t + 1) * 128], rhs=w2s[:, :], start=True, stop=True)
                ez_, ezk = ez.next()
                P.act([pzk], [ezk], "activation", out=ez_[:], in_=pz[:], func=AF.Exp, scale=-1.0)
                P.act([ezk], [tfk], "activation", out=tf2[:, 384:896], in_=ez_[:], func=AF.Ln, bias=1.0)
                P.store(tokb[g * 128:(g + 1) * 128, :], tb2[:], tbk)
                P.store(tokf[g * 128:(g + 1) * 128, :], tf2[:], tfk)
        P.end()


A_Q0, A_K0, A_V0 = 0, 256, 512
B_Q0, B_K0, B_V0 = 768, 1152, 1280
C_Q0, C_K0, C_V0, C_R0, C_G0 = 1408, 1600, 1792, 2176, 2560


def _rope_perm(dim):
    q = dim // 4
    perm = np.zeros(dim, np.int64)
    sign = np.zeros(dim, np.float32)
    for d in range(dim):
        blk = d // q
        if blk % 2 == 0:
            perm[d] = d + q
            sign[d] = -1.0
        else:
            perm[d] = d - q
            sign[d] = 1.0
    return perm, sign


def w1_columns():
    cols = []
    pA, _ = _rope_perm(32)
    pB, _ = _rope_perm(64)

    def permuted(base, n, dim, perm):
        out = []
        for j in range(n):
            hd, d = divmod(j, dim)
            out.append(base + hd * dim + int(perm[d]))
        return out
    cols += list(range(A_Q0, A_Q0 + 256)) + permuted(A_Q0, 256, 32, pA)
    cols += list(range(A_K0, A_K0 + 256)) + permuted(A_K0, 256, 32, pA)
    cols += list(range(B_Q0, B_Q0 + 384)) + permuted(B_Q0, 384, 64, pB)
    cols += list(range(B_K0, B_K0 + 128)) + permuted(B_K0, 128, 64, pB)

    def padded(base):
        out = []
        for h in range(4):
            out += list(range(base + 48 * h, base + 48 * h + 48)) + [-1] * 16
        return out
    cols += padded(C_Q0) + padded(C_K0)
    cols += list(range(C_G0, C_G0 + 32))
    assert len(cols) == NWF
    cols += list(range(A_V0, A_V0 + 256)) + list(range(B_V0, B_V0 + 128)) + list(range(C_V0, C_V0 + 384))
    cols += padded(C_K0) + list(range(C_R0, C_R0 + 384))
    assert len(cols) == NW1
    return np.array(cols, np.int64)


def take_cols(w, cols):
    out = np.zeros((w.shape[0], len(cols)), w.dtype)
    m = cols >= 0
    out[:, m] = w[:, cols[m]]
    return out


def kmajor(w):
    K, N = w.shape
    return np.ascontiguousarray(w.reshape(K // 128, 128, N).transpose(1, 0, 2))


def rope_tables():
    out = np.zeros((4, 128, TB), np.float32)
    tok = np.arange(SEQ)
    row = (tok // 64).astype(np.float32)
    col = (tok % 64).astype(np.float32)
    for ti, dim in ((0, 32), (2, 64)):
        q = dim // 4
        inv = (10000.0 ** (-np.arange(q, dtype=np.float32) / q)).astype(np.float32)
        ang_r = row[:, None] * inv[None, :]
        ang_c = col[:, None] * inv[None, :]
        _, sign = _rope_perm(dim)
        cosd = np.zeros((dim, SEQ), np.float32)
        sind = np.zeros((dim, SEQ), np.float32)
        for d in range(dim):
            ang = ang_r if d < dim // 2 else ang_c
            cosd[d] = np.cos(ang[:, d % q])
            sind[d] = sign[d] * np.sin(ang[:, d % q])
        reps = 128 // dim
        out[ti, :, :CTX] = 1.0
        out[ti + 1, :, :CTX] = 0.0
        out[ti, :, CTX:] = np.tile(cosd, (reps, 1))
        out[ti + 1, :, CTX:] = np.tile(sind, (reps, 1))
    return out


def w2full(w2, bg):
    out = np.zeros((33, 512), np.float32)
    for d in range(2):
        for h in range(4):
            c0 = d * 256 + h * 64
            out[16 * d:16 * d + 16, c0:c0 + 48] = w2[d][:, 48 * h:48 * h + 48]
            out[32, c0:c0 + 48] = bg[d][48 * h:48 * h + 48]
    return out


NT = TB // 128


def phase_p2a(P, io):
    scale = 32 ** -0.5
    aqt = io["aqt"]
    akt = io["akt"]
    av = io["av"]
    lamb = io["lamb"]
    cst = io["cst"]
    idn = io["idn"]
    mo = io["mo"]
    if True:
        P.begin()
        qT = P.sb("qT", [128, TB], BF16)
        kTm = [P.sb("kTm%d" % j, [128, TB], BF16) for j in range(4)]
        va = P.sb("va", [128, NT, 2, 65], BF16)
        idf = P.sb("idf", [128, 128], F32)
        lb = P.sb("lb", [128, 4, 32], F32)
        cs = P.sb("cs", [128, 66], F32)
        sm = P.sb("sm", [128, 8], F32)
        tmp32 = P.sb("tmp32", [128, 32], F32)
        gfin = P.sb("gfin", [128, 64], F32)
        pt = sb_rot(P, "pt", [128, 1024], BF16, 3)
        oT = sb_rot(P, "oT", [65, 512], F32, 2)
        rec = sb_rot(P, "rec", [128, 2, 4], F32, 2)
        d1 = sb_rot(P, "d1", [128, 64], F32, 2)
        dd = sb_rot(P, "dd", [128, 64], F32, 2)
        jk = sb_rot(P, "jk", [128, 64], F32, 2)
        ssr = sb_rot(P, "ssa", [128, 2], F32, 2)
        mot = sb_rot(P, "mot", [128, 4, 128], F32, 2)
        psb = ps_rot(P, "psD", 2, (128, 1024))
        pob = ps_rot(P, "poT", 2)
        ptr = ps_rot(P, "ptr", 1)
        osb = sb_rot(P, "osb", [128, 4, 65], F32, 4)
        P.load(qT[:], aqt, "qT")
        for j in range(4):
            (P.pool if j % 2 == 0 else P.dve)([], ["kT"], "memset", kTm[j][:], 0.0)
        for j in range(4):
            P.load(kTm[j][32 * j:32 * j + 32, :], akt[32 * j:32 * j + 32, :], "kT")
        P.load(lb[:], lamb, "lb")
        P.load(cs[:], cst, "cs")
        P.load(idf[:], idn, "idf")
        P.pool([], ["va"], "memset", va[:], 1.0)
        for h in range(2):
            P.load(va[:, :, h, 0:64], av[:, h * 64:(h + 1) * 64].rearrange("(n p) d -> p n d", p=128), "va")
        for i in range(2):
            P.dve(["lb"], ["tmp32"], "tensor_tensor", out=tmp32[:], in0=lb[:, 2 * i, :], in1=lb[:, 2 * i + 1, :], op=ALU.mult)
            P.dve(["tmp32"], ["sm"], "reduce_sum", out=sm[:, i:i + 1], in_=tmp32[:], axis=AX.X)
        P.act(["sm"], ["sm"], "activation", out=sm[:, 0:2], in_=sm[:, 0:2], func=AF.Exp)
        P.dve(["sm"], ["sm"], "tensor_tensor", out=sm[:, 2:3], in0=sm[:, 0:1], in1=sm[:, 1:2], op=ALU.subtract)
        P.dve(["sm", "cs"], ["sm"], "tensor_tensor", out=sm[:, 2:3], in0=sm[:, 2:3], in1=cs[:, 64:65], op=ALU.add)
        P.dve(["sm"], ["sm"], "tensor_scalar", out=sm[:, 3:4], in0=sm[:, 2:3], scalar1=-1.0, scalar2=None, op0=ALU.mult)
        P.dve(["cs"], ["gfin"], "tensor_scalar", out=gfin[:], in0=cs[:, 0:64], scalar1=cs[:, 65:66], scalar2=None, op0=ALU.mult)

        groups = [(0, 2, 2)] + [(2 + 4 * g, 4, NT) for g in range(16)]
        steps = []
        for (t0, nq, nk) in groups:
            for hl in range(2):
                for m in range(2):
                    for kp in range(nk // 2):
                        steps.append((t0, nq, nk, hl, m, kp))

        def emit_scores(st_):
            t0, nq, nk, hl, m, kp = st_
            nqc = nq * 128
            j = 2 * hl + m
            ps_, psk = psb.next()
            for u in range(2):
                kt = 2 * kp + u
                P.pe(["kT", "qT"], [psk], "matmul", ps_[:, u * 512:u * 512 + nqc], lhsT=kTm[j][:, kt * 128:(kt + 1) * 128],
                     rhs=qT[:, t0 * 128:t0 * 128 + nqc], start=True, stop=True)
            return ps_, psk

        nxt = emit_scores(steps[0])
        pos = []
        po = pok = mt = mk = None
        for si, st_ in enumerate(steps):
            t0, nq, nk, hl, m, kp = st_
            nqc = nq * 128
            ps_, psk = nxt
            if si + 1 < len(steps):
                nxt = emit_scores(steps[si + 1])
            if kp == 0:
                po, pok = pob.next()
                if hl == 0 and m == 0:
                    mt, mk = mot.next()
                if m == 0:
                    pos = []
            p_, pk_ = pt.next()
            P.act([psk], [pk_], "activation", out=p_[:].rearrange("p (u n) -> p u n", u=2)[:, :, 0:nqc],
                  in_=ps_[:].rearrange("p (u n) -> p u n", u=2)[:, :, 0:nqc], func=AF.Exp, scale=scale)
            for u in range(2):
                kt = 2 * kp + u
                P.pe([pk_, "va"], [pok], "matmul", po[0:65, 0:nqc], lhsT=va[:, kt, hl, :], rhs=p_[:, u * 512:u * 512 + nqc],
                     start=(kt == 0), stop=(kt == nk - 1))
            if kp != nk // 2 - 1:
                continue
            o_, ok_ = oT.next()
            P.act([pok], [ok_], "activation", out=o_[:, 0:nqc], in_=po[0:65, 0:nqc], func=AF.Copy)
            tr, trk = ptr.next()
            for qb in range(nq):
                P.pe([ok_, "idf"], [trk], "transpose", out=tr[:, qb * 65:(qb + 1) * 65], in_=o_[:, qb * 128:(qb + 1) * 128], identity=idf[0:65, 0:65])
            os_, osk = osb.next()
            P.dve([trk], [osk], "tensor_copy", out=os_[:, 0:nq, :], in_=tr[:, 0:nq * 65].rearrange("p (q d) -> p q d", d=65))
            pos.append((os_, osk))
            if m == 0:
                continue
            if True:
                (po1, k1), (po2, k2) = pos
                rc, rck = rec.next()
                P.dve([k1], [rck], "reciprocal", out=rc[:, 0, 0:nq], in_=po1[:, 0:nq, 64])
                P.dve([k2], [rck], "reciprocal", out=rc[:, 1, 0:nq], in_=po2[:, 0:nq, 64])
                P.dve([rck, "sm"], [rck], "tensor_scalar", out=rc[:, 1, 0:nq], in0=rc[:, 1, 0:nq], scalar1=sm[:, 3:4], scalar2=None, op0=ALU.mult)
                for qb in range(nq):
                    a_, ak = d1.next()
                    P.dve([k1, rck], [ak], "tensor_scalar", out=a_[:], in0=po1[:, qb, 0:64], scalar1=rc[:, 0, qb:qb + 1], scalar2=None, op0=ALU.mult)
                    d_, dk = dd.next()
                    P.dve([k2, rck, ak], [dk], "scalar_tensor_tensor", out=d_[:], in0=po2[:, qb, 0:64], scalar=rc[:, 1, qb:qb + 1], in1=a_[:],
                          op0=ALU.mult, op1=ALU.add)
                    j_, jkk = jk.next()
                    s_, sk_ = ssr.next()
                    P.act([dk], [jkk, sk_], "activation", out=j_[:], in_=d_[:], func=AF.Square, accum_out=s_[:, 0:1])
                    P.dve([sk_], [sk_], "tensor_scalar", out=s_[:, 1:2], in0=s_[:, 0:1], scalar1=1.0 / 64, scalar2=EPS, op0=ALU.mult, op1=ALU.add)
                    P.act([sk_], [sk_], "activation", out=s_[:, 1:2], in_=s_[:, 1:2], func=AF.Sqrt)
                    P.dve([sk_], [sk_], "reciprocal", out=s_[:, 1:2], in_=s_[:, 1:2])
                    P.dve([dk, sk_, "gfin"], [mk], "scalar_tensor_tensor", out=mt[:, qb, hl * 64:(hl + 1) * 64], in0=d_[:], scalar=s_[:, 1:2],
                          in1=gfin[:], op0=ALU.mult, op1=ALU.mult)
            if hl == 1:
                P.store(mo[t0 * 128:(t0 + nq) * 128, :].rearrange("(q p) c -> p q c", p=128), mt[:, 0:nq, :], mk)
        P.end()


def phase_p2b(P, io):
    scale = 64 ** -0.5
    bqt = io["bqt"]
    bkt = io["bkt"]
    bv = io["bv"]
    sink = io["sink"]
    masks = io["masks"]
    mo = io["mo"]
    if True:
        P.begin()
        qT = P.sb("qT", [64, 3, TB], BF16)
        kT = P.sb("kT", [64, TB], BF16)
        va = P.sb("va", [128, NT, 65], BF16)
        sk = P.sb("sk", [128, 3], F32)
        mk = P.sb("mk", [128, 2, 3, 128], F32)
        pe_ = sb_rot(P, "pe", [128, 3, 128], BF16, 10)
        den = sb_rot(P, "den", [128, 3], F32, 2)
        mot = sb_rot(P, "mot", [128, 3, 64], F32, 3)
        psb = ps_rot(P, "ps", 3)
        pob = ps_rot(P, "po", 2, (128, 3, 65))
        P.load(qT[:], bqt.rearrange("(h d) t -> d h t", d=64), "qT")
        P.load(kT[:], bkt, "kT")
        P.load(sk[:], sink, "sk")
        P.load(mk[:], masks, "mk")
        P.pool([], ["va"], "memset", va[:], 1.0)
        P.load(va[:, :, 0:64], bv.rearrange("(n p) d -> p n d", p=128), "va")
        P.act(["sk"], ["sk"], "activation", out=sk[:], in_=sk[:], func=AF.Exp)
        for n in range(NT):
            if n < 2:
                kts = [(0, None), (1, None)]
            else:
                kts = [(0, None), (1, None)]
                if n - 1 >= 2:
                    kts.append((n - 1, 0))
                kts.append((n, None))
                if n + 1 < NT:
                    kts.append((n + 1, 1))
            po, pok = pob.next()
            pts = []
            for i, (kt, msk) in enumerate(kts):
                ps_, psk = psb.next()
                P.pe(["kT", "qT"], [psk], "matmul", ps_[:, 0:384].rearrange("p (h q) -> p h q", h=3), lhsT=kT[:, kt * 128:(kt + 1) * 128],
                     rhs=qT[:, :, n * 128:(n + 1) * 128], start=True, stop=True)
                p_, pk_ = pe_.next()
                P.act([psk], [pk_], "activation", out=p_[:], in_=ps_[:, 0:384].rearrange("p (h q) -> p h q", h=3), func=AF.Exp, scale=scale)
                if msk is not None:
                    P.dve([pk_, "mk"], [pk_], "tensor_tensor", out=p_[:], in0=p_[:], in1=mk[:, msk, :, :], op=ALU.mult)
                pts.append((p_, pk_, kt))
            for h in range(3):
                for i, (p_, pk_, kt) in enumerate(pts):
                    P.pe([pk_, "va"], [pok], "matmul", po[:, h, :], lhsT=p_[:, h, :], rhs=va[:, kt, :], start=(i == 0), stop=(i == len(pts) - 1))
            dn, dnk = den.next()
            P.dve([pok, "sk"], [dnk], "tensor_tensor", out=dn[:], in0=po[:, :, 64], in1=sk[:], op=ALU.add)
            P.dve([dnk], [dnk], "reciprocal", out=dn[:], in_=dn[:])
            mt, mtk = mot.next()
            for h in range(3):
                P.dve([pok, dnk], [mtk], "tensor_scalar", out=mt[:, h, :], in0=po[:, h, 0:64], scalar1=dn[:, h:h + 1], scalar2=None, op0=ALU.mult)
            P.store(mo[n * 128:(n + 1) * 128, :], mt[:].rearrange("p h d -> p (h d)"), mtk)
        P.end()


def band_masks():
    j = np.arange(128)[:, None]
    i = np.arange(128)[None, :]
    m = np.zeros((128, 2, 3, 128), np.float32)
    m[:, 0, :, :] = (i <= j).astype(np.float32)[:, None, :]
    m[:, 1, :, :] = (j <= i).astype(np.float32)[:, None, :]
    return m


NCH = TB // 64


def gla_consts():
    s_ = np.arange(64)[:, None]
    t_ = np.arange(64)[None, :]
    tri = np.zeros((64, 2, 65), np.float32)
    trix = np.zeros((64, 2, 64), np.float32)
    mask = np.zeros((64, 2, 2, 64), np.float32)
    c = -1.0 / 16.0
    tri[:, 0, :64] = c * (s_ <= t_)
    tri[:, 1, :64] = c * (s_ >= t_)
    tri[:, :, 64] = c
    trix[:, 0, :] = c * (s_ > t_)
    trix[:, 1, :] = c * (s_ < t_)
    mask[:, 0, :, :] = (s_ <= t_).astype(np.float32)[:, None, :]
    mask[:, 1, :, :] = (s_ >= t_).astype(np.float32)[:, None, :]
    return tri, trix, mask


def phase_p2c(P, io):
    cqt = io["cqt"]
    ckt = io["ckt"]
    cktok = io["cktok"]
    cv = io["cv"]
    crs = io["crs"]
    sp = io["sp"]
    tri_d = io["tri_d"]
    trix_d = io["trix_d"]
    mask_d = io["mask_d"]
    gc_d = io["gc_d"]
    mo = io["mo"]
    if True:
        P.begin()
        qT = P.sb("qT", [64, 2, TB], BF16)
        kT = P.sb("kT", [64, 2, TB], BF16)
        OF = P.sb("OF", [64, NCH, 192], F32)
        tri = P.sb("tri_s", [64, 2, 65], F32)
        trix = P.sb("trix_s", [64, 2, 64], F32)
        mask = P.sb("mask_s", [64, 2, 2, 64], F32)
        gc = P.sb("gc_s", [64, 192], F32)
        S = [P.sb("S%d" % d, [64, 2, 96], F32) for d in range(2)]
        Sb = [P.sb("Sb%d" % d, [64, 2, 96], BF16) for d in range(2)]
        spr = sb_rot(P, "spc", [64, 2, 128], F32, 6)
        ktr = sb_rot(P, "ktk", [64, 128], BF16, 6)
        vr = sb_rot(P, "vv", [64, 192], BF16, 6)
        rr = sb_rot(P, "rs", [64, 192], F32, 4)
        E1 = sb_rot(P, "E1", [64, 2, 65], F32, 4)
        E2 = sb_rot(P, "E2", [64, 2, 64], F32, 4)
        E3 = sb_rot(P, "E3", [64, 128], F32, 4)
        qd = sb_rot(P, "qd", [64, 2, 64], BF16, 4)
        ki = sb_rot(P, "ki", [64, 2, 64], BF16, 4)
        ke = sb_rot(P, "ke", [64, 128], BF16, 4)
        att = sb_rot(P, "att", [64, 2, 64], BF16, 4)
        osum = sb_rot(P, "osum", [64, 192], F32, 2)
        jk = sb_rot(P, "jk", [64, 96], F32, 2)
        ssr = sb_rot(P, "ssc", [64, 4], F32, 2)
        yo = sb_rot(P, "yo", [64, 192], F32, 3)
        pb = ps_rot(P, "pb", 2)
        pbd = ps_rot(P, "pbd", 1)
        patt = ps_rot(P, "patt", 2)
        po = ps_rot(P, "po", 2)
        pu = ps_rot(P, "pu", 1)
        P.load(qT[:], cqt.rearrange("(h d) t -> d h t", d=64), "qT")
        P.load(kT[:], ckt.rearrange("(h d) t -> d h t", d=64), "kT")
        P.load(tri[:], tri_d, "tri")
        P.load(trix[:], trix_d, "trix")
        P.load(mask[:], mask_d, "mask")
        P.load(gc[:], gc_d, "gc")
        for d in range(2):
            P.pool([], ["S%d" % d], "memset", S[d][:], 0.0)
            P.pool([], ["Sb%d" % d], "memset", Sb[d][:], 0.0)
        fwd = [(c, 0) for c in range(NCH)]
        bwd = [(c, 1) for c in (3, 2, 1, 0)] + [(c, 1) for c in range(NCH - 1, 3, -1)]
        order = [x for pair in zip(fwd, bwd) for x in pair]
        seen_c = set()
        for (c, d) in order:
            second = c in seen_c
            seen_c.add(c)
            tk = slice(c * 64, (c + 1) * 64)
            sp_, spk = spr.next()
            P.load(sp_[:], sp[tk, :, :], spk)
            kt_, ktk = ktr.next()
            P.load(kt_[:], cktok[tk, :], ktk)
            v_, vk = vr.next()
            P.load(v_[:], cv[tk, :], vk)
            if second:
                r_, rk = rr.next()
                P.load(r_[:], crs[tk, :], rk)
            pb_, pbk = pb.next()
            for h in range(2):
                P.pe([spk, "tri"], [pbk], "matmul", pb_[0:64, h * 65:(h + 1) * 65], lhsT=sp_[:, d, 64 * h:64 * h + 64], rhs=tri[:, d, :], start=True, stop=True)
            pbd_, pbdk = pbd.next()
            P.pe([spk, "trix"], [pbdk], "matmul", pbd_[0:64, 0:128], lhsT=trix[:, d, :], rhs=sp_[:, d, :], start=True, stop=True)
            e1, e1k = E1.next()
            e2, e2k = E2.next()
            e3, e3k = E3.next()
            pbv = pb_[0:64, 0:130].rearrange("p (h n) -> p h n", h=2)
            P.act([pbk], [e1k], "activation", out=e1[:], in_=pbv, func=AF.Exp)
            P.act([pbk], [e2k], "activation", out=e2[:], in_=pbv[:, :, 0:64], func=AF.Exp, scale=-1.0)
            P.act([pbdk], [e3k], "activation", out=e3[:], in_=pbd_[0:64, 0:128], func=AF.Exp)
            qd_, qdk = qd.next()
            ki_, kik = ki.next()
            ke_, kek = ke.next()
            P.dve(["qT", e1k], [qdk], "tensor_tensor", out=qd_[:], in0=qT[:, :, tk], in1=e1[:, :, 0:64], op=ALU.mult)
            P.dve(["kT", e2k], [kik], "tensor_tensor", out=ki_[:], in0=kT[:, :, tk], in1=e2[:], op=ALU.mult)
            P.pool([ktk, e3k], [kek], "tensor_tensor", out=ke_[:], in0=kt_[:], in1=e3[:], op=ALU.mult)
            pa_, pak = patt.next()
            pav = pa_[0:64, 0:128].rearrange("p (h n) -> p h n", h=2)
            for h in range(2):
                P.pe([kik, qdk], [pak], "matmul", pa_[0:64, h * 64:(h + 1) * 64], lhsT=ki_[:, h, :], rhs=qd_[:, h, :], start=True, stop=True)
            at_, atk = att.next()
            P.dve([pak, "mask"], [atk], "tensor_tensor", out=at_[:], in0=pav, in1=mask[:, d, :, :], op=ALU.mult)
            po_, pok = po.next()
            for h in range(2):
                P.pe([atk, vk], [pok], "matmul", po_[0:64, 96 * h:96 * h + 96], lhsT=at_[:, h, :], rhs=v_[:, 96 * h:96 * h + 96], start=True, stop=False)
                P.pe([qdk, "Sb%d" % d], [pok], "matmul", po_[0:64, 96 * h:96 * h + 96], lhsT=qd_[:, h, :], rhs=Sb[d][:, h, :], start=False, stop=True)
            pu_, puk = pu.next()
            for h in range(2):
                P.pe([kek, vk], [puk], "matmul", pu_[0:64, 96 * h:96 * h + 96], lhsT=ke_[:, 64 * h:64 * h + 64], rhs=v_[:, 96 * h:96 * h + 96], start=True, stop=True)
            for h in range(2):
                P.dve(["S%d" % d, e1k, puk], ["S%d" % d], "scalar_tensor_tensor", out=S[d][:, h, :], in0=S[d][:, h, :], scalar=e1[:, h, 64:65],
                      in1=pu_[0:64, 96 * h:96 * h + 96], op0=ALU.mult, op1=ALU.add)
            P.pool(["S%d" % d], ["Sb%d" % d], "tensor_copy", out=Sb[d][:], in_=S[d][:])
            if not second:
                P.act([pok], ["OF%d" % c], "activation", out=OF[:, c, :], in_=po_[0:64, 0:192], func=AF.Copy)
            else:
                os_, osk = osum.next()
                P.dve([pok, "OF%d" % c], [osk], "tensor_tensor", out=os_[:], in0=po_[0:64, 0:192], in1=OF[:, c, :], op=ALU.add)
                s_, sk_ = ssr.next()
                for h in range(2):
                    j_, jkk = jk.next()
                    P.act([osk], [jkk, sk_], "activation", out=j_[:], in_=os_[:, 96 * h:96 * h + 96], func=AF.Square, accum_out=s_[:, h:h + 1])
                P.dve([sk_], [sk_], "tensor_scalar", out=s_[:, 2:4], in0=s_[:, 0:2], scalar1=1.0 / 96, scalar2=EPS, op0=ALU.mult, op1=ALU.add)
                P.act([sk_], [sk_], "activation", out=s_[:, 2:4], in_=s_[:, 2:4], func=AF.Sqrt)
                P.dve([sk_], [sk_], "reciprocal", out=s_[:, 2:4], in_=s_[:, 2:4])
                y_, yk = yo.next()
                for h in range(2):
                    P.dve([osk, sk_, "gc"], [yk], "scalar_tensor_tensor", out=y_[:, 96 * h:96 * h + 96], in0=os_[:, 96 * h:96 * h + 96],
                          scalar=s_[:, 2 + h:3 + h], in1=gc[:, 96 * h:96 * h + 96], op0=ALU.mult, op1=ALU.mult)
                P.pool([yk, rk], [yk], "tensor_tensor", out=y_[:], in0=y_[:], in1=r_[:], op=ALU.mult)
                P.store(mo[tk, :], y_[:], yk)
        P.end()


def phase_p3(P, io, E, FF, moe):
    FC = FF // 128
    NG = FF // 256
    xs = io["xs"]
    mo = io["mo"]
    modrow = io["modrow"]
    wout = io["wout"]
    router = io["router"]
    wg = io["wg"]
    wu = io["wu"]
    wd = io["wd"]
    idn = io["idn"]
    xo = io["xo"]
    wgd, wud, wdd = io["wgd"], io["wud"], io["wdd"]
    if True:
        P.begin()
        idf = P.sb("idf", [128, 128], F32)
        woutb = P.sb("woutb", [128, 8, D], BF16)
        rts = P.sb("rts", [128, 8, 8], F32)
        G1 = [P.sb("G1_%d" % i, [128, D], F32) for i in range(2)]
        A2 = [P.sb("A2_%d" % i, [128, D], F32) for i in range(2)]
        B2 = [P.sb("B2_%d" % i, [128, D], F32) for i in range(2)]
        G2 = [P.sb("G2_%d" % i, [128, D], F32) for i in range(2)]
        stage = sb_rot(P, "stage", [128, 2048], F32, 3)
        xrot = sb_rot(P, "xt", [128, D], F32, 2)
        mrot = sb_rot(P, "mt", [128, D], F32, 2)
        xnew = sb_rot(P, "xn", [128, D], F32, 3)
        yacc = sb_rot(P, "ya", [128, D], F32, 3)
        hrot = sb_rot(P, "hx", [128, D], F32, 2)
        ssr = sb_rot(P, "ss", [128, 2], F32, 2)
        catT = sb_rot(P, "catT", [128, 8, 128], BF16, 2)
        h2T = sb_rot(P, "h2T", [128, 8, ST], BF16, 2)
        h2Tf = P.sb("h2Tf", [128, 8, ST], F32) if moe else None
        cw = sb_rot(P, "cw", [128, 3, 8], F32, 2)
        lg = sb_rot(P, "lg", [128, 8], F32, 2)
        rt = sb_rot(P, "rtmp", [128, 4, 8], F32, 2)
        rs_ = sb_rot(P, "rsc", [128, 4], F32, 2)
        wgb = sb_rot(P, "wgb", [128, 8, 256], BF16, 2)
        wub = sb_rot(P, "wub", [128, 8, 256], BF16, 2)
        wdb = sb_rot(P, "wdb", [128, 2, D], BF16, 3)
        sgr = sb_rot(P, "sg", [128, ST], F32, 2)
        aTr = sb_rot(P, "aT", [128, ST], BF16, 3)
        bank = [P.ps("bk%d" % i, [128, 512], F32) for i in range(8)]
        bk = ["bk%d" % i for i in range(8)]
        P.load(idf[:], idn, "idf")
        if moe:
            P.load(rts[:], router, "rts")
        for v in range(2):
            P.load(G1[v][:], modrow[v:v + 1, 2, :].partition_broadcast(128), "G1_%d" % v)
            P.load(B2[v][:], modrow[v:v + 1, 3, :].partition_broadcast(128), "B2_%d" % v)
            P.load(A2[v][:], modrow[v:v + 1, 4, :].partition_broadcast(128), "A2_%d" % v)
            P.load(G2[v][:], modrow[v:v + 1, 5, :].partition_broadcast(128), "G2_%d" % v)
        for c in range(8):
            sg, sk = stage.next()
            P.load(sg[:, 0:D], wout[:, c, :], sk)
            (P.dve if c % 2 == 0 else P.pool)([sk], ["woutb"], "tensor_copy", out=woutb[:, c, :], in_=sg[:, 0:D])
        ccast = 0
        for e in range(E):
            for gi in range(NG):
                for (src_, dst_, rot_, shp) in ((wg[e, :, :, gi * 256:(gi + 1) * 256], wgd[e, :, :, gi * 256:(gi + 1) * 256], wgb, 8),
                                                (wu[e, :, :, gi * 256:(gi + 1) * 256], wud[e, :, :, gi * 256:(gi + 1) * 256], wub, 8),
                                                (wd[e, :, 2 * gi:2 * gi + 2, :], wdd[e, :, 2 * gi:2 * gi + 2, :], wdb, 2)):
                    sg, sk = stage.next()
                    P.load(sg[:].rearrange("p (c n) -> p c n", c=shp), src_, sk)
                    wb_, wbk = rot_.next()
                    if ccast % 3 == 0:
                        P.dve([sk], [wbk], "tensor_copy", out=wb_[:], in_=sg[:].rearrange("p (c n) -> p c n", c=shp))
                    elif ccast % 3 == 1:
                        P.pool([sk], [wbk], "tensor_copy", out=wb_[:], in_=sg[:].rearrange("p (c n) -> p c n", c=shp))
                    else:
                        P.act([sk], [wbk], "activation", out=wb_[:], in_=sg[:].rearrange("p (c n) -> p c n", c=shp), func=AF.Copy)
                    ccast += 1
                    P.store(dst_, wb_[:], wbk)
        for eng in ENGS:
            P.finish(eng)
        for s in range(NST):
            h2, h2k = h2T.next()
            cw_, cwk = cw.next()
            xns = []
            yas = []
            for t in range(3):
                g = 3 * s + t
                v = 1 if g < 2 else 0
                mt, mtk = mrot.next()
                P.load(mt[:], mo[g * 128:(g + 1) * 128, :], mtk)
                xt_, xk = xrot.next()
                P.load(xt_[:], xs[g * 128:(g + 1) * 128, :], xk)
                ct, ctk = catT.next()
                for hf in range(2):
                    for c in range(4):
                        P.pe([mtk, "idf"], [bk[6 + hf]], "transpose", out=bank[6 + hf][:, c * 128:(c + 1) * 128],
                             in_=mt[:, (4 * hf + c) * 128:(4 * hf + c + 1) * 128], identity=idf[:])
                    (P.act if hf == 0 else P.dve)([bk[6 + hf]], [ctk], *(("activation",) if hf == 0 else ("tensor_copy",)),
                                                  **(dict(out=ct[:, 4 * hf:4 * hf + 4, :], in_=bank[6 + hf][:].rearrange("p (c n) -> p c n", c=4), func=AF.Copy)
                                                     if hf == 0 else dict(out=ct[:, 4 * hf:4 * hf + 4, :], in_=bank[6 + hf][:].rearrange("p (c n) -> p c n", c=4))))
                xn_, xnk = xnew.next()
                for hf in range(2):
                    for c in range(8):
                        P.pe([ctk, "woutb"], [bk[1 + hf]], "matmul", bank[1 + hf][:, :], lhsT=ct[:, c, :], rhs=woutb[:, c, hf * 512:(hf + 1) * 512],
                             start=(c == 0), stop=(c == 7))
                    P.dve([bk[1 + hf], "G1_%d" % v], [xnk], "tensor_tensor", out=xn_[:, hf * 512:(hf + 1) * 512], in0=bank[1 + hf][:, :],
                          in1=G1[v][:, hf * 512:(hf + 1) * 512], op=ALU.mult)
                P.pool([xnk, xk], [xnk], "tensor_tensor", out=xn_[:], in0=xn_[:], in1=xt_[:], op=ALU.add)
                xns.append((xn_, xnk, v))
                ss_, sk_ = ssr.next()
                hx_, hxk = hrot.next()
                P.act([xnk], [hxk, sk_], "activation", out=hx_[:], in_=xn_[:], func=AF.Square, accum_out=ss_[:, 0:1])
                P.dve([sk_], [sk_], "tensor_scalar", out=ss_[:, 1:2], in0=ss_[:, 0:1], scalar1=1.0 / D, scalar2=EPS, op0=ALU.mult, op1=ALU.add)
                P.act([sk_], [sk_], "activation", out=ss_[:, 1:2], in_=ss_[:, 1:2], func=AF.Sqrt)
                P.dve([sk_], [sk_], "reciprocal", out=ss_[:, 1:2], in_=ss_[:, 1:2])
                P.dve([xnk, sk_, "A2_%d" % v], [hxk], "scalar_tensor_tensor", out=hx_[:], in0=xn_[:], scalar=ss_[:, 1:2], in1=A2[v][:],
                      op0=ALU.mult, op1=ALU.mult)
                P.pool([hxk, "B2_%d" % v], [hxk], "tensor_tensor", out=hx_[:], in0=hx_[:], in1=B2[v][:], op=ALU.add)
                for hf in range(2):
                    for c in range(4):
                        P.pe([hxk, "idf"], [bk[6 + hf]], "transpose", out=bank[6 + hf][:, c * 128:(c + 1) * 128],
                             in_=hx_[:, (4 * hf + c) * 128:(4 * hf + c + 1) * 128], identity=idf[:])
                    src = bank[6 + hf][:].rearrange("p (c n) -> p c n", c=4)
                    if moe:
                        P.dve([bk[6 + hf]], ["h2Tf"], "tensor_copy", out=h2Tf[:, 4 * hf:4 * hf + 4, t * 128:(t + 1) * 128], in_=src)
                        P.pool(["h2Tf"], [h2k], "tensor_copy", out=h2[:, 4 * hf:4 * hf + 4, t * 128:(t + 1) * 128],
                               in_=h2Tf[:, 4 * hf:4 * hf + 4, t * 128:(t + 1) * 128])
                    else:
                        P.act([bk[6 + hf]], [h2k], "activation", out=h2[:, 4 * hf:4 * hf + 4, t * 128:(t + 1) * 128], in_=src, func=AF.Copy)
                if moe:
                    for c in range(8):
                        P.pe(["h2Tf", "rts"], [bk[3]], "matmul", bank[3][:, 0:8], lhsT=h2Tf[:, c, t * 128:(t + 1) * 128], rhs=rts[:, c, :],
                             start=(c == 0), stop=(c == 7))
                    l_, lk = lg.next()
                    r_, rk = rt.next()
                    q_, qk = rs_.next()
                    P.dve([bk[3]], [lk], "tensor_copy", out=l_[:], in_=bank[3][:, 0:8])
                    P.dve([lk], [qk], "reduce_max", out=q_[:, 0:1], in_=l_[:], axis=AX.X)
                    P.dve([lk, qk], [rk], "tensor_scalar", out=r_[:, 0, :], in0=l_[:], scalar1=q_[:, 0:1], scalar2=None, op0=ALU.is_equal)
                    P.dve([rk, lk], [rk], "scalar_tensor_tensor", out=r_[:, 1, :], in0=r_[:, 0, :], scalar=-1e30, in1=l_[:], op0=ALU.mult, op1=ALU.add)
                    P.dve([rk], [qk], "reduce_max", out=q_[:, 1:2], in_=r_[:, 1, :], axis=AX.X)
                    P.dve([lk, qk], [rk], "tensor_scalar", out=r_[:, 2, :], in0=l_[:], scalar1=q_[:, 1:2], scalar2=None, op0=ALU.is_ge)
                    P.dve([qk], [qk], "tensor_scalar", out=q_[:, 2:3], in0=q_[:, 0:1], scalar1=-1.0, scalar2=None, op0=ALU.mult)
                    P.act([lk, qk], [rk], "activation", out=r_[:, 3, :], in_=l_[:], func=AF.Exp, bias=q_[:, 2:3])
                    P.dve([rk], [rk], "tensor_tensor", out=r_[:, 3, :], in0=r_[:, 3, :], in1=r_[:, 2, :], op=ALU.mult)
                    P.dve([rk], [qk], "reduce_sum", out=q_[:, 3:4], in_=r_[:, 3, :], axis=AX.X)
                    P.dve([qk], [qk], "reciprocal", out=q_[:, 3:4], in_=q_[:, 3:4])
                    P.dve([rk, qk], [cwk], "tensor_scalar", out=cw_[:, t, :], in0=r_[:, 3, :], scalar1=q_[:, 3:4], scalar2=None, op0=ALU.mult)
            for t in range(3):
                ya_, yak = yacc.next()
                yas.append((ya_, yak))
            pending = None
            for e in range(E):
                for gi in range(NG):
                    tiles = []
                    for (src_, rot_) in ((wgd[e, :, :, gi * 256:(gi + 1) * 256], wgb), (wud[e, :, :, gi * 256:(gi + 1) * 256], wub),
                                         (wdd[e, :, 2 * gi:2 * gi + 2, :], wdb)):
                        wb_, wbk = rot_.next()
                        P.load(wb_[:], src_, wbk)
                        tiles.append((wb_, wbk))
                    (wg_, wgk), (wu_, wuk), (wd_, wdk) = tiles
                    for j in range(2):
                        for c in range(8):
                            P.pe([wgk, h2k], [bk[6]], "matmul", bank[6][:, 0:ST], lhsT=wg_[:, c, j * 128:(j + 1) * 128], rhs=h2[:, c, :],
                                 start=(c == 0), stop=(c == 7))
                        for c in range(8):
                            P.pe([wuk, h2k], [bk[7]], "matmul", bank[7][:, 0:ST], lhsT=wu_[:, c, j * 128:(j + 1) * 128], rhs=h2[:, c, :],
                                 start=(c == 0), stop=(c == 7))
                        sg_, sgk = sgr.next()
                        P.act([bk[6]], [sgk], "activation", out=sg_[:], in_=bank[6][:, 0:ST], func=AF.Silu)
                        a_, ak = aTr.next()
                        P.dve([sgk, bk[7]], [ak], "tensor_tensor", out=a_[:], in0=sg_[:], in1=bank[7][:, 0:ST], op=ALU.mult)
                        first = (gi == 0 and j == 0)
                        last = (gi == NG - 1 and j == 1)
                        if pending is not None:
                            pending()

                        def down(a_=a_, ak=ak, wd_=wd_, wdk=wdk, j=j, first=first, last=last):
                            for t in range(3):
                                for hf in range(2):
                                    b_ = 2 * t + hf
                                    P.pe([ak, wdk], [bk[b_]], "matmul", bank[b_][:, :], lhsT=a_[:, t * 128:(t + 1) * 128],
                                         rhs=wd_[:, j, hf * 512:(hf + 1) * 512], start=first, stop=last)
                        pending = down
                pending()
                pending = None
                for t in range(3):
                    ya_, yak = yas[t]
                    for hf in range(2):
                        b_ = 2 * t + hf
                        osl = ya_[:, hf * 512:(hf + 1) * 512]
                        if not moe:
                            P.act([bk[b_]], [yak], "activation", out=osl, in_=bank[b_][:, :], func=AF.Copy)
                        elif e == 0:
                            P.dve([bk[b_], cwk], [yak], "tensor_scalar", out=osl, in0=bank[b_][:, :], scalar1=cw_[:, t, e:e + 1], scalar2=None, op0=ALU.mult)
                        else:
                            P.dve([bk[b_], cwk, yak], [yak], "scalar_tensor_tensor", out=osl, in0=bank[b_][:, :], scalar=cw_[:, t, e:e + 1], in1=osl,
                                  op0=ALU.mult, op1=ALU.add)
            for t in range(3):
                g = 3 * s + t
                ya_, yak = yas[t]
                xn_, xnk, v = xns[t]
                P.dve([yak, "G2_%d" % v], [yak], "tensor_tensor", out=ya_[:], in0=ya_[:], in1=G2[v][:], op=ALU.mult)
                P.pool([yak, xnk], [yak], "tensor_tensor", out=ya_[:], in0=ya_[:], in1=xn_[:], op=ALU.add)
                P.store(xo[g * 128:(g + 1) * 128, :], ya_[:], yak)
        P.end()


def phase_p4(P, io):
    xs = io["xs"]
    fg = io["fg"]
    xo = io["xo"]
    if True:
        P.begin()
        g_ = P.sb("g_", [128, D], F32)
        xrot = sb_rot(P, "xt", [128, D], F32, 3)
        orot = sb_rot(P, "ot", [128, D], F32, 3)
        ssr = sb_rot(P, "ss", [128, 2], F32, 3)
        P.load(g_[:], fg[0:1, :].partition_broadcast(128), "g_")
        for g in range(2, TC // 128):
            xt_, xk = xrot.next()
            P.load(xt_[:], xs[g * 128:(g + 1) * 128, :], xk)
            o_, ok = orot.next()
            ss_, sk_ = ssr.next()
            P.act([xk], [ok, sk_], "activation", out=o_[:], in_=xt_[:], func=AF.Square, accum_out=ss_[:, 0:1])
            P.dve([sk_], [sk_], "tensor_scalar", out=ss_[:, 1:2], in0=ss_[:, 0:1], scalar1=1.0 / D, scalar2=EPS, op0=ALU.mult, op1=ALU.add)
            P.act([sk_], [sk_], "activation", out=ss_[:, 1:2], in_=ss_[:, 1:2], func=AF.Sqrt)
            P.dve([sk_], [sk_], "reciprocal", out=ss_[:, 1:2], in_=ss_[:, 1:2])
            P.dve([xk, sk_, "g_"], [ok], "scalar_tensor_tensor", out=o_[:], in0=xt_[:], scalar=ss_[:, 1:2], in1=g_[:], op0=ALU.mult, op1=ALU.mult)
            P.store(xo[(g - 2) * 128:(g - 1) * 128, :], o_[:], ok)
        P.end()


def build_fused(depth=DEPTH):
    nc = bass.Bass("TRN2", target_bir_lowering=False)

    def din(name, shape, dt=F32):
        return nc.dram_tensor(name, list(shape), dt, kind="ExternalInput").ap()

    def scratch(name, shape, dt=F32):
        return nc.dram_tensor(name, list(shape), dt).ap()

    xs = din("xs", [TB, D])
    cT = din("cT", [128, 8, 2])
    tabs = din("tabs", [128, 4, TB])
    idn = din("idn", [128, 128])
    ada_w = din("ada_w", [DEPTH, 128, 8, 6 * D])
    ada_b2 = din("ada_b2", [DEPTH, 2, 6 * D])
    ng = din("ng", [DEPTH, 2, 2, D])
    w1 = din("w1", [DEPTH, 128, 8, NW1])
    w2f = din("w2f", [DEPTH, 33, 512])
    lamb = din("lamb", [DEPTH, 128, 4, 32])
    cst = din("cst", [DEPTH, 128, 66])
    sink = din("sink", [DEPTH, 2, 128, 3])
    masks = din("masks", [128, 2, 3, 128])
    tri = din("tri", [64, 2, 65])
    trix = din("trix", [64, 2, 64])
    gmask = din("gmask", [64, 2, 2, 64])
    gc = din("gc", [DEPTH, 64, 192])
    wout = din("wout", [DEPTH, 128, 8, D])
    ffg = din("ffg", [2, 1, 128, 8, D_FF])
    ffu = din("ffu", [2, 1, 128, 8, D_FF])
    ffd = din("ffd", [2, 1, 128, D_FF // 128, D])
    mog = din("mog", [2, NEXP, 128, 8, D_FFE])
    mou = din("mou", [2, NEXP, 128, 8, D_FFE])
    mod_ = din("mod_", [2, NEXP, 128, D_FFE // 128, D])
    router = din("router", [2, 128, 8, 8])
    fg = din("fg", [1, D])
    out = nc.dram_tensor("out", [SEQ, D], F32, kind="ExternalOutput").ap()
    X = [scratch("X%d" % i, [TB, D]) for i in range(2)]
    MODROW = scratch("MODROW", [2, 6, D])
    FEAT = scratch("FEAT", [FEAT_ROWS, TB], BF16)
    TOKB = scratch("TOKB", [TB, 1024], BF16)
    TOKF = scratch("TOKF", [TB, 896])
    MO = scratch("MO", [TB, D])
    WGD = scratch("WGD", [NEXP, 128, 8, D_FFE], BF16)
    WUD = scratch("WUD", [NEXP, 128, 8, D_FFE], BF16)
    WDD = scratch("WDD", [NEXP, 128, D_FFE // 128, D], BF16)
    FGD = scratch("FGD", [1, 128, 8, D_FF], BF16)
    FUD = scratch("FUD", [1, 128, 8, D_FF], BF16)
    FDD = scratch("FDD", [1, 128, D_FF // 128, D], BF16)
    with ExitStack() as st:
        P = Prog(nc, st)
        xin = xs
        for L in range(depth):
            xout = X[L % 2]
            phase_p1(P, dict(xs=xin, cT=cT, ada_w=ada_w[L], ada_b2=ada_b2[L], ng=ng[L], w1=w1[L], w2f=w2f[L], tabs=tabs, idn=idn,
                             modrow=MODROW, feat=FEAT, tokb=TOKB, tokf=TOKF))
            for hh in range(2):
                phase_p2a(P, dict(aqt=FEAT[128 * hh:128 * hh + 128, :], akt=FEAT[256 + 128 * hh:256 + 128 * hh + 128, :],
                                  av=TOKB[:, 128 * hh:128 * hh + 128], lamb=lamb[L], cst=cst[L], idn=idn, mo=MO[:, 128 * hh:128 * hh + 128]))
                phase_p2b(P, dict(bqt=FEAT[512 + 192 * hh:512 + 192 * hh + 192, :], bkt=FEAT[896 + 64 * hh:896 + 64 * hh + 64, :],
                                  bv=TOKB[:, 256 + 64 * hh:256 + 64 * hh + 64], sink=sink[L, hh], masks=masks,
                                  mo=MO[:, 256 + 192 * hh:256 + 192 * hh + 192]))
                phase_p2c(P, dict(cqt=FEAT[1024 + 128 * hh:1024 + 128 * hh + 128, :], ckt=FEAT[1280 + 128 * hh:1280 + 128 * hh + 128, :],
                                  cktok=TOKB[:, 768 + 128 * hh:768 + 128 * hh + 128], cv=TOKB[:, 384 + 192 * hh:384 + 192 * hh + 192],
                                  crs=TOKF[:, 192 * hh:192 * hh + 192],
                                  sp=TOKF[:, 384:896].rearrange("t (d c) -> t d c", d=2)[:, :, 128 * hh:128 * hh + 128],
                                  tri_d=tri, trix_d=trix, mask_d=gmask, gc_d=gc[L], mo=MO[:, 640 + 192 * hh:640 + 192 * hh + 192]))
            j = L // 2
            if L % 2 == 0:
                phase_p3(P, dict(xs=xin, mo=MO, modrow=MODROW, wout=wout[L], router=router[0], wg=ffg[j], wu=ffu[j], wd=ffd[j],
                                 idn=idn, xo=xout, wgd=FGD, wud=FUD, wdd=FDD), 1, D_FF, False)
            else:
                phase_p3(P, dict(xs=xin, mo=MO, modrow=MODROW, wout=wout[L], router=router[j], wg=mog[j], wu=mou[j], wd=mod_[j],
                                 idn=idn, xo=xout, wgd=WGD, wud=WUD, wdd=WDD), NEXP, D_FFE, True)
            xin = xout
        phase_p4(P, dict(xs=xin, fg=fg, xo=out))
    return nc


_PROG = []


def _c(a):
    return np.ascontiguousarray(a)


def kernel(x, c, ctx, c_ctx, norm1_g, norm2_g, ada_w, ada_b, w_in, w_out, a_lambda, a_norm_g,
           b_sink, c_gate_w2, c_gate_b, c_norm_g, ffn_w_gate, ffn_w_up, ffn_w_down,
           moe_router, moe_w_gate, moe_w_up, moe_w_down, final_g):
    f = lambda a: np.asarray(a, np.float32)
    x, c, ctx, c_ctx = f(x), f(c), f(ctx), f(c_ctx)
    if not _PROG:
        _PROG.append(build_fused())
    nc = _PROG[0]
    cols = w1_columns()
    tabs = _c(rope_tables().transpose(1, 0, 2))
    tri, trix, gmask = gla_consts()
    shared = {
        "tabs": tabs, "idn": np.eye(128, dtype=np.float32),
        "ada_w": np.stack([kmajor(f(ada_w[L])) for L in range(DEPTH)]),
        "ada_b2": np.stack([np.stack([f(ada_b[L])] * 2) for L in range(DEPTH)]),
        "ng": np.stack([np.stack([np.stack([f(norm1_g[L]), f(norm2_g[L])])] * 2) for L in range(DEPTH)]),
        "w1": np.stack([kmajor(take_cols(f(w_in[L]), cols)) for L in range(DEPTH)]),
        "w2f": np.stack([w2full(f(c_gate_w2[L]), f(c_gate_b[L])) for L in range(DEPTH)]),
        "lamb": _c(np.broadcast_to(f(a_lambda)[:, None], (DEPTH, 128, 4, 32))),
        "masks": band_masks(), "tri": tri, "trix": trix, "gmask": gmask,
        "gc": _c(np.broadcast_to(np.tile(f(c_norm_g), (1, 2))[:, None, :], (DEPTH, 64, 192))),
        "wout": np.stack([kmajor(f(w_out[L])) for L in range(DEPTH)]),
        "ffg": np.stack([kmajor(f(ffn_w_gate[j]))[None] for j in range(2)]),
        "ffu": np.stack([kmajor(f(ffn_w_up[j]))[None] for j in range(2)]),
        "ffd": np.stack([kmajor(f(ffn_w_down[j]))[None] for j in range(2)]),
        "mog": np.stack([np.stack([kmajor(f(moe_w_gate[j][e])) for e in range(NEXP)]) for j in range(2)]),
        "mou": np.stack([np.stack([kmajor(f(moe_w_up[j][e])) for e in range(NEXP)]) for j in range(2)]),
        "mod_": np.stack([np.stack([kmajor(f(moe_w_down[j][e])) for e in range(NEXP)]) for j in range(2)]),
        "router": np.stack([kmajor(f(moe_router[j])) for j in range(2)]),
        "fg": _c(f(final_g)[None, :]),
    }
    cst = np.zeros((DEPTH, 128, 66), np.float32)
    for L in range(DEPTH):
        lam_init = 0.8 - 0.6 * math.exp(-0.3 * L)
        cst[L, :, :64] = f(a_norm_g[L])[None, :]
        cst[L, :, 64] = lam_init
        cst[L, :, 65] = 1.0 - lam_init
    shared["cst"] = cst
    shared["sink"] = _c(np.broadcast_to(f(b_sink).reshape(DEPTH, 2, 1, 3), (DEPTH, 2, 128, 3)))
    in_maps = []
    for i in range(NCORE):
        b = i // 2
        m = dict(shared)
        m["xs"] = _c(np.concatenate([ctx[b], x[b]], 0))
        m["cT"] = _c(np.stack([c[b].reshape(8, 128).T, c_ctx.reshape(8, 128).T], -1))
        in_maps.append(m)
    res = run_bass_kernel_spmd(nc, in_maps, core_ids=list(range(NCORE)))
    out = np.stack([np.asarray(res.results[2 * b]["out"]) for b in range(BATCH)], 0)
    return np.ascontiguousarray(out.astype(np.float32))
```

```python
import math
from contextlib import ExitStack

import ml_dtypes
import numpy as np

import concourse.bass as bass
import concourse.mybir as mybir
from concourse.bass_utils import run_bass_kernel_spmd

F32 = mybir.dt.float32
BF16 = mybir.dt.bfloat16
AF = mybir.ActivationFunctionType
ALU = mybir.AluOpType
AX = mybir.AxisListType
ENGS = ("tensor", "vector", "scalar", "gpsimd", "sync")
NPBF = ml_dtypes.bfloat16

D = 1024
BATCH = 4
SEQ = 8192
CTX = 256
DEPTH = 4
TB = CTX + SEQ
NCORE = 8
TC = TB
EPS = 1e-6
D_FF = 2816
D_FFE = 3584
NEXP = 8


class Prog:
    def __init__(self, nc, stack, strict_same_engine=True):
        self.nc = nc
        self.sem_stack = stack
        self.stack = stack
        self.phase = 0
        self.ops = {e: [] for e in ENGS}
        self.sems = {}
        self.inc = {}
        self.cnt = {}
        self.seen = {e: {} for e in ENGS}
        self.buf = {}
        self.strict = strict_same_engine
        self.nps = 0
        for e in ENGS[:4]:
            self._mk(e, 1)

    def _mk(self, v, inc):
        self.sems[v] = self.sem_stack.enter_context(self.nc.semaphore("s_" + v.replace(":", "_")))
        self.inc[v] = inc
        self.cnt[v] = 0

    def sb(self, name, shape, dt):
        return self.stack.enter_context(self.nc.sbuf_tensor("%s_p%d" % (name, self.phase), list(shape), dt))

    def ps(self, name, shape, dt=F32):
        return self.stack.enter_context(self.nc.psum_tensor("%s_p%d" % (name, self.phase), list(shape), dt))

    def begin(self):
        self.phase += 1
        self.stack = ExitStack()
        self.stack.__enter__()

    def end(self):
        for eng in ENGS:
            self.finish(eng)
        self.emit()
        self.ops = {e: [] for e in ENGS}
        self.stack.__exit__(None, None, None)
        self.stack = None

    def _deps(self, reads, writes):
        deps = {}

        def add(vk):
            if vk is None:
                return
            v, k = vk
            if deps.get(v, 0) < k:
                deps[v] = k
        for b in reads:
            st = self.buf.get(b)
            if st:
                add(st[0])
        for b in writes:
            st = self.buf.get(b)
            if st:
                add(st[0])
                for r in st[1]:
                    add(r)
        return deps

    def op(self, eng, fn, reads=(), writes=(), slot=None):
        v = eng if slot is None else "dma:" + slot
        if v not in self.sems:
            self._mk(v, 16)
        deps = self._deps(reads, writes)
        for dv, k in deps.items():
            if dv == eng and slot is None:
                if eng == "tensor" or not self.strict or self.cnt[eng] + 1 - k >= 3:
                    continue
            if self.seen[eng].get(dv, 0) >= k:
                continue
            self.seen[eng][dv] = k
            self.ops[eng].append(("w", dv, k * self.inc[dv]))
        self.cnt[v] += 1
        k = self.cnt[v]
        self.ops[eng].append(("i", fn, v))
        for b in reads:
            st = self.buf.setdefault(b, [None, []])
            st[1].append((v, k))
        for b in writes:
            self.buf[b] = [(v, k), []]
        return (v, k)

    def pe(self, r, w, m, *a, **k):
        return self.op("tensor", (m, a, k), r, w)

    def dve(self, r, w, m, *a, **k):
        return self.op("vector", (m, a, k), r, w)

    def act(self, r, w, m, *a, **k):
        return self.op("scalar", (m, a, k), r, w)

    def pool(self, r, w, m, *a, **k):
        return self.op("gpsimd", (m, a, k), r, w)

    def load(self, out_ap, in_ap, key, r=()):
        return self.op("sync", ("dma_start", (), dict(out=out_ap, in_=in_ap)), r, [key], slot=key)

    def store(self, out_ap, in_ap, key, w=()):
        return self.op("gpsimd", ("dma_start", (), dict(out=out_ap, in_=in_ap)), [key], w, slot="st_" + key)

    def finish(self, eng="sync"):
        for v, c in self.cnt.items():
            if c and self.seen[eng].get(v, 0) < c:
                self.ops[eng].append(("w", v, c * self.inc[v]))
                self.seen[eng][v] = c

    def emit(self):
        nc = self.nc
        with nc.Block() as block:
            def run(engname):
                def body(e):
                    for o in self.ops[engname]:
                        if o[0] == "w":
                            e.wait_ge(self.sems[o[1]], o[2])
                        else:
                            getattr(e, o[1][0])(*o[1][1], **o[1][2]).then_inc(self.sems[o[2]], self.inc[o[2]])
                return body
            block.sync(run("sync"))
            block.tensor(run("tensor"))
            block.vector(run("vector"))
            block.scalar(run("scalar"))
            block.gpsimd(run("gpsimd"))


class Rot:
    def __init__(self, tiles, name):
        self.tiles = tiles
        self.name = name
        self.i = 0

    def next(self):
        j = self.i % len(self.tiles)
        self.i += 1
        return self.tiles[j], "%s%d" % (self.name, j)


def sb_rot(P, name, shape, dt, n):
    return Rot([P.sb("%s%d" % (name, j), shape, dt) for j in range(n)], name)


def ps_rot(P, name, n, shape=(128, 512), dt=F32):
    return Rot([P.ps("%s%d" % (name, j), shape, dt) for j in range(n)], name)


NWF = 2592
NWT = 1408
NW1 = NWF + NWT
ST = 384
NST = TC // ST
FEAT_ROWS = 1536


def phase_p1(P, io):
    xs = io["xs"]
    cT = io["cT"]
    ada_w = io["ada_w"]
    ada_b2 = io["ada_b2"]
    ng = io["ng"]
    w1 = io["w1"]
    w2f = io["w2f"]
    tabs = io["tabs"]
    idn = io["idn"]
    modrow = io["modrow"]
    feat = io["feat"]
    tokb = io["tokb"]
    tokf = io["tokf"]
    if True:
        P.begin()
        idf = P.sb("idf", [128, 128], F32)
        cTs = P.sb("cTs", [128, 8, 2], F32)
        scT = P.sb("scT", [128, 8, 2], F32)
        wfb = P.sb("wfb", [128, 8, NW1], BF16)
        w2s = P.sb("w2s", [33, 512], F32)
        ngs = P.sb("ngs", [2, 2, D], F32)
        modsb = P.sb("modsb", [2, 6 * D], F32)
        A1 = [P.sb("A1_%d" % i, [128, D], F32) for i in range(2)]
        B1 = [P.sb("B1_%d" % i, [128, D], F32) for i in range(2)]
        stage = sb_rot(P, "stage", [128, 2048], F32, 2)
        xrot = sb_rot(P, "xt", [128, D], F32, 2)
        hrot = sb_rot(P, "hx", [128, D], F32, 2)
        ssr = sb_rot(P, "ss", [128, 2], F32, 2)
        hT = sb_rot(P, "hT", [128, 8, ST], BF16, 2)
        glT = P.sb("glT", [33, ST], F32)
        tabr = sb_rot(P, "tab", [128, 4, ST], F32, 2)
        t1r = sb_rot(P, "t1", [128, ST], F32, 2)
        t2r = sb_rot(P, "t2", [128, ST], F32, 2)
        fo = sb_rot(P, "fo", [128, ST], BF16, 4)
        tbo = sb_rot(P, "tbo", [128, 1024], BF16, 2)
        tfo = sb_rot(P, "tfo", [128, 896], F32, 2)
        ez = sb_rot(P, "ez", [128, 512], F32, 2)
        pT = ps_rot(P, "pT", 2, (128, 4, 128))
        pg = ps_rot(P, "pg", 6)

        P.load(idf[:], idn, "idf")
        P.load(cTs[:], cT, "cTs")
        P.load(w2s[:], w2f, "w2s")
        P.load(modsb[:], ada_b2, "modsb")
        P.load(ngs[:], ng, "ngs")
        P.act(["cTs"], ["scT"], "activation", out=scT[:], in_=cTs[:], func=AF.Silu)
        P.pool([], ["glT"], "memset", glT[:], 1.0)
        for j in range(24):
            sg, sk = stage.next()
            P.load(sg[:].rearrange("p (c n) -> p c n", c=8), ada_w[:, :, j * 256:(j + 1) * 256], sk)
            pm, pk = pg.next()
            for c in range(8):
                P.pe(["scT", sk], [pk], "matmul", pm[0:2, 0:256], lhsT=scT[:, c, :], rhs=sg[:, c * 256:(c + 1) * 256],
                     start=(c == 0), stop=(c == 7))
            P.dve([pk, "modsb"], ["modsb"], "tensor_tensor", out=modsb[:, j * 256:(j + 1) * 256], in0=pm[0:2, 0:256],
                  in1=modsb[:, j * 256:(j + 1) * 256], op=ALU.add)
        for which in range(2):
            isc = 3 * which + 1
            P.dve(["modsb", "ngs"], ["modsb"], "scalar_tensor_tensor",
                  out=modsb[:, isc * D:(isc + 1) * D], in0=modsb[:, isc * D:(isc + 1) * D], scalar=1.0, in1=ngs[:, which, :],
                  op0=ALU.add, op1=ALU.mult)
        P.store(modrow, modsb[:].rearrange("p (a d) -> p a d", a=6), "modsb", ["MODROW"])
        for v in range(2):
            P.load(A1[v][:], modrow[v:v + 1, 1, :].partition_broadcast(128), "A1_%d" % v, ["MODROW"])
            P.load(B1[v][:], modrow[v:v + 1, 0, :].partition_broadcast(128), "B1_%d" % v, ["MODROW"])
        HW1 = NW1 // 2
        for c in range(16):
            sg, sk = stage.next()
            kc, hf = divmod(c, 2)
            P.load(sg[:, 0:HW1], w1[:, kc, hf * HW1:(hf + 1) * HW1], sk)
            (P.dve if c % 2 == 0 else P.pool)([sk], ["wfb"], "tensor_copy", out=wfb[:, kc, hf * HW1:(hf + 1) * HW1], in_=sg[:, 0:HW1])

        rope_pairs = [(0, 2, 0, 0), (1, 3, 0, 128), (4, 6, 0, 256), (5, 7, 0, 384),
                      (8, 11, 2, 512), (9, 12, 2, 640), (10, 13, 2, 768), (14, 15, 2, 896)]
        plain = [(16, 1024, 48 ** -0.5), (17, 1152, 48 ** -0.5), (18, 1280, 1.0), (19, 1408, 1.0)]
        for s in range(NST):
            hTt, hk = hT.next()
            tb_, tk = tabr.next()
            P.load(tb_[:], tabs[:, :, s * ST:(s + 1) * ST], tk)
            for t in range(3):
                g = 3 * s + t
                v = 1 if g < 2 else 0
                xt_, xk = xrot.next()
                P.load(xt_[:], xs[g * 128:(g + 1) * 128, :], xk)
                ss_, sk_ = ssr.next()
                hx_, hxk = hrot.next()
                P.act([xk], [hxk, sk_], "activation", out=hx_[:], in_=xt_[:], func=AF.Square, accum_out=ss_[:, 0:1])
                P.act([sk_], [sk_], "activation", out=ss_[:, 1:2], in_=ss_[:, 0:1], func=AF.Ln, scale=1.0 / D, bias=EPS)
                P.act([sk_], [sk_], "activation", out=ss_[:, 1:2], in_=ss_[:, 1:2], func=AF.Exp, scale=-0.5)
                P.dve([xk, sk_, "A1_%d" % v], [hxk], "scalar_tensor_tensor",
                      out=hx_[:], in0=xt_[:], scalar=ss_[:, 1:2], in1=A1[v][:], op0=ALU.mult, op1=ALU.mult)
                P.pool([hxk, "B1_%d" % v], [hxk], "tensor_tensor", out=hx_[:], in0=hx_[:], in1=B1[v][:], op=ALU.add)
                for hf in range(2):
                    pt_, ptk = pT.next()
                    for c in range(4):
                        P.pe([hxk, "idf"], [ptk], "transpose", out=pt_[:, c, :], in_=hx_[:, (4 * hf + c) * 128:(4 * hf + c + 1) * 128], identity=idf[:])
                    if hf == 0:
                        P.act([ptk], [hk], "activation", out=hTt[:, 0:4, t * 128:(t + 1) * 128], in_=pt_[:], func=AF.Copy)
                    else:
                        P.dve([ptk], [hk], "tensor_copy", out=hTt[:, 4:8, t * 128:(t + 1) * 128], in_=pt_[:])

            def fm(pd, pk_, f0, ncols, hTt, hk):
                for c in range(8):
                    P.pe(["wfb", hk], [pk_], "matmul", pd[0:ncols, 0:ST], lhsT=wfb[:, c, f0:f0 + ncols], rhs=hTt[:, c, :],
                         start=(c == 0), stop=(c == 7))

            for (fx, fp, ti, row0) in rope_pairs:
                px, pxk = pg.next()
                pp, ppk = pg.next()
                fm(px, pxk, fx * 128, 128, hTt, hk)
                fm(pp, ppk, fp * 128, 128, hTt, hk)
                a_, ak = t1r.next()
                b_, bk = t2r.next()
                P.dve([pxk, tk], [ak], "tensor_tensor", out=a_[:], in0=px[:, 0:ST], in1=tb_[:, ti, :], op=ALU.mult)
                P.dve([ppk, tk], [bk], "tensor_tensor", out=b_[:], in0=pp[:, 0:ST], in1=tb_[:, ti + 1, :], op=ALU.mult)
                o_, ok = fo.next()
                P.pool([ak, bk], [ok], "tensor_tensor", out=o_[:], in0=a_[:], in1=b_[:], op=ALU.add)
                P.store(feat[row0:row0 + 128, s * ST:(s + 1) * ST], o_[:], ok)
            for (f, row0, scl) in plain:
                px, pxk = pg.next()
                fm(px, pxk, f * 128, 128, hTt, hk)
                o_, ok = fo.next()
                P.act([pxk], [ok], "activation", out=o_[:], in_=px[:, 0:ST], func=AF.Copy, scale=scl)
                P.store(feat[row0:row0 + 128, s * ST:(s + 1) * ST], o_[:], ok)
            px, pxk = pg.next()
            fm(px, pxk, 2560, 32, hTt, hk)
            P.act([pxk], ["glT"], "activation", out=glT[0:32, :], in_=px[0:32, 0:ST], func=AF.Copy)
            for t in range(3):
                g = 3 * s + t
                tb2, tbk = tbo.next()
                tf2, tfk = tfo.next()
                for (c0, n) in ((0, 512), (512, 512), (1024, 384)):
                    px, pxk = pg.next()
                    for c in range(8):
                        P.pe(["wfb", hk], [pxk], "matmul", px[:, 0:n], lhsT=hTt[:, c, t * 128:(t + 1) * 128],
                             rhs=wfb[:, c, NWF + c0:NWF + c0 + n], start=(c == 0), stop=(c == 7))
                    if c0 < 1024:
                        P.dve([pxk], [tbk], "tensor_copy", out=tb2[:, c0:c0 + 512], in_=px[:, 0:512])
                    else:
                        P.act([pxk], [tfk], "activation", out=tf2[:, 0:384], in_=px[:, 0:384], func=AF.Silu)
                pz, pzk = pg.next()
                P.pe(["glT", "w2s"], [pzk], "matmul", pz[:, :], lhsT=glT[0:33, t * 128:(t + 1) * 128], rhs=w2s[:, :], start=True, stop=True)
                ez_, ezk = ez.next()
                P.act([pzk], [ezk], "activation", out=ez_[:], in_=pz[:], func=AF.Exp, scale=-1.0)
                P.act([ezk], [tfk], "activation", out=tf2[:, 384:896], in_=ez_[:], func=AF.Ln, bias=1.0)
                P.store(tokb[g * 128:(g + 1) * 128, :], tb2[:], tbk)
                P.store(tokf[g * 128:(g + 1) * 128, :], tf2[:], tfk)
        P.end()


A_Q0, A_K0, A_V0 = 0, 256, 512
B_Q0, B_K0, B_V0 = 768, 1152, 1280
C_Q0, C_K0, C_V0, C_R0, C_G0 = 1408, 1600, 1792, 2176, 2560


def _rope_perm(dim):
    q = dim // 4
    perm = np.zeros(dim, np.int64)
    sign = np.zeros(dim, np.float32)
    for d in range(dim):
        blk = d // q
        if blk % 2 == 0:
            perm[d] = d + q
            sign[d] = -1.0
        else:
            perm[d] = d - q
            sign[d] = 1.0
    return perm, sign


def w1_columns():
    cols = []
    pA, _ = _rope_perm(32)
    pB, _ = _rope_perm(64)

    def permuted(base, n, dim, perm):
        out = []
        for j in range(n):
            hd, d = divmod(j, dim)
            out.append(base + hd * dim + int(perm[d]))
        return out
    cols += list(range(A_Q0, A_Q0 + 256)) + permuted(A_Q0, 256, 32, pA)
    cols += list(range(A_K0, A_K0 + 256)) + permuted(A_K0, 256, 32, pA)
    cols += list(range(B_Q0, B_Q0 + 384)) + permuted(B_Q0, 384, 64, pB)
    cols += list(range(B_K0, B_K0 + 128)) + permuted(B_K0, 128, 64, pB)

    def padded(base):
        out = []
        for h in range(4):
            out += list(range(base + 48 * h, base + 48 * h + 48)) + [-1] * 16
        return out
    cols += padded(C_Q0) + padded(C_K0)
    cols += list(range(C_G0, C_G0 + 32))
    assert len(cols) == NWF
    cols += list(range(A_V0, A_V0 + 256)) + list(range(B_V0, B_V0 + 128)) + list(range(C_V0, C_V0 + 384))
    cols += padded(C_K0) + list(range(C_R0, C_R0 + 384))
    assert len(cols) == NW1
    return np.array(cols, np.int64)


def take_cols(w, cols):
    out = np.zeros((w.shape[0], len(cols)), w.dtype)
    m = cols >= 0
    out[:, m] = w[:, cols[m]]
    return out


def kmajor(w):
    K, N = w.shape
    return np.ascontiguousarray(w.reshape(K // 128, 128, N).transpose(1, 0, 2))


def rope_tables():
    out = np.zeros((4, 128, TB), np.float32)
    tok = np.arange(SEQ)
    row = (tok // 64).astype(np.float32)
    col = (tok % 64).astype(np.float32)
    for ti, dim in ((0, 32), (2, 64)):
        q = dim // 4
        inv = (10000.0 ** (-np.arange(q, dtype=np.float32) / q)).astype(np.float32)
        ang_r = row[:, None] * inv[None, :]
        ang_c = col[:, None] * inv[None, :]
        _, sign = _rope_perm(dim)
        cosd = np.zeros((dim, SEQ), np.float32)
        sind = np.zeros((dim, SEQ), np.float32)
        for d in range(dim):
            ang = ang_r if d < dim // 2 else ang_c
            cosd[d] = np.cos(ang[:, d % q])
            sind[d] = sign[d] * np.sin(ang[:, d % q])
        reps = 128 // dim
        out[ti, :, :CTX] = 1.0
        out[ti + 1, :, :CTX] = 0.0
        out[ti, :, CTX:] = np.tile(cosd, (reps, 1))
        out[ti + 1, :, CTX:] = np.tile(sind, (reps, 1))
    return out


def w2full(w2, bg):
    out = np.zeros((33, 512), np.float32)
    for d in range(2):
        for h in range(4):
            c0 = d * 256 + h * 64
            out[16 * d:16 * d + 16, c0:c0 + 48] = w2[d][:, 48 * h:48 * h + 48]
            out[32, c0:c0 + 48] = bg[d][48 * h:48 * h + 48]
    return out


NT = TB // 128


def phase_p2a(P, io):
    scale = 32 ** -0.5
    aqt = io["aqt"]
    akt = io["akt"]
    av = io["av"]
    lamb = io["lamb"]
    cst = io["cst"]
    idn = io["idn"]
    mo = io["mo"]
    if True:
        P.begin()
        qT = P.sb("qT", [128, TB], BF16)
        kTm = [P.sb("kTm%d" % j, [128, TB], BF16) for j in range(4)]
        va = P.sb("va", [128, NT, 2, 65], BF16)
        idf = P.sb("idf", [128, 128], F32)
        lb = P.sb("lb", [128, 4, 32], F32)
        cs = P.sb("cs", [128, 66], F32)
        sm = P.sb("sm", [128, 8], F32)
        tmp32 = P.sb("tmp32", [128, 32], F32)
        gfin = P.sb("gfin", [128, 64], F32)
        pt = sb_rot(P, "pt", [128, 1024], BF16, 3)
        oT = sb_rot(P, "oT", [65, 512], F32, 2)
        rec = sb_rot(P, "rec", [128, 2, 4], F32, 2)
        d1 = sb_rot(P, "d1", [128, 64], F32, 2)
        dd = sb_rot(P, "dd", [128, 64], F32, 2)
        jk = sb_rot(P, "jk", [128, 64], F32, 2)
        ssr = sb_rot(P, "ssa", [128, 2], F32, 2)
        mot = sb_rot(P, "mot", [128, 4, 128], F32, 2)
        psb = ps_rot(P, "psD", 2, (128, 1024))
        pob = ps_rot(P, "poT", 2)
        ptr = ps_rot(P, "ptr", 1)
        osb = sb_rot(P, "osb", [128, 4, 65], F32, 4)
        P.load(qT[:], aqt, "qT")
        for j in range(4):
            (P.pool if j % 2 == 0 else P.dve)([], ["kT"], "memset", kTm[j][:], 0.0)
        for j in range(4):
            P.load(kTm[j][32 * j:32 * j + 32, :], akt[32 * j:32 * j + 32, :], "kT")
        P.load(lb[:], lamb, "lb")
        P.load(cs[:], cst, "cs")
        P.load(idf[:], idn, "idf")
        P.pool([], ["va"], "memset", va[:], 1.0)
        for h in range(2):
            P.load(va[:, :, h, 0:64], av[:, h * 64:(h + 1) * 64].rearrange("(n p) d -> p n d", p=128), "va")
        for i in range(2):
            P.dve(["lb"], ["tmp32"], "tensor_tensor", out=tmp32[:], in0=lb[:, 2 * i, :], in1=lb[:, 2 * i + 1, :], op=ALU.mult)
            P.dve(["tmp32"], ["sm"], "reduce_sum", out=sm[:, i:i + 1], in_=tmp32[:], axis=AX.X)
        P.act(["sm"], ["sm"], "activation", out=sm[:, 0:2], in_=sm[:, 0:2], func=AF.Exp)
        P.dve(["sm"], ["sm"], "tensor_tensor", out=sm[:, 2:3], in0=sm[:, 0:1], in1=sm[:, 1:2], op=ALU.subtract)
        P.dve(["sm", "cs"], ["sm"], "tensor_tensor", out=sm[:, 2:3], in0=sm[:, 2:3], in1=cs[:, 64:65], op=ALU.add)
        P.dve(["sm"], ["sm"], "tensor_scalar", out=sm[:, 3:4], in0=sm[:, 2:3], scalar1=-1.0, scalar2=None, op0=ALU.mult)
        P.dve(["cs"], ["gfin"], "tensor_scalar", out=gfin[:], in0=cs[:, 0:64], scalar1=cs[:, 65:66], scalar2=None, op0=ALU.mult)

        groups = [(0, 2, 2)] + [(2 + 4 * g, 4, NT) for g in range(16)]
        steps = []
        for (t0, nq, nk) in groups:
            for hl in range(2):
                for m in range(2):
                    for kp in range(nk // 2):
                        steps.append((t0, nq, nk, hl, m, kp))

        def emit_scores(st_):
            t0, nq, nk, hl, m, kp = st_
            nqc = nq * 128
            j = 2 * hl + m
            ps_, psk = psb.next()
            for u in range(2):
                kt = 2 * kp + u
                P.pe(["kT", "qT"], [psk], "matmul", ps_[:, u * 512:u * 512 + nqc], lhsT=kTm[j][:, kt * 128:(kt + 1) * 128],
                     rhs=qT[:, t0 * 128:t0 * 128 + nqc], start=True, stop=True)
            return ps_, psk

        nxt = emit_scores(steps[0])
        pos = []
        po = pok = mt = mk = None
        for si, st_ in enumerate(steps):
            t0, nq, nk, hl, m, kp = st_
            nqc = nq * 128
            ps_, psk = nxt
            if si + 1 < len(steps):
                nxt = emit_scores(steps[si + 1])
            if kp == 0:
                po, pok = pob.next()
                if hl == 0 and m == 0:
                    mt, mk = mot.next()
                if m == 0:
                    pos = []
            p_, pk_ = pt.next()
            P.act([psk], [pk_], "activation", out=p_[:].rearrange("p (u n) -> p u n", u=2)[:, :, 0:nqc],
                  in_=ps_[:].rearrange("p (u n) -> p u n", u=2)[:, :, 0:nqc], func=AF.Exp, scale=scale)
            for u in range(2):
                kt = 2 * kp + u
                P.pe([pk_, "va"], [pok], "matmul", po[0:65, 0:nqc], lhsT=va[:, kt, hl, :], rhs=p_[:, u * 512:u * 512 + nqc],
                     start=(kt == 0), stop=(kt == nk - 1))
            if kp != nk // 2 - 1:
                continue
            o_, ok_ = oT.next()
            P.act([pok], [ok_], "activation", out=o_[:, 0:nqc], in_=po[0:65, 0:nqc], func=AF.Copy)
            tr, trk = ptr.next()
            for qb in range(nq):
                P.pe([ok_, "idf"], [trk], "transpose", out=tr[:, qb * 65:(qb + 1) * 65], in_=o_[:, qb * 128:(qb + 1) * 128], identity=idf[0:65, 0:65])
            os_, osk = osb.next()
            P.dve([trk], [osk], "tensor_copy", out=os_[:, 0:nq, :], in_=tr[:, 0:nq * 65].rearrange("p (q d) -> p q d", d=65))
            pos.append((os_, osk))
            if m == 0:
                continue
            if True:
                (po1, k1), (po2, k2) = pos
                rc, rck = rec.next()
                P.dve([k1], [rck], "reciprocal", out=rc[:, 0, 0:nq], in_=po1[:, 0:nq, 64])
                P.dve([k2], [rck], "reciprocal", out=rc[:, 1, 0:nq], in_=po2[:, 0:nq, 64])
                P.dve([rck, "sm"], [rck], "tensor_scalar", out=rc[:, 1, 0:nq], in0=rc[:, 1, 0:nq], scalar1=sm[:, 3:4], scalar2=None, op0=ALU.mult)
                for qb in range(nq):
                    a_, ak = d1.next()
                    P.dve([k1, rck], [ak], "tensor_scalar", out=a_[:], in0=po1[:, qb, 0:64], scalar1=rc[:, 0, qb:qb + 1], scalar2=None, op0=ALU.mult)
                    d_, dk = dd.next()
                    P.dve([k2, rck, ak], [dk], "scalar_tensor_tensor", out=d_[:], in0=po2[:, qb, 0:64], scalar=rc[:, 1, qb:qb + 1], in1=a_[:],
                          op0=ALU.mult, op1=ALU.add)
                    j_, jkk = jk.next()
                    s_, sk_ = ssr.next()
                    P.act([dk], [jkk, sk_], "activation", out=j_[:], in_=d_[:], func=AF.Square, accum_out=s_[:, 0:1])
                    P.act([sk_], [sk_], "activation", out=s_[:, 1:2], in_=s_[:, 0:1], func=AF.Ln, scale=1.0 / 64, bias=EPS)
                    P.act([sk_], [sk_], "activation", out=s_[:, 1:2], in_=s_[:, 1:2], func=AF.Exp, scale=-0.5)
                    P.dve([dk, sk_, "gfin"], [mk], "scalar_tensor_tensor", out=mt[:, qb, hl * 64:(hl + 1) * 64], in0=d_[:], scalar=s_[:, 1:2],
                          in1=gfin[:], op0=ALU.mult, op1=ALU.mult)
            if hl == 1:
                P.store(mo[t0 * 128:(t0 + nq) * 128, :].rearrange("(q p) c -> p q c", p=128), mt[:, 0:nq, :], mk)
        P.end()


def phase_p2b(P, io):
    scale = 64 ** -0.5
    bqt = io["bqt"]
    bkt = io["bkt"]
    bv = io["bv"]
    sink = io["sink"]
    masks = io["masks"]
    mo = io["mo"]
    if True:
        P.begin()
        qT = P.sb("qT", [64, 3, TB], BF16)
        kT = P.sb("kT", [64, TB], BF16)
        va = P.sb("va", [128, NT, 65], BF16)
        sk = P.sb("sk", [128, 3], F32)
        mk = P.sb("mk", [128, 2, 3, 128], F32)
        pe_ = sb_rot(P, "pe", [128, 3, 128], BF16, 10)
        den = sb_rot(P, "den", [128, 3], F32, 2)
        mot = sb_rot(P, "mot", [128, 3, 64], F32, 3)
        psb = ps_rot(P, "ps", 3)
        pob = ps_rot(P, "po", 2, (128, 3, 65))
        P.load(qT[:], bqt.rearrange("(h d) t -> d h t", d=64), "qT")
        P.load(kT[:], bkt, "kT")
        P.load(sk[:], sink, "sk")
        P.load(mk[:], masks, "mk")
        P.pool([], ["va"], "memset", va[:], 1.0)
        P.load(va[:, :, 0:64], bv.rearrange("(n p) d -> p n d", p=128), "va")
        P.act(["sk"], ["sk"], "activation", out=sk[:], in_=sk[:], func=AF.Exp)
        for n in range(NT):
            if n < 2:
                kts = [(0, None), (1, None)]
            else:
                kts = [(0, None), (1, None)]
                if n - 1 >= 2:
                    kts.append((n - 1, 0))
                kts.append((n, None))
                if n + 1 < NT:
                    kts.append((n + 1, 1))
            po, pok = pob.next()
            pts = []
            for i, (kt, msk) in enumerate(kts):
                ps_, psk = psb.next()
                P.pe(["kT", "qT"], [psk], "matmul", ps_[:, 0:384].rearrange("p (h q) -> p h q", h=3), lhsT=kT[:, kt * 128:(kt + 1) * 128],
                     rhs=qT[:, :, n * 128:(n + 1) * 128], start=True, stop=True)
                p_, pk_ = pe_.next()
                P.act([psk], [pk_], "activation", out=p_[:], in_=ps_[:, 0:384].rearrange("p (h q) -> p h q", h=3), func=AF.Exp, scale=scale)
                if msk is not None:
                    P.dve([pk_, "mk"], [pk_], "tensor_tensor", out=p_[:], in0=p_[:], in1=mk[:, msk, :, :], op=ALU.mult)
                pts.append((p_, pk_, kt))
            for h in range(3):
                for i, (p_, pk_, kt) in enumerate(pts):
                    P.pe([pk_, "va"], [pok], "matmul", po[:, h, :], lhsT=p_[:, h, :], rhs=va[:, kt, :], start=(i == 0), stop=(i == len(pts) - 1))
            dn, dnk = den.next()
            P.dve([pok, "sk"], [dnk], "tensor_tensor", out=dn[:], in0=po[:, :, 64], in1=sk[:], op=ALU.add)
            P.dve([dnk], [dnk], "reciprocal", out=dn[:], in_=dn[:])
            mt, mtk = mot.next()
            for h in range(3):
                P.dve([pok, dnk], [mtk], "tensor_scalar", out=mt[:, h, :], in0=po[:, h, 0:64], scalar1=dn[:, h:h + 1], scalar2=None, op0=ALU.mult)
            P.store(mo[n * 128:(n + 1) * 128, :], mt[:].rearrange("p h d -> p (h d)"), mtk)
        P.end()


def band_masks():
    j = np.arange(128)[:, None]
    i = np.arange(128)[None, :]
    m = np.zeros((128, 2, 3, 128), np.float32)
    m[:, 0, :, :] = (i <= j).astype(np.float32)[:, None, :]
    m[:, 1, :, :] = (j <= i).astype(np.float32)[:, None, :]
    return m


NCH = TB // 64


def gla_consts():
    s_ = np.arange(64)[:, None]
    t_ = np.arange(64)[None, :]
    tri = np.zeros((64, 2, 65), np.float32)
    trix = np.zeros((64, 2, 64), np.float32)
    mask = np.zeros((64, 2, 2, 64), np.float32)
    c = -1.0 / 16.0
    tri[:, 0, :64] = c * (s_ <= t_)
    tri[:, 1, :64] = c * (s_ >= t_)
    tri[:, :, 64] = c
    trix[:, 0, :] = c * (s_ > t_)
    trix[:, 1, :] = c * (s_ < t_)
    mask[:, 0, :, :] = (s_ <= t_).astype(np.float32)[:, None, :]
    mask[:, 1, :, :] = (s_ >= t_).astype(np.float32)[:, None, :]
    return tri, trix, mask


def phase_p2c(P, io):
    cqt = io["cqt"]
    ckt = io["ckt"]
    cktok = io["cktok"]
    cv = io["cv"]
    crs = io["crs"]
    sp = io["sp"]
    tri_d = io["tri_d"]
    trix_d = io["trix_d"]
    mask_d = io["mask_d"]
    gc_d = io["gc_d"]
    mo = io["mo"]
    if True:
        P.begin()
        qT = P.sb("qT", [64, 2, TB], BF16)
        kT = P.sb("kT", [64, 2, TB], BF16)
        OF = P.sb("OF", [64, NCH, 192], F32)
        tri = P.sb("tri_s", [64, 2, 65], F32)
        trix = P.sb("trix_s", [64, 2, 64], F32)
        mask = P.sb("mask_s", [64, 2, 2, 64], F32)
        gc = P.sb("gc_s", [64, 192], F32)
        S = [P.sb("S%d" % d, [64, 2, 96], F32) for d in range(2)]
        Sb = [P.sb("Sb%d" % d, [64, 2, 96], BF16) for d in range(2)]
        spr = sb_rot(P, "spc", [64, 2, 128], F32, 6)
        ktr = sb_rot(P, "ktk", [64, 128], BF16, 6)
        vr = sb_rot(P, "vv", [64, 192], BF16, 6)
        rr = sb_rot(P, "rs", [64, 192], F32, 4)
        E1 = sb_rot(P, "E1", [64, 2, 65], F32, 4)
        E2 = sb_rot(P, "E2", [64, 2, 64], F32, 4)
        E3 = sb_rot(P, "E3", [64, 128], F32, 4)
        qd = sb_rot(P, "qd", [64, 2, 64], BF16, 4)
        ki = sb_rot(P, "ki", [64, 2, 64], BF16, 4)
        ke = sb_rot(P, "ke", [64, 128], BF16, 4)
        att = sb_rot(P, "att", [64, 2, 64], BF16, 4)
        osum = sb_rot(P, "osum", [64, 192], F32, 2)
        jk = sb_rot(P, "jk", [64, 96], F32, 2)
        ssr = sb_rot(P, "ssc", [64, 4], F32, 2)
        yo = sb_rot(P, "yo", [64, 192], F32, 3)
        pb = ps_rot(P, "pb", 2)
        pbd = ps_rot(P, "pbd", 1)
        patt = ps_rot(P, "patt", 2)
        po = ps_rot(P, "po", 2)
        pu = ps_rot(P, "pu", 1)
        P.load(qT[:], cqt.rearrange("(h d) t -> d h t", d=64), "qT")
        P.load(kT[:], ckt.rearrange("(h d) t -> d h t", d=64), "kT")
        P.load(tri[:], tri_d, "tri")
        P.load(trix[:], trix_d, "trix")
        P.load(mask[:], mask_d, "mask")
        P.load(gc[:], gc_d, "gc")
        for d in range(2):
            P.pool([], ["S%d" % d], "memset", S[d][:], 0.0)
            P.pool([], ["Sb%d" % d], "memset", Sb[d][:], 0.0)
        fwd = [(c, 0) for c in range(NCH)]
        bwd = [(c, 1) for c in (3, 2, 1, 0)] + [(c, 1) for c in range(NCH - 1, 3, -1)]
        order = [x for pair in zip(fwd, bwd) for x in pair]
        seen_c = set()
        for (c, d) in order:
            second = c in seen_c
            seen_c.add(c)
            tk = slice(c * 64, (c + 1) * 64)
            sp_, spk = spr.next()
            P.load(sp_[:], sp[tk, :, :], spk)
            kt_, ktk = ktr.next()
            P.load(kt_[:], cktok[tk, :], ktk)
            v_, vk = vr.next()
            P.load(v_[:], cv[tk, :], vk)
            if second:
                r_, rk = rr.next()
                P.load(r_[:], crs[tk, :], rk)
            pb_, pbk = pb.next()
            for h in range(2):
                P.pe([spk, "tri"], [pbk], "matmul", pb_[0:64, h * 65:(h + 1) * 65], lhsT=sp_[:, d, 64 * h:64 * h + 64], rhs=tri[:, d, :], start=True, stop=True)
            pbd_, pbdk = pbd.next()
            P.pe([spk, "trix"], [pbdk], "matmul", pbd_[0:64, 0:128], lhsT=trix[:, d, :], rhs=sp_[:, d, :], start=True, stop=True)
            e1, e1k = E1.next()
            e2, e2k = E2.next()
            e3, e3k = E3.next()
            pbv = pb_[0:64, 0:130].rearrange("p (h n) -> p h n", h=2)
            P.act([pbk], [e1k], "activation", out=e1[:], in_=pbv, func=AF.Exp)
            P.act([pbk], [e2k], "activation", out=e2[:], in_=pbv[:, :, 0:64], func=AF.Exp, scale=-1.0)
            P.act([pbdk], [e3k], "activation", out=e3[:], in_=pbd_[0:64, 0:128], func=AF.Exp)
            qd_, qdk = qd.next()
            ki_, kik = ki.next()
            ke_, kek = ke.next()
            P.dve(["qT", e1k], [qdk], "tensor_tensor", out=qd_[:], in0=qT[:, :, tk], in1=e1[:, :, 0:64], op=ALU.mult)
            P.dve(["kT", e2k], [kik], "tensor_tensor", out=ki_[:], in0=kT[:, :, tk], in1=e2[:], op=ALU.mult)
            P.pool([ktk, e3k], [kek], "tensor_tensor", out=ke_[:], in0=kt_[:], in1=e3[:], op=ALU.mult)
            pa_, pak = patt.next()
            pav = pa_[0:64, 0:128].rearrange("p (h n) -> p h n", h=2)
            for h in range(2):
                P.pe([kik, qdk], [pak], "matmul", pa_[0:64, h * 64:(h + 1) * 64], lhsT=ki_[:, h, :], rhs=qd_[:, h, :], start=True, stop=True)
            at_, atk = att.next()
            P.dve([pak, "mask"], [atk], "tensor_tensor", out=at_[:], in0=pav, in1=mask[:, d, :, :], op=ALU.mult)
            po_, pok = po.next()
            for h in range(2):
                P.pe([atk, vk], [pok], "matmul", po_[0:64, 96 * h:96 * h + 96], lhsT=at_[:, h, :], rhs=v_[:, 96 * h:96 * h + 96], start=True, stop=False)
                P.pe([qdk, "Sb%d" % d], [pok], "matmul", po_[0:64, 96 * h:96 * h + 96], lhsT=qd_[:, h, :], rhs=Sb[d][:, h, :], start=False, stop=True)
            pu_, puk = pu.next()
            for h in range(2):
                P.pe([kek, vk], [puk], "matmul", pu_[0:64, 96 * h:96 * h + 96], lhsT=ke_[:, 64 * h:64 * h + 64], rhs=v_[:, 96 * h:96 * h + 96], start=True, stop=True)
            for h in range(2):
                P.dve(["S%d" % d, e1k, puk], ["S%d" % d], "scalar_tensor_tensor", out=S[d][:, h, :], in0=S[d][:, h, :], scalar=e1[:, h, 64:65],
                      in1=pu_[0:64, 96 * h:96 * h + 96], op0=ALU.mult, op1=ALU.add)
            P.pool(["S%d" % d], ["Sb%d" % d], "tensor_copy", out=Sb[d][:], in_=S[d][:])
            if not second:
                P.act([pok], ["OF%d" % c], "activation", out=OF[:, c, :], in_=po_[0:64, 0:192], func=AF.Copy)
            else:
                os_, osk = osum.next()
                P.dve([pok, "OF%d" % c], [osk], "tensor_tensor", out=os_[:], in0=po_[0:64, 0:192], in1=OF[:, c, :], op=ALU.add)
                s_, sk_ = ssr.next()
                for h in range(2):
                    j_, jkk = jk.next()
                    P.act([osk], [jkk, sk_], "activation", out=j_[:], in_=os_[:, 96 * h:96 * h + 96], func=AF.Square, accum_out=s_[:, h:h + 1])
                P.act([sk_], [sk_], "activation", out=s_[:, 2:4], in_=s_[:, 0:2], func=AF.Ln, scale=1.0 / 96, bias=EPS)
                P.act([sk_], [sk_], "activation", out=s_[:, 2:4], in_=s_[:, 2:4], func=AF.Exp, scale=-0.5)
                y_, yk = yo.next()
                for h in range(2):
                    P.dve([osk, sk_, "gc"], [yk], "scalar_tensor_tensor", out=y_[:, 96 * h:96 * h + 96], in0=os_[:, 96 * h:96 * h + 96],
                          scalar=s_[:, 2 + h:3 + h], in1=gc[:, 96 * h:96 * h + 96], op0=ALU.mult, op1=ALU.mult)
                P.pool([yk, rk], [yk], "tensor_tensor", out=y_[:], in0=y_[:], in1=r_[:], op=ALU.mult)
                P.store(mo[tk, :], y_[:], yk)
        P.end()


def phase_p3(P, io, E, FF, moe):
    FC = FF // 128
    NG = FF // 256
    xs = io["xs"]
    mo = io["mo"]
    modrow = io["modrow"]
    wout = io["wout"]
    router = io["router"]
    wg = io["wg"]
    wu = io["wu"]
    wd = io["wd"]
    idn = io["idn"]
    xo = io["xo"]
    wgd, wud, wdd = io["wgd"], io["wud"], io["wdd"]
    if True:
        P.begin()
        idf = P.sb("idf", [128, 128], F32)
        woutb = P.sb("woutb", [128, 8, D], BF16)
        rts = P.sb("rts", [128, 8, 8], F32)
        G1 = [P.sb("G1_%d" % i, [128, D], F32) for i in range(2)]
        A2 = [P.sb("A2_%d" % i, [128, D], F32) for i in range(2)]
        B2 = [P.sb("B2_%d" % i, [128, D], F32) for i in range(2)]
        G2 = [P.sb("G2_%d" % i, [128, D], F32) for i in range(2)]
        stage = sb_rot(P, "stage", [128, 2048], F32, 3)
        xrot = sb_rot(P, "xt", [128, D], F32, 2)
        mrot = sb_rot(P, "mt", [128, D], F32, 2)
        xnew = sb_rot(P, "xn", [128, D], F32, 3)
        yacc = sb_rot(P, "ya", [128, D], F32, 3)
        hrot = sb_rot(P, "hx", [128, D], F32, 2)
        ssr = sb_rot(P, "ss", [128, 2], F32, 2)
        catT = sb_rot(P, "catT", [128, 8, 128], BF16, 2)
        h2T = sb_rot(P, "h2T", [128, 8, ST], BF16, 2)
        h2Tf = P.sb("h2Tf", [128, 8, ST], F32) if moe else None
        cw = sb_rot(P, "cw", [128, 3, 8], F32, 2)
        lg = sb_rot(P, "lg", [128, 8], F32, 2)
        rt = sb_rot(P, "rtmp", [128, 4, 8], F32, 2)
        rs_ = sb_rot(P, "rsc", [128, 4], F32, 2)
        wgb = sb_rot(P, "wgb", [128, 8, 256], BF16, 2)
        wub = sb_rot(P, "wub", [128, 8, 256], BF16, 2)
        wdb = sb_rot(P, "wdb", [128, 2, D], BF16, 3)
        sgr = sb_rot(P, "sg", [128, ST], F32, 2)
        aTr = sb_rot(P, "aT", [128, ST], BF16, 3)
        bank = [P.ps("bk%d" % i, [128, 512], F32) for i in range(8)]
        bk = ["bk%d" % i for i in range(8)]
        P.load(idf[:], idn, "idf")
        if moe:
            P.load(rts[:], router, "rts")
        for v in range(2):
            P.load(G1[v][:], modrow[v:v + 1, 2, :].partition_broadcast(128), "G1_%d" % v)
            P.load(B2[v][:], modrow[v:v + 1, 3, :].partition_broadcast(128), "B2_%d" % v)
            P.load(A2[v][:], modrow[v:v + 1, 4, :].partition_broadcast(128), "A2_%d" % v)
            P.load(G2[v][:], modrow[v:v + 1, 5, :].partition_broadcast(128), "G2_%d" % v)
        for c in range(8):
            sg, sk = stage.next()
            P.load(sg[:, 0:D], wout[:, c, :], sk)
            (P.dve if c % 2 == 0 else P.pool)([sk], ["woutb"], "tensor_copy", out=woutb[:, c, :], in_=sg[:, 0:D])
        ccast = 0
        for e in range(E):
            for gi in range(NG):
                for (src_, dst_, rot_, shp) in ((wg[e, :, :, gi * 256:(gi + 1) * 256], wgd[e, :, :, gi * 256:(gi + 1) * 256], wgb, 8),
                                                (wu[e, :, :, gi * 256:(gi + 1) * 256], wud[e, :, :, gi * 256:(gi + 1) * 256], wub, 8),
                                                (wd[e, :, 2 * gi:2 * gi + 2, :], wdd[e, :, 2 * gi:2 * gi + 2, :], wdb, 2)):
                    sg, sk = stage.next()
                    P.load(sg[:].rearrange("p (c n) -> p c n", c=shp), src_, sk)
                    wb_, wbk = rot_.next()
                    if ccast % 3 == 0:
                        P.dve([sk], [wbk], "tensor_copy", out=wb_[:], in_=sg[:].rearrange("p (c n) -> p c n", c=shp))
                    elif ccast % 3 == 1:
                        P.pool([sk], [wbk], "tensor_copy", out=wb_[:], in_=sg[:].rearrange("p (c n) -> p c n", c=shp))
                    else:
                        P.act([sk], [wbk], "activation", out=wb_[:], in_=sg[:].rearrange("p (c n) -> p c n", c=shp), func=AF.Copy)
                    ccast += 1
                    P.store(dst_, wb_[:], wbk)
        for eng in ENGS:
            P.finish(eng)
        for s in range(NST):
            h2, h2k = h2T.next()
            cw_, cwk = cw.next()
            xns = []
            yas = []
            for t in range(3):
                g = 3 * s + t
                v = 1 if g < 2 else 0
                mt, mtk = mrot.next()
                P.load(mt[:], mo[g * 128:(g + 1) * 128, :], mtk)
                xt_, xk = xrot.next()
                P.load(xt_[:], xs[g * 128:(g + 1) * 128, :], xk)
                ct, ctk = catT.next()
                for hf in range(2):
                    for c in range(4):
                        P.pe([mtk, "idf"], [bk[6 + hf]], "transpose", out=bank[6 + hf][:, c * 128:(c + 1) * 128],
                             in_=mt[:, (4 * hf + c) * 128:(4 * hf + c + 1) * 128], identity=idf[:])
                    (P.act if hf == 0 else P.dve)([bk[6 + hf]], [ctk], *(("activation",) if hf == 0 else ("tensor_copy",)),
                                                  **(dict(out=ct[:, 4 * hf:4 * hf + 4, :], in_=bank[6 + hf][:].rearrange("p (c n) -> p c n", c=4), func=AF.Copy)
                                                     if hf == 0 else dict(out=ct[:, 4 * hf:4 * hf + 4, :], in_=bank[6 + hf][:].rearrange("p (c n) -> p c n", c=4))))
                xn_, xnk = xnew.next()
                for hf in range(2):
                    for c in range(8):
                        P.pe([ctk, "woutb"], [bk[1 + hf]], "matmul", bank[1 + hf][:, :], lhsT=ct[:, c, :], rhs=woutb[:, c, hf * 512:(hf + 1) * 512],
                             start=(c == 0), stop=(c == 7))
                    P.dve([bk[1 + hf], "G1_%d" % v], [xnk], "tensor_tensor", out=xn_[:, hf * 512:(hf + 1) * 512], in0=bank[1 + hf][:, :],
                          in1=G1[v][:, hf * 512:(hf + 1) * 512], op=ALU.mult)
                P.pool([xnk, xk], [xnk], "tensor_tensor", out=xn_[:], in0=xn_[:], in1=xt_[:], op=ALU.add)
                xns.append((xn_, xnk, v))
                ss_, sk_ = ssr.next()
                hx_, hxk = hrot.next()
                P.act([xnk], [hxk, sk_], "activation", out=hx_[:], in_=xn_[:], func=AF.Square, accum_out=ss_[:, 0:1])
                P.act([sk_], [sk_], "activation", out=ss_[:, 1:2], in_=ss_[:, 0:1], func=AF.Ln, scale=1.0 / D, bias=EPS)
                P.act([sk_], [sk_], "activation", out=ss_[:, 1:2], in_=ss_[:, 1:2], func=AF.Exp, scale=-0.5)
                P.dve([xnk, sk_, "A2_%d" % v], [hxk], "scalar_tensor_tensor", out=hx_[:], in0=xn_[:], scalar=ss_[:, 1:2], in1=A2[v][:],
                      op0=ALU.mult, op1=ALU.mult)
                P.pool([hxk, "B2_%d" % v], [hxk], "tensor_tensor", out=hx_[:], in0=hx_[:], in1=B2[v][:], op=ALU.add)
                for hf in range(2):
                    for c in range(4):
                        P.pe([hxk, "idf"], [bk[6 + hf]], "transpose", out=bank[6 + hf][:, c * 128:(c + 1) * 128],
                             in_=hx_[:, (4 * hf + c) * 128:(4 * hf + c + 1) * 128], identity=idf[:])
                    src = bank[6 + hf][:].rearrange("p (c n) -> p c n", c=4)
                    if moe:
                        P.dve([bk[6 + hf]], ["h2Tf"], "tensor_copy", out=h2Tf[:, 4 * hf:4 * hf + 4, t * 128:(t + 1) * 128], in_=src)
                        P.pool(["h2Tf"], [h2k], "tensor_copy", out=h2[:, 4 * hf:4 * hf + 4, t * 128:(t + 1) * 128],
                               in_=h2Tf[:, 4 * hf:4 * hf + 4, t * 128:(t + 1) * 128])
                    else:
                        P.act([bk[6 + hf]], [h2k], "activation", out=h2[:, 4 * hf:4 * hf + 4, t * 128:(t + 1) * 128], in_=src, func=AF.Copy)
                if moe:
                    for c in range(8):
                        P.pe(["h2Tf", "rts"], [bk[3]], "matmul", bank[3][:, 0:8], lhsT=h2Tf[:, c, t * 128:(t + 1) * 128], rhs=rts[:, c, :],
                             start=(c == 0), stop=(c == 7))
                    l_, lk = lg.next()
                    r_, rk = rt.next()
                    q_, qk = rs_.next()
                    P.dve([bk[3]], [lk], "tensor_copy", out=l_[:], in_=bank[3][:, 0:8])
                    P.dve([lk], [qk], "reduce_max", out=q_[:, 0:1], in_=l_[:], axis=AX.X)
                    P.dve([lk, qk], [rk], "tensor_scalar", out=r_[:, 0, :], in0=l_[:], scalar1=q_[:, 0:1], scalar2=None, op0=ALU.is_equal)
                    P.dve([rk, lk], [rk], "scalar_tensor_tensor", out=r_[:, 1, :], in0=r_[:, 0, :], scalar=-1e30, in1=l_[:], op0=ALU.mult, op1=ALU.add)
                    P.dve([rk], [qk], "reduce_max", out=q_[:, 1:2], in_=r_[:, 1, :], axis=AX.X)
                    P.dve([lk, qk], [rk], "tensor_scalar", out=r_[:, 2, :], in0=l_[:], scalar1=q_[:, 1:2], scalar2=None, op0=ALU.is_ge)
                    P.dve([qk], [qk], "tensor_scalar", out=q_[:, 2:3], in0=q_[:, 0:1], scalar1=-1.0, scalar2=None, op0=ALU.mult)
                    P.act([lk, qk], [rk], "activation", out=r_[:, 3, :], in_=l_[:], func=AF.Exp, bias=q_[:, 2:3])
                    P.dve([rk], [rk], "tensor_tensor", out=r_[:, 3, :], in0=r_[:, 3, :], in1=r_[:, 2, :], op=ALU.mult)
                    P.dve([rk], [qk], "reduce_sum", out=q_[:, 3:4], in_=r_[:, 3, :], axis=AX.X)
                    P.dve([qk], [qk], "reciprocal", out=q_[:, 3:4], in_=q_[:, 3:4])
                    P.dve([rk, qk], [cwk], "tensor_scalar", out=cw_[:, t, :], in0=r_[:, 3, :], scalar1=q_[:, 3:4], scalar2=None, op0=ALU.mult)
            for t in range(3):
                ya_, yak = yacc.next()
                yas.append((ya_, yak))
            pending = None
            for e in range(E):
                for gi in range(NG):
                    tiles = []
                    for (src_, rot_) in ((wgd[e, :, :, gi * 256:(gi + 1) * 256], wgb), (wud[e, :, :, gi * 256:(gi + 1) * 256], wub),
                                         (wdd[e, :, 2 * gi:2 * gi + 2, :], wdb)):
                        wb_, wbk = rot_.next()
                        P.load(wb_[:], src_, wbk)
                        tiles.append((wb_, wbk))
                    (wg_, wgk), (wu_, wuk), (wd_, wdk) = tiles
                    for j in range(2):
                        for c in range(8):
                            P.pe([wgk, h2k], [bk[6]], "matmul", bank[6][:, 0:ST], lhsT=wg_[:, c, j * 128:(j + 1) * 128], rhs=h2[:, c, :],
                                 start=(c == 0), stop=(c == 7))
                        for c in range(8):
                            P.pe([wuk, h2k], [bk[7]], "matmul", bank[7][:, 0:ST], lhsT=wu_[:, c, j * 128:(j + 1) * 128], rhs=h2[:, c, :],
                                 start=(c == 0), stop=(c == 7))
                        sg_, sgk = sgr.next()
                        P.act([bk[6]], [sgk], "activation", out=sg_[:], in_=bank[6][:, 0:ST], func=AF.Silu)
                        a_, ak = aTr.next()
                        P.dve([sgk, bk[7]], [ak], "tensor_tensor", out=a_[:], in0=sg_[:], in1=bank[7][:, 0:ST], op=ALU.mult)
                        first = (gi == 0 and j == 0)
                        last = (gi == NG - 1 and j == 1)
                        if pending is not None:
                            pending()

                        def down(a_=a_, ak=ak, wd_=wd_, wdk=wdk, j=j, first=first, last=last):
                            for t in range(3):
                                for hf in range(2):
                                    b_ = 2 * t + hf
                                    P.pe([ak, wdk], [bk[b_]], "matmul", bank[b_][:, :], lhsT=a_[:, t * 128:(t + 1) * 128],
                                         rhs=wd_[:, j, hf * 512:(hf + 1) * 512], start=first, stop=last)
                        pending = down
                pending()
                pending = None
                for t in range(3):
                    ya_, yak = yas[t]
                    for hf in range(2):
                        b_ = 2 * t + hf
                        osl = ya_[:, hf * 512:(hf + 1) * 512]
                        if not moe:
                            P.act([bk[b_]], [yak], "activation", out=osl, in_=bank[b_][:, :], func=AF.Copy)
                        elif e == 0:
                            P.dve([bk[b_], cwk], [yak], "tensor_scalar", out=osl, in0=bank[b_][:, :], scalar1=cw_[:, t, e:e + 1], scalar2=None, op0=ALU.mult)
                        else:
                            P.dve([bk[b_], cwk, yak], [yak], "scalar_tensor_tensor", out=osl, in0=bank[b_][:, :], scalar=cw_[:, t, e:e + 1], in1=osl,
                                  op0=ALU.mult, op1=ALU.add)
            for t in range(3):
                g = 3 * s + t
                ya_, yak = yas[t]
                xn_, xnk, v = xns[t]
                P.dve([yak, "G2_%d" % v], [yak], "tensor_tensor", out=ya_[:], in0=ya_[:], in1=G2[v][:], op=ALU.mult)
                P.pool([yak, xnk], [yak], "tensor_tensor", out=ya_[:], in0=ya_[:], in1=xn_[:], op=ALU.add)
                P.store(xo[g * 128:(g + 1) * 128, :], ya_[:], yak)
        P.end()


def phase_p4(P, io):
    xs = io["xs"]
    fg = io["fg"]
    xo = io["xo"]
    if True:
        P.begin()
        g_ = P.sb("g_", [128, D], F32)
        xrot = sb_rot(P, "xt", [128, D], F32, 3)
        orot = sb_rot(P, "ot", [128, D], F32, 3)
        ssr = sb_rot(P, "ss", [128, 2], F32, 3)
        P.load(g_[:], fg[0:1, :].partition_broadcast(128), "g_")
        for g in range(2, TC // 128):
            xt_, xk = xrot.next()
            P.load(xt_[:], xs[g * 128:(g + 1) * 128, :], xk)
            o_, ok = orot.next()
            ss_, sk_ = ssr.next()
            P.act([xk], [ok, sk_], "activation", out=o_[:], in_=xt_[:], func=AF.Square, accum_out=ss_[:, 0:1])
            P.act([sk_], [sk_], "activation", out=ss_[:, 1:2], in_=ss_[:, 0:1], func=AF.Ln, scale=1.0 / D, bias=EPS)
            P.act([sk_], [sk_], "activation", out=ss_[:, 1:2], in_=ss_[:, 1:2], func=AF.Exp, scale=-0.5)
            P.dve([xk, sk_, "g_"], [ok], "scalar_tensor_tensor", out=o_[:], in0=xt_[:], scalar=ss_[:, 1:2], in1=g_[:], op0=ALU.mult, op1=ALU.mult)
            P.store(xo[(g - 2) * 128:(g - 1) * 128, :], o_[:], ok)
        P.end()


def build_fused(depth=DEPTH):
    nc = bass.Bass("TRN2", target_bir_lowering=False)

    def din(name, shape, dt=F32):
        return nc.dram_tensor(name, list(shape), dt, kind="ExternalInput").ap()

    def scratch(name, shape, dt=F32):
        return nc.dram_tensor(name, list(shape), dt).ap()

    xs = din("xs", [TB, D])
    cT = din("cT", [128, 8, 2])
    tabs = din("tabs", [128, 4, TB])
    idn = din("idn", [128, 128])
    ada_w = din("ada_w", [DEPTH, 128, 8, 6 * D])
    ada_b2 = din("ada_b2", [DEPTH, 2, 6 * D])
    ng = din("ng", [DEPTH, 2, 2, D])
    w1 = din("w1", [DEPTH, 128, 8, NW1])
    w2f = din("w2f", [DEPTH, 33, 512])
    lamb = din("lamb", [DEPTH, 128, 4, 32])
    cst = din("cst", [DEPTH, 128, 66])
    sink = din("sink", [DEPTH, 2, 128, 3])
    masks = din("masks", [128, 2, 3, 128])
    tri = din("tri", [64, 2, 65])
    trix = din("trix", [64, 2, 64])
    gmask = din("gmask", [64, 2, 2, 64])
    gc = din("gc", [DEPTH, 64, 192])
    wout = din("wout", [DEPTH, 128, 8, D])
    ffg = din("ffg", [2, 1, 128, 8, D_FF])
    ffu = din("ffu", [2, 1, 128, 8, D_FF])
    ffd = din("ffd", [2, 1, 128, D_FF // 128, D])
    mog = din("mog", [2, NEXP, 128, 8, D_FFE])
    mou = din("mou", [2, NEXP, 128, 8, D_FFE])
    mod_ = din("mod_", [2, NEXP, 128, D_FFE // 128, D])
    router = din("router", [2, 128, 8, 8])
    fg = din("fg", [1, D])
    out = nc.dram_tensor("out", [SEQ, D], F32, kind="ExternalOutput").ap()
    X = [scratch("X%d" % i, [TB, D]) for i in range(2)]
    MODROW = scratch("MODROW", [2, 6, D])
    FEAT = scratch("FEAT", [FEAT_ROWS, TB], BF16)
    TOKB = scratch("TOKB", [TB, 1024], BF16)
    TOKF = scratch("TOKF", [TB, 896])
    MO = scratch("MO", [TB, D])
    WGD = scratch("WGD", [NEXP, 128, 8, D_FFE], BF16)
    WUD = scratch("WUD", [NEXP, 128, 8, D_FFE], BF16)
    WDD = scratch("WDD", [NEXP, 128, D_FFE // 128, D], BF16)
    FGD = scratch("FGD", [1, 128, 8, D_FF], BF16)
    FUD = scratch("FUD", [1, 128, 8, D_FF], BF16)
    FDD = scratch("FDD", [1, 128, D_FF // 128, D], BF16)
    with ExitStack() as st:
        P = Prog(nc, st)
        xin = xs
        for L in range(depth):
            xout = X[L % 2]
            phase_p1(P, dict(xs=xin, cT=cT, ada_w=ada_w[L], ada_b2=ada_b2[L], ng=ng[L], w1=w1[L], w2f=w2f[L], tabs=tabs, idn=idn,
                             modrow=MODROW, feat=FEAT, tokb=TOKB, tokf=TOKF))
            for hh in range(2):
                phase_p2a(P, dict(aqt=FEAT[128 * hh:128 * hh + 128, :], akt=FEAT[256 + 128 * hh:256 + 128 * hh + 128, :],
                                  av=TOKB[:, 128 * hh:128 * hh + 128], lamb=lamb[L], cst=cst[L], idn=idn, mo=MO[:, 128 * hh:128 * hh + 128]))
                phase_p2b(P, dict(bqt=FEAT[512 + 192 * hh:512 + 192 * hh + 192, :], bkt=FEAT[896 + 64 * hh:896 + 64 * hh + 64, :],
                                  bv=TOKB[:, 256 + 64 * hh:256 + 64 * hh + 64], sink=sink[L, hh], masks=masks,
                                  mo=MO[:, 256 + 192 * hh:256 + 192 * hh + 192]))
                phase_p2c(P, dict(cqt=FEAT[1024 + 128 * hh:1024 + 128 * hh + 128, :], ckt=FEAT[1280 + 128 * hh:1280 + 128 * hh + 128, :],
                                  cktok=TOKB[:, 768 + 128 * hh:768 + 128 * hh + 128], cv=TOKB[:, 384 + 192 * hh:384 + 192 * hh + 192],
                                  crs=TOKF[:, 192 * hh:192 * hh + 192],
                                  sp=TOKF[:, 384:896].rearrange("t (d c) -> t d c", d=2)[:, :, 128 * hh:128 * hh + 128],
                                  tri_d=tri, trix_d=trix, mask_d=gmask, gc_d=gc[L], mo=MO[:, 640 + 192 * hh:640 + 192 * hh + 192]))
            j = L // 2
            if L % 2 == 0:
                phase_p3(P, dict(xs=xin, mo=MO, modrow=MODROW, wout=wout[L], router=router[0], wg=ffg[j], wu=ffu[j], wd=ffd[j],
                                 idn=idn, xo=xout, wgd=FGD, wud=FUD, wdd=FDD), 1, D_FF, False)
            else:
                phase_p3(P, dict(xs=xin, mo=MO, modrow=MODROW, wout=wout[L], router=router[j], wg=mog[j], wu=mou[j], wd=mod_[j],
                                 idn=idn, xo=xout, wgd=WGD, wud=WUD, wdd=WDD), NEXP, D_FFE, True)
            xin = xout
        phase_p4(P, dict(xs=xin, fg=fg, xo=out))
    return nc


_PROG = []


def _c(a):
    return np.ascontiguousarray(a)


def kernel(x, c, ctx, c_ctx, norm1_g, norm2_g, ada_w, ada_b, w_in, w_out, a_lambda, a_norm_g,
           b_sink, c_gate_w2, c_gate_b, c_norm_g, ffn_w_gate, ffn_w_up, ffn_w_down,
           moe_router, moe_w_gate, moe_w_up, moe_w_down, final_g):
    f = lambda a: np.asarray(a, np.float32)
    x, c, ctx, c_ctx = f(x), f(c), f(ctx), f(c_ctx)
    if not _PROG:
        _PROG.append(build_fused())
    nc = _PROG[0]
    cols = w1_columns()
    tabs = _c(rope_tables().transpose(1, 0, 2))
    tri, trix, gmask = gla_consts()
    shared = {
        "tabs": tabs, "idn": np.eye(128, dtype=np.float32),
        "ada_w": np.stack([kmajor(f(ada_w[L])) for L in range(DEPTH)]),
        "ada_b2": np.stack([np.stack([f(ada_b[L])] * 2) for L in range(DEPTH)]),
        "ng": np.stack([np.stack([np.stack([f(norm1_g[L]), f(norm2_g[L])])] * 2) for L in range(DEPTH)]),
        "w1": np.stack([kmajor(take_cols(f(w_in[L]), cols)) for L in range(DEPTH)]),
        "w2f": np.stack([w2full(f(c_gate_w2[L]), f(c_gate_b[L])) for L in range(DEPTH)]),
        "lamb": _c(np.broadcast_to(f(a_lambda)[:, None], (DEPTH, 128, 4, 32))),
        "masks": band_masks(), "tri": tri, "trix": trix, "gmask": gmask,
        "gc": _c(np.broadcast_to(np.tile(f(c_norm_g), (1, 2))[:, None, :], (DEPTH, 64, 192))),
        "wout": np.stack([kmajor(f(w_out[L])) for L in range(DEPTH)]),
        "ffg": np.stack([kmajor(f(ffn_w_gate[j]))[None] for j in range(2)]),
        "ffu": np.stack([kmajor(f(ffn_w_up[j]))[None] for j in range(2)]),
        "ffd": np.stack([kmajor(f(ffn_w_down[j]))[None] for j in range(2)]),
        "mog": np.stack([np.stack([kmajor(f(moe_w_gate[j][e])) for e in range(NEXP)]) for j in range(2)]),
        "mou": np.stack([np.stack([kmajor(f(moe_w_up[j][e])) for e in range(NEXP)]) for j in range(2)]),
        "mod_": np.stack([np.stack([kmajor(f(moe_w_down[j][e])) for e in range(NEXP)]) for j in range(2)]),
        "router": np.stack([kmajor(f(moe_router[j])) for j in range(2)]),
        "fg": _c(f(final_g)[None, :]),
    }
    cst = np.zeros((DEPTH, 128, 66), np.float32)
    for L in range(DEPTH):
        lam_init = 0.8 - 0.6 * math.exp(-0.3 * L)
        cst[L, :, :64] = f(a_norm_g[L])[None, :]
        cst[L, :, 64] = lam_init
        cst[L, :, 65] = 1.0 - lam_init
    shared["cst"] = cst
    shared["sink"] = _c(np.broadcast_to(f(b_sink).reshape(DEPTH, 2, 1, 3), (DEPTH, 2, 128, 3)))
    in_maps = []
    for i in range(NCORE):
        b = i // 2
        m = dict(shared)
        m["xs"] = _c(np.concatenate([ctx[b], x[b]], 0))
        m["cT"] = _c(np.stack([c[b].reshape(8, 128).T, c_ctx.reshape(8, 128).T], -1))
        in_maps.append(m)
    res = run_bass_kernel_spmd(nc, in_maps, core_ids=list(range(NCORE)))
    out = np.stack([np.asarray(res.results[2 * b]["out"]) for b in range(BATCH)], 0)
    return np.ascontiguousarray(out.astype(np.float32))
```

```python
import math
from contextlib import ExitStack

import ml_dtypes
import numpy as np

import concourse.bass as bass
import concourse.mybir as mybir
from concourse.bass_utils import run_bass_kernel_spmd

F32 = mybir.dt.float32
BF16 = mybir.dt.bfloat16
AF = mybir.ActivationFunctionType
ALU = mybir.AluOpType
AX = mybir.AxisListType
ENGS = ("tensor", "vector", "scalar", "gpsimd", "sync")
NPBF = ml_dtypes.bfloat16

D = 1024
BATCH = 4
SEQ = 8192
CTX = 256
DEPTH = 4
TB = CTX + SEQ
NCORE = 8
TC = TB
EPS = 1e-6
D_FF = 2816
D_FFE = 3584
NEXP = 8


class Prog:
    def __init__(self, nc, stack, strict_same_engine=True):
        self.nc = nc
        self.sem_stack = stack
        self.stack = stack
        self.phase = 0
        self.ops = {e: [] for e in ENGS}
        self.sems = {}
        self.inc = {}
        self.cnt = {}
        self.seen = {e: {} for e in ENGS}
        self.buf = {}
        self.strict = strict_same_engine
        self.nps = 0
        for e in ENGS[:4]:
            self._mk(e, 1)

    def _mk(self, v, inc):
        self.sems[v] = self.sem_stack.enter_context(self.nc.semaphore("s_" + v.replace(":", "_")))
        self.inc[v] = inc
        self.cnt[v] = 0

    def sb(self, name, shape, dt):
        return self.stack.enter_context(self.nc.sbuf_tensor("%s_p%d" % (name, self.phase), list(shape), dt))

    def ps(self, name, shape, dt=F32):
        return self.stack.enter_context(self.nc.psum_tensor("%s_p%d" % (name, self.phase), list(shape), dt))

    def begin(self):
        self.phase += 1
        self.stack = ExitStack()
        self.stack.__enter__()

    def end(self):
        for eng in ENGS:
            self.finish(eng)
        self.emit()
        self.ops = {e: [] for e in ENGS}
        self.stack.__exit__(None, None, None)
        self.stack = None

    def _deps(self, reads, writes):
        deps = {}

        def add(vk):
            if vk is None:
                return
            v, k = vk
            if deps.get(v, 0) < k:
                deps[v] = k
        for b in reads:
            st = self.buf.get(b)
            if st:
                add(st[0])
        for b in writes:
            st = self.buf.get(b)
            if st:
                add(st[0])
                for r in st[1]:
                    add(r)
        return deps

    def op(self, eng, fn, reads=(), writes=(), slot=None):
        v = eng if slot is None else "dma:" + slot
        if v not in self.sems:
            self._mk(v, 16)
        deps = self._deps(reads, writes)
        for dv, k in deps.items():
            if dv == eng and slot is None:
                if eng == "tensor" or not self.strict or self.cnt[eng] + 1 - k >= 3:
                    continue
            if self.seen[eng].get(dv, 0) >= k:
                continue
            self.seen[eng][dv] = k
            self.ops[eng].append(("w", dv, k * self.inc[dv]))
        self.cnt[v] += 1
        k = self.cnt[v]
        self.ops[eng].append(("i", fn, v))
        for b in reads:
            st = self.buf.setdefault(b, [None, []])
            st[1].append((v, k))
        for b in writes:
            self.buf[b] = [(v, k), []]
        return (v, k)

    def pe(self, r, w, m, *a, **k):
        return self.op("tensor", (m, a, k), r, w)

    def dve(self, r, w, m, *a, **k):
        return self.op("vector", (m, a, k), r, w)

    def act(self, r, w, m, *a, **k):
        return self.op("scalar", (m, a, k), r, w)

    def pool(self, r, w, m, *a, **k):
        return self.op("gpsimd", (m, a, k), r, w)

    def load(self, out_ap, in_ap, key, r=()):
        return self.op("sync", ("dma_start", (), dict(out=out_ap, in_=in_ap)), r, [key], slot=key)

    def store(self, out_ap, in_ap, key, w=()):
        return self.op("gpsimd", ("dma_start", (), dict(out=out_ap, in_=in_ap)), [key], w, slot="st_" + key)

    def finish(self, eng="sync"):
        for v, c in self.cnt.items():
            if c and self.seen[eng].get(v, 0) < c:
                self.ops[eng].append(("w", v, c * self.inc[v]))
                self.seen[eng][v] = c

    def emit(self):
        nc = self.nc
        with nc.Block() as block:
            def run(engname):
                def body(e):
                    for o in self.ops[engname]:
                        if o[0] == "w":
                            e.wait_ge(self.sems[o[1]], o[2])
                        else:
                            getattr(e, o[1][0])(*o[1][1], **o[1][2]).then_inc(self.sems[o[2]], self.inc[o[2]])
                return body
            block.sync(run("sync"))
            block.tensor(run("tensor"))
            block.vector(run("vector"))
            block.scalar(run("scalar"))
            block.gpsimd(run("gpsimd"))


class Rot:
    def __init__(self, tiles, name):
        self.tiles = tiles
        self.name = name
        self.i = 0

    def next(self):
        j = self.i % len(self.tiles)
        self.i += 1
        return self.tiles[j], "%s%d" % (self.name, j)


def sb_rot(P, name, shape, dt, n):
    return Rot([P.sb("%s%d" % (name, j), shape, dt) for j in range(n)], name)


def ps_rot(P, name, n, shape=(128, 512), dt=F32):
    return Rot([P.ps("%s%d" % (name, j), shape, dt) for j in range(n)], name)


NWF = 2592
NWT = 1408
NW1 = NWF + NWT
ST = 384
NST = TC // ST
FEAT_ROWS = 1536


def phase_p1(P, io):
    xs = io["xs"]
    cT = io["cT"]
    ada_w = io["ada_w"]
    ada_b2 = io["ada_b2"]
    ng = io["ng"]
    w1 = io["w1"]
    w2f = io["w2f"]
    tabs = io["tabs"]
    idn = io["idn"]
    modrow = io["modrow"]
    feat = io["feat"]
    tokb = io["tokb"]
    tokf = io["tokf"]
    if True:
        P.begin()
        idf = P.sb("idf", [128, 128], F32)
        cTs = P.sb("cTs", [128, 8, 2], F32)
        scT = P.sb("scT", [128, 8, 2], F32)
        wfb = P.sb("wfb", [128, 8, NW1], BF16)
        w2s = P.sb("w2s", [33, 512], F32)
        ngs = P.sb("ngs", [2, 2, D], F32)
        modsb = P.sb("modsb", [2, 6 * D], F32)
        A1 = [P.sb("A1_%d" % i, [128, D], F32) for i in range(2)]
        B1 = [P.sb("B1_%d" % i, [128, D], F32) for i in range(2)]
        stage = sb_rot(P, "stage", [128, 2048], F32, 2)
        xrot = sb_rot(P, "xt", [128, D], F32, 2)
        hrot = sb_rot(P, "hx", [128, D], F32, 2)
        ssr = sb_rot(P, "ss", [128, 2], F32, 2)
        hT = sb_rot(P, "hT", [128, 8, ST], BF16, 2)
        glT = P.sb("glT", [33, ST], F32)
        tabr = sb_rot(P, "tab", [128, 4, ST], F32, 2)
        t1r = sb_rot(P, "t1", [128, ST], F32, 2)
        t2r = sb_rot(P, "t2", [128, ST], F32, 2)
        fo = sb_rot(P, "fo", [128, ST], BF16, 4)
        tbo = sb_rot(P, "tbo", [128, 1024], BF16, 2)
        tfo = sb_rot(P, "tfo", [128, 896], F32, 2)
        ez = sb_rot(P, "ez", [128, 512], F32, 2)
        pT = ps_rot(P, "pT", 2, (128, 4, 128))
        pg = ps_rot(P, "pg", 6)

        P.load(idf[:], idn, "idf")
        P.load(cTs[:], cT, "cTs")
        P.load(w2s[:], w2f, "w2s")
        P.load(modsb[:], ada_b2, "modsb")
        P.load(ngs[:], ng, "ngs")
        P.act(["cTs"], ["scT"], "activation", out=scT[:], in_=cTs[:], func=AF.Silu)
        P.pool([], ["glT"], "memset", glT[:], 1.0)
        for j in range(24):
            sg, sk = stage.next()
            P.load(sg[:].rearrange("p (c n) -> p c n", c=8), ada_w[:, :, j * 256:(j + 1) * 256], sk)
            pm, pk = pg.next()
            for c in range(8):
                P.pe(["scT", sk], [pk], "matmul", pm[0:2, 0:256], lhsT=scT[:, c, :], rhs=sg[:, c * 256:(c + 1) * 256],
                     start=(c == 0), stop=(c == 7))
            P.dve([pk, "modsb"], ["modsb"], "tensor_tensor", out=modsb[:, j * 256:(j + 1) * 256], in0=pm[0:2, 0:256],
                  in1=modsb[:, j * 256:(j + 1) * 256], op=ALU.add)
        for which in range(2):
            isc = 3 * which + 1
            P.dve(["modsb", "ngs"], ["modsb"], "scalar_tensor_tensor",
                  out=modsb[:, isc * D:(isc + 1) * D], in0=modsb[:, isc * D:(isc + 1) * D], scalar=1.0, in1=ngs[:, which, :],
                  op0=ALU.add, op1=ALU.mult)
        P.store(modrow, modsb[:].rearrange("p (a d) -> p a d", a=6), "modsb", ["MODROW"])
        for v in range(2):
            P.load(A1[v][:], modrow[v:v + 1, 1, :].partition_broadcast(128), "A1_%d" % v, ["MODROW"])
            P.load(B1[v][:], modrow[v:v + 1, 0, :].partition_broadcast(128), "B1_%d" % v, ["MODROW"])
        HW1 = NW1 // 2
        for c in range(16):
            sg, sk = stage.next()
            kc, hf = divmod(c, 2)
            P.load(sg[:, 0:HW1], w1[:, kc, hf * HW1:(hf + 1) * HW1], sk)
            (P.dve if c % 2 == 0 else P.pool)([sk], ["wfb"], "tensor_copy", out=wfb[:, kc, hf * HW1:(hf + 1) * HW1], in_=sg[:, 0:HW1])

        rope_pairs = [(0, 2, 0, 0), (1, 3, 0, 128), (4, 6, 0, 256), (5, 7, 0, 384),
                      (8, 11, 2, 512), (9, 12, 2, 640), (10, 13, 2, 768), (14, 15, 2, 896)]
        plain = [(16, 1024, 48 ** -0.5), (17, 1152, 48 ** -0.5), (18, 1280, 1.0), (19, 1408, 1.0)]
        for s in range(NST):
            hTt, hk = hT.next()
            tb_, tk = tabr.next()
            P.load(tb_[:], tabs[:, :, s * ST:(s + 1) * ST], tk)
            for t in range(3):
                g = 3 * s + t
                v = 1 if g < 2 else 0
                xt_, xk = xrot.next()
                P.load(xt_[:], xs[g * 128:(g + 1) * 128, :], xk)
                ss_, sk_ = ssr.next()
                hx_, hxk = hrot.next()
                P.act([xk], [hxk, sk_], "activation", out=hx_[:], in_=xt_[:], func=AF.Square, accum_out=ss_[:, 0:1])
                P.act([sk_], [sk_], "activation", out=ss_[:, 1:2], in_=ss_[:, 0:1], func=AF.Ln, scale=1.0 / D, bias=EPS)
                P.act([sk_], [sk_], "activation", out=ss_[:, 1:2], in_=ss_[:, 1:2], func=AF.Exp, scale=-0.5)
                P.dve([xk, sk_, "A1_%d" % v], [hxk], "scalar_tensor_tensor",
                      out=hx_[:], in0=xt_[:], scalar=ss_[:, 1:2], in1=A1[v][:], op0=ALU.mult, op1=ALU.mult)
                P.pool([hxk, "B1_%d" % v], [hxk], "tensor_tensor", out=hx_[:], in0=hx_[:], in1=B1[v][:], op=ALU.add)
                for hf in range(2):
                    pt_, ptk = pT.next()
                    for c in range(4):
                        P.pe([hxk, "idf"], [ptk], "transpose", out=pt_[:, c, :], in_=hx_[:, (4 * hf + c) * 128:(4 * hf + c + 1) * 128], identity=idf[:])
                    if hf == 0:
                        P.act([ptk], [hk], "activation", out=hTt[:, 0:4, t * 128:(t + 1) * 128], in_=pt_[:], func=AF.Copy)
                    else:
                        P.dve([ptk], [hk], "tensor_copy", out=hTt[:, 4:8, t * 128:(t + 1) * 128], in_=pt_[:])

            def fm(pd, pk_, f0, ncols, hTt, hk):
                for c in range(8):
                    P.pe(["wfb", hk], [pk_], "matmul", pd[0:ncols, 0:ST], lhsT=wfb[:, c, f0:f0 + ncols], rhs=hTt[:, c, :],
                         start=(c == 0), stop=(c == 7))

            for (fx, fp, ti, row0) in rope_pairs:
                px, pxk = pg.next()
                pp, ppk = pg.next()
                fm(px, pxk, fx * 128, 128, hTt, hk)
                fm(pp, ppk, fp * 128, 128, hTt, hk)
                a_, ak = t1r.next()
                b_, bk = t2r.next()
                P.dve([pxk, tk], [ak], "tensor_tensor", out=a_[:], in0=px[:, 0:ST], in1=tb_[:, ti, :], op=ALU.mult)
                P.dve([ppk, tk], [bk], "tensor_tensor", out=b_[:], in0=pp[:, 0:ST], in1=tb_[:, ti + 1, :], op=ALU.mult)
                o_, ok = fo.next()
                P.pool([ak, bk], [ok], "tensor_tensor", out=o_[:], in0=a_[:], in1=b_[:], op=ALU.add)
                P.store(feat[row0:row0 + 128, s * ST:(s + 1) * ST], o_[:], ok)
            for (f, row0, scl) in plain:
                px, pxk = pg.next()
                fm(px, pxk, f * 128, 128, hTt, hk)
                o_, ok = fo.next()
                P.act([pxk], [ok], "activation", out=o_[:], in_=px[:, 0:ST], func=AF.Copy, scale=scl)
                P.store(feat[row0:row0 + 128, s * ST:(s + 1) * ST], o_[:], ok)
            px, pxk = pg.next()
            fm(px, pxk, 2560, 32, hTt, hk)
            P.act([pxk], ["glT"], "activation", out=glT[0:32, :], in_=px[0:32, 0:ST], func=AF.Copy)
            for t in range(3):
                g = 3 * s + t
                tb2, tbk = tbo.next()
                tf2, tfk = tfo.next()
                for (c0, n) in ((0, 512), (512, 512), (1024, 384)):
                    px, pxk = pg.next()
                    for c in range(8):
                        P.pe(["wfb", hk], [pxk], "matmul", px[:, 0:n], lhsT=hTt[:, c, t * 128:(t + 1) * 128],
                             rhs=wfb[:, c, NWF + c0:NWF + c0 + n], start=(c == 0), stop=(c == 7))
                    if c0 < 1024:
                        P.dve([pxk], [tbk], "tensor_copy", out=tb2[:, c0:c0 + 512], in_=px[:, 0:512])
                    else:
                        P.act([pxk], [tfk], "activation", out=tf2[:, 0:384], in_=px[:, 0:384], func=AF.Silu)
                pz, pzk = pg.next()
                P.pe(["glT", "w2s"], [pzk], "matmul", pz[:, :], lhsT=glT[0:33, t * 128:(t + 1) * 128], rhs=w2s[:, :], start=True, stop=True)
                ez_, ezk = ez.next()
                P.act([pzk], [ezk], "activation", out=ez_[:], in_=pz[:], func=AF.Exp, scale=-1.0)
                P.act([ezk], [tfk], "activation", out=tf2[:, 384:896], in_=ez_[:], func=AF.Ln, bias=1.0)
                P.store(tokb[g * 128:(g + 1) * 128, :], tb2[:], tbk)
                P.store(tokf[g * 128:(g + 1) * 128, :], tf2[:], tfk)
        P.end()


A_Q0, A_K0, A_V0 = 0, 256, 512
B_Q0, B_K0, B_V0 = 768, 1152, 1280
C_Q0, C_K0, C_V0, C_R0, C_G0 = 1408, 1600, 1792, 2176, 2560


def _rope_perm(dim):
    q = dim // 4
    perm = np.zeros(dim, np.int64)
    sign = np.zeros(dim, np.float32)
    for d in range(dim):
        blk = d // q
        if blk % 2 == 0:
            perm[d] = d + q
            sign[d] = -1.0
        else:
            perm[d] = d - q
            sign[d] = 1.0
    return perm, sign


def w1_columns():
    cols = []
    pA, _ = _rope_perm(32)
    pB, _ = _rope_perm(64)

    def permuted(base, n, dim, perm):
        out = []
        for j in range(n):
            hd, d = divmod(j, dim)
            out.append(base + hd * dim + int(perm[d]))
        return out
    cols += list(range(A_Q0, A_Q0 + 256)) + permuted(A_Q0, 256, 32, pA)
    cols += list(range(A_K0, A_K0 + 256)) + permuted(A_K0, 256, 32, pA)
    cols += list(range(B_Q0, B_Q0 + 384)) + permuted(B_Q0, 384, 64, pB)
    cols += list(range(B_K0, B_K0 + 128)) + permuted(B_K0, 128, 64, pB)

    def padded(base):
        out = []
        for h in range(4):
            out += list(range(base + 48 * h, base + 48 * h + 48)) + [-1] * 16
        return out
    cols += padded(C_Q0) + padded(C_K0)
    cols += list(range(C_G0, C_G0 + 32))
    assert len(cols) == NWF
    cols += list(range(A_V0, A_V0 + 256)) + list(range(B_V0, B_V0 + 128)) + list(range(C_V0, C_V0 + 384))
    cols += padded(C_K0) + list(range(C_R0, C_R0 + 384))
    assert len(cols) == NW1
    return np.array(cols, np.int64)


def take_cols(w, cols):
    out = np.zeros((w.shape[0], len(cols)), w.dtype)
    m = cols >= 0
    out[:, m] = w[:, cols[m]]
    return out


def kmajor(w):
    K, N = w.shape
    return np.ascontiguousarray(w.reshape(K // 128, 128, N).transpose(1, 0, 2))


def rope_tables():
    out = np.zeros((4, 128, TB), np.float32)
    tok = np.arange(SEQ)
    row = (tok // 64).astype(np.float32)
    col = (tok % 64).astype(np.float32)
    for ti, dim in ((0, 32), (2, 64)):
        q = dim // 4
        inv = (10000.0 ** (-np.arange(q, dtype=np.float32) / q)).astype(np.float32)
        ang_r = row[:, None] * inv[None, :]
        ang_c = col[:, None] * inv[None, :]
        _, sign = _rope_perm(dim)
        cosd = np.zeros((dim, SEQ), np.float32)
        sind = np.zeros((dim, SEQ), np.float32)
        for d in range(dim):
            ang = ang_r if d < dim // 2 else ang_c
            cosd[d] = np.cos(ang[:, d % q])
            sind[d] = sign[d] * np.sin(ang[:, d % q])
        reps = 128 // dim
        out[ti, :, :CTX] = 1.0
        out[ti + 1, :, :CTX] = 0.0
        out[ti, :, CTX:] = np.tile(cosd, (reps, 1))
        out[ti + 1, :, CTX:] = np.tile(sind, (reps, 1))
    return out


def w2full(w2, bg):
    out = np.zeros((33, 512), np.float32)
    for d in range(2):
        for h in range(4):
            c0 = d * 256 + h * 64
            out[16 * d:16 * d + 16, c0:c0 + 48] = w2[d][:, 48 * h:48 * h + 48]
            out[32, c0:c0 + 48] = bg[d][48 * h:48 * h + 48]
    return out


NT = TB // 128


def phase_p2a(P, io):
    scale = 32 ** -0.5
    aqt = io["aqt"]
    akt = io["akt"]
    av = io["av"]
    lamb = io["lamb"]
    cst = io["cst"]
    idn = io["idn"]
    mo = io["mo"]
    if True:
        P.begin()
        qT = P.sb("qT", [128, TB], BF16)
        kTm = [P.sb("kTm%d" % j, [128, TB], BF16) for j in range(4)]
        va = P.sb("va", [128, NT, 2, 65], BF16)
        idf = P.sb("idf", [128, 128], F32)
        lb = P.sb("lb", [128, 4, 32], F32)
        cs = P.sb("cs", [128, 66], F32)
        sm = P.sb("sm", [128, 8], F32)
        tmp32 = P.sb("tmp32", [128, 32], F32)
        gfin = P.sb("gfin", [128, 64], F32)
        pt = sb_rot(P, "pt", [128, 1024], BF16, 3)
        oT = sb_rot(P, "oT", [65, 512], F32, 2)
        rec = sb_rot(P, "rec", [128, 2, 4], F32, 2)
        d1 = sb_rot(P, "d1", [128, 64], F32, 2)
        dd = sb_rot(P, "dd", [128, 64], F32, 2)
        jk = sb_rot(P, "jk", [128, 64], F32, 2)
        ssr = sb_rot(P, "ssa", [128, 2], F32, 2)
        mot = sb_rot(P, "mot", [128, 4, 128], F32, 2)
        psb = ps_rot(P, "psD", 2, (128, 1024))
        pob = ps_rot(P, "poT", 2)
        ptr = ps_rot(P, "ptr", 1)
        osb = sb_rot(P, "osb", [128, 4, 65], F32, 4)
        P.load(qT[:], aqt, "qT")
        for j in range(4):
            (P.pool if j % 2 == 0 else P.dve)([], ["kT"], "memset", kTm[j][:], 0.0)
        for j in range(4):
            P.load(kTm[j][32 * j:32 * j + 32, :], akt[32 * j:32 * j + 32, :], "kT")
        P.load(lb[:], lamb, "lb")
        P.load(cs[:], cst, "cs")
        P.load(idf[:], idn, "idf")
        P.pool([], ["va"], "memset", va[:], 1.0)
        for h in range(2):
            P.load(va[:, :, h, 0:64], av[:, h * 64:(h + 1) * 64].rearrange("(n p) d -> p n d", p=128), "va")
        for i in range(2):
            P.dve(["lb"], ["tmp32"], "tensor_tensor", out=tmp32[:], in0=lb[:, 2 * i, :], in1=lb[:, 2 * i + 1, :], op=ALU.mult)
            P.dve(["tmp32"], ["sm"], "reduce_sum", out=sm[:, i:i + 1], in_=tmp32[:], axis=AX.X)
        P.act(["sm"], ["sm"], "activation", out=sm[:, 0:2], in_=sm[:, 0:2], func=AF.Exp)
        P.dve(["sm"], ["sm"], "tensor_tensor", out=sm[:, 2:3], in0=sm[:, 0:1], in1=sm[:, 1:2], op=ALU.subtract)
        P.dve(["sm", "cs"], ["sm"], "tensor_tensor", out=sm[:, 2:3], in0=sm[:, 2:3], in1=cs[:, 64:65], op=ALU.add)
        P.dve(["sm"], ["sm"], "tensor_scalar", out=sm[:, 3:4], in0=sm[:, 2:3], scalar1=-1.0, scalar2=None, op0=ALU.mult)
        P.dve(["cs"], ["gfin"], "tensor_scalar", out=gfin[:], in0=cs[:, 0:64], scalar1=cs[:, 65:66], scalar2=None, op0=ALU.mult)

        groups = [(0, 2, 2)] + [(2 + 4 * g, 4, NT) for g in range(16)]
        steps = []
        for (t0, nq, nk) in groups:
            for hl in range(2):
                for m in range(2):
                    for kp in range(nk // 2):
                        steps.append((t0, nq, nk, hl, m, kp))

        def emit_scores(st_):
            t0, nq, nk, hl, m, kp = st_
            nqc = nq * 128
            j = 2 * hl + m
            ps_, psk = psb.next()
            for u in range(2):
                kt = 2 * kp + u
                P.pe(["kT", "qT"], [psk], "matmul", ps_[:, u * 512:u * 512 + nqc], lhsT=kTm[j][:, kt * 128:(kt + 1) * 128],
                     rhs=qT[:, t0 * 128:t0 * 128 + nqc], start=True, stop=True)
            return ps_, psk

        nxt = emit_scores(steps[0])
        pos = []
        po = pok = mt = mk = None
        for si, st_ in enumerate(steps):
            t0, nq, nk, hl, m, kp = st_
            nqc = nq * 128
            ps_, psk = nxt
            if si + 1 < len(steps):
                nxt = emit_scores(steps[si + 1])
            if kp == 0:
                po, pok = pob.next()
                if hl == 0 and m == 0:
                    mt, mk = mot.next()
                if m == 0:
                    pos = []
            p_, pk_ = pt.next()
            P.act([psk], [pk_], "activation", out=p_[:].rearrange("p (u n) -> p u n", u=2)[:, :, 0:nqc],
                  in_=ps_[:].rearrange("p (u n) -> p u n", u=2)[:, :, 0:nqc], func=AF.Exp, scale=scale)
            for u in range(2):
                kt = 2 * kp + u
                P.pe([pk_, "va"], [pok], "matmul", po[0:65, 0:nqc], lhsT=va[:, kt, hl, :], rhs=p_[:, u * 512:u * 512 + nqc],
                     start=(kt == 0), stop=(kt == nk - 1))
            if kp != nk // 2 - 1:
                continue
            o_, ok_ = oT.next()
            P.act([pok], [ok_], "activation", out=o_[:, 0:nqc], in_=po[0:65, 0:nqc], func=AF.Copy)
            tr, trk = ptr.next()
            for qb in range(nq):
                P.pe([ok_, "idf"], [trk], "transpose", out=tr[:, qb * 65:(qb + 1) * 65], in_=o_[:, qb * 128:(qb + 1) * 128], identity=idf[0:65, 0:65])
            os_, osk = osb.next()
            P.dve([trk], [osk], "tensor_copy", out=os_[:, 0:nq, :], in_=tr[:, 0:nq * 65].rearrange("p (q d) -> p q d", d=65))
            pos.append((os_, osk))
            if m == 0:
                continue
            if True:
                (po1, k1), (po2, k2) = pos
                rc, rck = rec.next()
                P.dve([k1], [rck], "reciprocal", out=rc[:, 0, 0:nq], in_=po1[:, 0:nq, 64])
                P.dve([k2], [rck], "reciprocal", out=rc[:, 1, 0:nq], in_=po2[:, 0:nq, 64])
                P.dve([rck, "sm"], [rck], "tensor_scalar", out=rc[:, 1, 0:nq], in0=rc[:, 1, 0:nq], scalar1=sm[:, 3:4], scalar2=None, op0=ALU.mult)
                for qb in range(nq):
                    a_, ak = d1.next()
                    P.dve([k1, rck], [ak], "tensor_scalar", out=a_[:], in0=po1[:, qb, 0:64], scalar1=rc[:, 0, qb:qb + 1], scalar2=None, op0=ALU.mult)
                    d_, dk = dd.next()
                    P.dve([k2, rck, ak], [dk], "scalar_tensor_tensor", out=d_[:], in0=po2[:, qb, 0:64], scalar=rc[:, 1, qb:qb + 1], in1=a_[:],
                          op0=ALU.mult, op1=ALU.add)
                    j_, jkk = jk.next()
                    s_, sk_ = ssr.next()
                    P.act([dk], [jkk, sk_], "activation", out=j_[:], in_=d_[:], func=AF.Square, accum_out=s_[:, 0:1])
                    P.act([sk_], [sk_], "activation", out=s_[:, 1:2], in_=s_[:, 0:1], func=AF.Ln, scale=1.0 / 64, bias=EPS)
                    P.act([sk_], [sk_], "activation", out=s_[:, 1:2], in_=s_[:, 1:2], func=AF.Exp, scale=-0.5)
                    P.dve([dk, sk_, "gfin"], [mk], "scalar_tensor_tensor", out=mt[:, qb, hl * 64:(hl + 1) * 64], in0=d_[:], scalar=s_[:, 1:2],
                          in1=gfin[:], op0=ALU.mult, op1=ALU.mult)
            if hl == 1:
                P.store(mo[t0 * 128:(t0 + nq) * 128, :].rearrange("(q p) c -> p q c", p=128), mt[:, 0:nq, :], mk)
        P.end()


def phase_p2b(P, io):
    scale = 64 ** -0.5
    bqt = io["bqt"]
    bkt = io["bkt"]
    bv = io["bv"]
    sink = io["sink"]
    masks = io["masks"]
    mo = io["mo"]
    if True:
        P.begin()
        qT = P.sb("qT", [64, 3, TB], BF16)
        kT = P.sb("kT", [64, TB], BF16)
        va = P.sb("va", [128, NT, 65], BF16)
        sk = P.sb("sk", [128, 3], F32)
        mk = P.sb("mk", [128, 2, 3, 128], F32)
        pe_ = sb_rot(P, "pe", [128, 3, 128], BF16, 10)
        den = sb_rot(P, "den", [128, 3], F32, 2)
        mot = sb_rot(P, "mot", [128, 3, 64], F32, 3)
        psb = ps_rot(P, "ps", 3)
        pob = ps_rot(P, "po", 2, (128, 3, 65))
        P.load(qT[:], bqt.rearrange("(h d) t -> d h t", d=64), "qT")
        P.load(kT[:], bkt, "kT")
        P.load(sk[:], sink, "sk")
        P.load(mk[:], masks, "mk")
        P.pool([], ["va"], "memset", va[:], 1.0)
        P.load(va[:, :, 0:64], bv.rearrange("(n p) d -> p n d", p=128), "va")
        P.act(["sk"], ["sk"], "activation", out=sk[:], in_=sk[:], func=AF.Exp)
        for n in range(NT):
            if n < 2:
                kts = [(0, None), (1, None)]
            else:
                kts = [(0, None), (1, None)]
                if n - 1 >= 2:
                    kts.append((n - 1, 0))
                kts.append((n, None))
                if n + 1 < NT:
                    kts.append((n + 1, 1))
            po, pok = pob.next()
            pts = []
            for i, (kt, msk) in enumerate(kts):
                ps_, psk = psb.next()
                P.pe(["kT", "qT"], [psk], "matmul", ps_[:, 0:384].rearrange("p (h q) -> p h q", h=3), lhsT=kT[:, kt * 128:(kt + 1) * 128],
                     rhs=qT[:, :, n * 128:(n + 1) * 128], start=True, stop=True)
                p_, pk_ = pe_.next()
                P.act([psk], [pk_], "activation", out=p_[:], in_=ps_[:, 0:384].rearrange("p (h q) -> p h q", h=3), func=AF.Exp, scale=scale)
                if msk is not None:
                    P.dve([pk_, "mk"], [pk_], "tensor_tensor", out=p_[:], in0=p_[:], in1=mk[:, msk, :, :], op=ALU.mult)
                pts.append((p_, pk_, kt))
            for h in range(3):
                for i, (p_, pk_, kt) in enumerate(pts):
                    P.pe([pk_, "va"], [pok], "matmul", po[:, h, :], lhsT=p_[:, h, :], rhs=va[:, kt, :], start=(i == 0), stop=(i == len(pts) - 1))
            dn, dnk = den.next()
            P.dve([pok, "sk"], [dnk], "tensor_tensor", out=dn[:], in0=po[:, :, 64], in1=sk[:], op=ALU.add)
            P.dve([dnk], [dnk], "reciprocal", out=dn[:], in_=dn[:])
            mt, mtk = mot.next()
            for h in range(3):
                P.dve([pok, dnk], [mtk], "tensor_scalar", out=mt[:, h, :], in0=po[:, h, 0:64], scalar1=dn[:, h:h + 1], scalar2=None, op0=ALU.mult)
            P.store(mo[n * 128:(n + 1) * 128, :], mt[:].rearrange("p h d -> p (h d)"), mtk)
        P.end()


def band_masks():
    j = np.arange(128)[:, None]
    i = np.arange(128)[None, :]
    m = np.zeros((128, 2, 3, 128), np.float32)
    m[:, 0, :, :] = (i <= j).astype(np.float32)[:, None, :]
    m[:, 1, :, :] = (j <= i).astype(np.float32)[:, None, :]
    return m


NCH = TB // 64


def gla_consts():
    s_ = np.arange(64)[:, None]
    t_ = np.arange(64)[None, :]
    tri = np.zeros((64, 2, 65), np.float32)
    trix = np.zeros((64, 2, 64), np.float32)
    mask = np.zeros((64, 2, 2, 64), np.float32)
    c = -1.0 / 16.0
    tri[:, 0, :64] = c * (s_ <= t_)
    tri[:, 1, :64] = c * (s_ >= t_)
    tri[:, :, 64] = c
    trix[:, 0, :] = c * (s_ > t_)
    trix[:, 1, :] = c * (s_ < t_)
    mask[:, 0, :, :] = (s_ <= t_).astype(np.float32)[:, None, :]
    mask[:, 1, :, :] = (s_ >= t_).astype(np.float32)[:, None, :]
    return tri, trix, mask


def phase_p2c(P, io):
    cqt = io["cqt"]
    ckt = io["ckt"]
    cktok = io["cktok"]
    cv = io["cv"]
    crs = io["crs"]
    sp = io["sp"]
    tri_d = io["tri_d"]
    trix_d = io["trix_d"]
    mask_d = io["mask_d"]
    gc_d = io["gc_d"]
    mo = io["mo"]
    if True:
        P.begin()
        qT = P.sb("qT", [64, 2, TB], BF16)
        kT = P.sb("kT", [64, 2, TB], BF16)
        OF = P.sb("OF", [64, NCH, 192], F32)
        tri = P.sb("tri_s", [64, 2, 65], F32)
        trix = P.sb("trix_s", [64, 2, 64], F32)
        mask = P.sb("mask_s", [64, 2, 2, 64], F32)
        gc = P.sb("gc_s", [64, 192], F32)
        S = [P.sb("S%d" % d, [64, 2, 96], F32) for d in range(2)]
        Sb = [P.sb("Sb%d" % d, [64, 2, 96], BF16) for d in range(2)]
        spr = sb_rot(P, "spc", [64, 2, 128], F32, 6)
        ktr = sb_rot(P, "ktk", [64, 128], BF16, 6)
        vr = sb_rot(P, "vv", [64, 192], BF16, 6)
        rr = sb_rot(P, "rs", [64, 192], F32, 4)
        E1 = sb_rot(P, "E1", [64, 2, 65], F32, 4)
        E2 = sb_rot(P, "E2", [64, 2, 64], F32, 4)
        E3 = sb_rot(P, "E3", [64, 128], F32, 4)
        qd = sb_rot(P, "qd", [64, 2, 64], BF16, 4)
        ki = sb_rot(P, "ki", [64, 2, 64], BF16, 4)
        ke = sb_rot(P, "ke", [64, 128], BF16, 4)
        att = sb_rot(P, "att", [64, 2, 64], BF16, 4)
        osum = sb_rot(P, "osum", [64, 192], F32, 2)
        jk = sb_rot(P, "jk", [64, 96], F32, 2)
        ssr = sb_rot(P, "ssc", [64, 4], F32, 2)
        yo = sb_rot(P, "yo", [64, 192], F32, 3)
        pb = ps_rot(P, "pb", 2)
        pbd = ps_rot(P, "pbd", 1)
        patt = ps_rot(P, "patt", 2)
        po = ps_rot(P, "po", 2)
        pu = ps_rot(P, "pu", 1)
        P.load(qT[:], cqt.rearrange("(h d) t -> d h t", d=64), "qT")
        P.load(kT[:], ckt.rearrange("(h d) t -> d h t", d=64), "kT")
        P.load(tri[:], tri_d, "tri")
        P.load(trix[:], trix_d, "trix")
        P.load(mask[:], mask_d, "mask")
        P.load(gc[:], gc_d, "gc")
        for d in range(2):
            P.pool([], ["S%d" % d], "memset", S[d][:], 0.0)
            P.pool([], ["Sb%d" % d], "memset", Sb[d][:], 0.0)
        fwd = [(c, 0) for c in range(NCH)]
        bwd = [(c, 1) for c in (3, 2, 1, 0)] + [(c, 1) for c in range(NCH - 1, 3, -1)]
        order = [x for pair in zip(fwd, bwd) for x in pair]
        seen_c = set()
        for (c, d) in order:
            second = c in seen_c
            seen_c.add(c)
            tk = slice(c * 64, (c + 1) * 64)
            sp_, spk = spr.next()
            P.load(sp_[:], sp[tk, :, :], spk)
            kt_, ktk = ktr.next()
            P.load(kt_[:], cktok[tk, :], ktk)
            v_, vk = vr.next()
            P.load(v_[:], cv[tk, :], vk)
            if second:
                r_, rk = rr.next()
                P.load(r_[:], crs[tk, :], rk)
            pb_, pbk = pb.next()
            for h in range(2):
                P.pe([spk, "tri"], [pbk], "matmul", pb_[0:64, h * 65:(h + 1) * 65], lhsT=sp_[:, d, 64 * h:64 * h + 64], rhs=tri[:, d, :], start=True, stop=True)
            pbd_, pbdk = pbd.next()
            P.pe([spk, "trix"], [pbdk], "matmul", pbd_[0:64, 0:128], lhsT=trix[:, d, :], rhs=sp_[:, d, :], start=True, stop=True)
            e1, e1k = E1.next()
            e2, e2k = E2.next()
            e3, e3k = E3.next()
            pbv = pb_[0:64, 0:130].rearrange("p (h n) -> p h n", h=2)
            P.act([pbk], [e1k], "activation", out=e1[:], in_=pbv, func=AF.Exp)
            P.act([pbk], [e2k], "activation", out=e2[:], in_=pbv[:, :, 0:64], func=AF.Exp, scale=-1.0)
            P.act([pbdk], [e3k], "activation", out=e3[:], in_=pbd_[0:64, 0:128], func=AF.Exp)
            qd_, qdk = qd.next()
            ki_, kik = ki.next()
            ke_, kek = ke.next()
            P.dve(["qT", e1k], [qdk], "tensor_tensor", out=qd_[:], in0=qT[:, :, tk], in1=e1[:, :, 0:64], op=ALU.mult)
            P.dve(["kT", e2k], [kik], "tensor_tensor", out=ki_[:], in0=kT[:, :, tk], in1=e2[:], op=ALU.mult)
            P.pool([ktk, e3k], [kek], "tensor_tensor", out=ke_[:], in0=kt_[:], in1=e3[:], op=ALU.mult)
            pa_, pak = patt.next()
            pav = pa_[0:64, 0:128].rearrange("p (h n) -> p h n", h=2)
            for h in range(2):
                P.pe([kik, qdk], [pak], "matmul", pa_[0:64, h * 64:(h + 1) * 64], lhsT=ki_[:, h, :], rhs=qd_[:, h, :], start=True, stop=True)
            at_, atk = att.next()
            P.dve([pak, "mask"], [atk], "tensor_tensor", out=at_[:], in0=pav, in1=mask[:, d, :, :], op=ALU.mult)
            po_, pok = po.next()
            for h in range(2):
                P.pe([atk, vk], [pok], "matmul", po_[0:64, 96 * h:96 * h + 96], lhsT=at_[:, h, :], rhs=v_[:, 96 * h:96 * h + 96], start=True, stop=False)
                P.pe([qdk, "Sb%d" % d], [pok], "matmul", po_[0:64, 96 * h:96 * h + 96], lhsT=qd_[:, h, :], rhs=Sb[d][:, h, :], start=False, stop=True)
            pu_, puk = pu.next()
            for h in range(2):
                P.pe([kek, vk], [puk], "matmul", pu_[0:64, 96 * h:96 * h + 96], lhsT=ke_[:, 64 * h:64 * h + 64], rhs=v_[:, 96 * h:96 * h + 96], start=True, stop=True)
            for h in range(2):
                P.dve(["S%d" % d, e1k, puk], ["S%d" % d], "scalar_tensor_tensor", out=S[d][:, h, :], in0=S[d][:, h, :], scalar=e1[:, h, 64:65],
                      in1=pu_[0:64, 96 * h:96 * h + 96], op0=ALU.mult, op1=ALU.add)
            P.pool(["S%d" % d], ["Sb%d" % d], "tensor_copy", out=Sb[d][:], in_=S[d][:])
            if not second:
                P.act([pok], ["OF%d" % c], "activation", out=OF[:, c, :], in_=po_[0:64, 0:192], func=AF.Copy)
            else:
                os_, osk = osum.next()
                P.dve([pok, "OF%d" % c], [osk], "tensor_tensor", out=os_[:], in0=po_[0:64, 0:192], in1=OF[:, c, :], op=ALU.add)
                s_, sk_ = ssr.next()
                for h in range(2):
                    j_, jkk = jk.next()
                    P.act([osk], [jkk, sk_], "activation", out=j_[:], in_=os_[:, 96 * h:96 * h + 96], func=AF.Square, accum_out=s_[:, h:h + 1])
                P.act([sk_], [sk_], "activation", out=s_[:, 2:4], in_=s_[:, 0:2], func=AF.Ln, scale=1.0 / 96, bias=EPS)
                P.act([sk_], [sk_], "activation", out=s_[:, 2:4], in_=s_[:, 2:4], func=AF.Exp, scale=-0.5)
                y_, yk = yo.next()
                for h in range(2):
                    P.dve([osk, sk_, "gc"], [yk], "scalar_tensor_tensor", out=y_[:, 96 * h:96 * h + 96], in0=os_[:, 96 * h:96 * h + 96],
                          scalar=s_[:, 2 + h:3 + h], in1=gc[:, 96 * h:96 * h + 96], op0=ALU.mult, op1=ALU.mult)
                P.pool([yk, rk], [yk], "tensor_tensor", out=y_[:], in0=y_[:], in1=r_[:], op=ALU.mult)
                P.store(mo[tk, :], y_[:], yk)
        P.end()


def phase_p3(P, io, E, FF, moe):
    FC = FF // 128
    NG = FF // 256
    xs = io["xs"]
    mo = io["mo"]
    modrow = io["modrow"]
    wout = io["wout"]
    router = io["router"]
    wg = io["wg"]
    wu = io["wu"]
    wd = io["wd"]
    idn = io["idn"]
    xo = io["xo"]
    wgd, wud, wdd = io["wgd"], io["wud"], io["wdd"]
    if True:
        P.begin()
        idf = P.sb("idf", [128, 128], F32)
        woutb = P.sb("woutb", [128, 8, D], BF16)
        rts = P.sb("rts", [128, 8, 8], F32)
        G1 = [P.sb("G1_%d" % i, [128, D], F32) for i in range(2)]
        A2 = [P.sb("A2_%d" % i, [128, D], F32) for i in range(2)]
        B2 = [P.sb("B2_%d" % i, [128, D], F32) for i in range(2)]
        G2 = [P.sb("G2_%d" % i, [128, D], F32) for i in range(2)]
        stage = sb_rot(P, "stage", [128, 2048], F32, 3)
        xrot = sb_rot(P, "xt", [128, D], F32, 2)
        mrot = sb_rot(P, "mt", [128, D], F32, 2)
        xnew = sb_rot(P, "xn", [128, D], F32, 3)
        yacc = sb_rot(P, "ya", [128, D], F32, 3)
        hrot = sb_rot(P, "hx", [128, D], F32, 2)
        ssr = sb_rot(P, "ss", [128, 2], F32, 2)
        catT = sb_rot(P, "catT", [128, 8, 128], BF16, 2)
        h2T = sb_rot(P, "h2T", [128, 8, ST], BF16, 2)
        h2Tf = P.sb("h2Tf", [128, 8, ST], F32) if moe else None
        cw = sb_rot(P, "cw", [128, 3, 8], F32, 2)
        lg = sb_rot(P, "lg", [128, 8], F32, 2)
        rt = sb_rot(P, "rtmp", [128, 4, 8], F32, 2)
        rs_ = sb_rot(P, "rsc", [128, 4], F32, 2)
        wgb = sb_rot(P, "wgb", [128, 8, 256], BF16, 2)
        wub = sb_rot(P, "wub", [128, 8, 256], BF16, 2)
        wdb = sb_rot(P, "wdb", [128, 2, D], BF16, 3)
        sgr = sb_rot(P, "sg", [128, ST], F32, 2)
        aTr = sb_rot(P, "aT", [128, ST], BF16, 3)
        bank = [P.ps("bk%d" % i, [128, 512], F32) for i in range(8)]
        bk = ["bk%d" % i for i in range(8)]
        P.load(idf[:], idn, "idf")
        if moe:
            P.load(rts[:], router, "rts")
        for v in range(2):
            P.load(G1[v][:], modrow[v:v + 1, 2, :].partition_broadcast(128), "G1_%d" % v)
            P.load(B2[v][:], modrow[v:v + 1, 3, :].partition_broadcast(128), "B2_%d" % v)
            P.load(A2[v][:], modrow[v:v + 1, 4, :].partition_broadcast(128), "A2_%d" % v)
            P.load(G2[v][:], modrow[v:v + 1, 5, :].partition_broadcast(128), "G2_%d" % v)
        for c in range(8):
            sg, sk = stage.next()
            P.load(sg[:, 0:D], wout[:, c, :], sk)
            (P.dve if c % 2 == 0 else P.pool)([sk], ["woutb"], "tensor_copy", out=woutb[:, c, :], in_=sg[:, 0:D])
        ccast = 0
        for s in range(NST):
            h2, h2k = h2T.next()
            cw_, cwk = cw.next()
            xns = []
            yas = []
            for t in range(3):
                g = 3 * s + t
                v = 1 if g < 2 else 0
                mt, mtk = mrot.next()
                P.load(mt[:], mo[g * 128:(g + 1) * 128, :], mtk)
                xt_, xk = xrot.next()
                P.load(xt_[:], xs[g * 128:(g + 1) * 128, :], xk)
                ct, ctk = catT.next()
                for hf in range(2):
                    for c in range(4):
                        P.pe([mtk, "idf"], [bk[6 + hf]], "transpose", out=bank[6 + hf][:, c * 128:(c + 1) * 128],
                             in_=mt[:, (4 * hf + c) * 128:(4 * hf + c + 1) * 128], identity=idf[:])
                    (P.act if hf == 0 else P.dve)([bk[6 + hf]], [ctk], *(("activation",) if hf == 0 else ("tensor_copy",)),
                                                  **(dict(out=ct[:, 4 * hf:4 * hf + 4, :], in_=bank[6 + hf][:].rearrange("p (c n) -> p c n", c=4), func=AF.Copy)
                                                     if hf == 0 else dict(out=ct[:, 4 * hf:4 * hf + 4, :], in_=bank[6 + hf][:].rearrange("p (c n) -> p c n", c=4))))
                xn_, xnk = xnew.next()
                for hf in range(2):
                    for c in range(8):
                        P.pe([ctk, "woutb"], [bk[1 + hf]], "matmul", bank[1 + hf][:, :], lhsT=ct[:, c, :], rhs=woutb[:, c, hf * 512:(hf + 1) * 512],
                             start=(c == 0), stop=(c == 7))
                    P.dve([bk[1 + hf], "G1_%d" % v], [xnk], "tensor_tensor", out=xn_[:, hf * 512:(hf + 1) * 512], in0=bank[1 + hf][:, :],
                          in1=G1[v][:, hf * 512:(hf + 1) * 512], op=ALU.mult)
                P.pool([xnk, xk], [xnk], "tensor_tensor", out=xn_[:], in0=xn_[:], in1=xt_[:], op=ALU.add)
                xns.append((xn_, xnk, v))
                ss_, sk_ = ssr.next()
                hx_, hxk = hrot.next()
                P.act([xnk], [hxk, sk_], "activation", out=hx_[:], in_=xn_[:], func=AF.Square, accum_out=ss_[:, 0:1])
                P.act([sk_], [sk_], "activation", out=ss_[:, 1:2], in_=ss_[:, 0:1], func=AF.Ln, scale=1.0 / D, bias=EPS)
                P.act([sk_], [sk_], "activation", out=ss_[:, 1:2], in_=ss_[:, 1:2], func=AF.Exp, scale=-0.5)
                P.dve([xnk, sk_, "A2_%d" % v], [hxk], "scalar_tensor_tensor", out=hx_[:], in0=xn_[:], scalar=ss_[:, 1:2], in1=A2[v][:],
                      op0=ALU.mult, op1=ALU.mult)
                P.pool([hxk, "B2_%d" % v], [hxk], "tensor_tensor", out=hx_[:], in0=hx_[:], in1=B2[v][:], op=ALU.add)
                for hf in range(2):
                    for c in range(4):
                        P.pe([hxk, "idf"], [bk[6 + hf]], "transpose", out=bank[6 + hf][:, c * 128:(c + 1) * 128],
                             in_=hx_[:, (4 * hf + c) * 128:(4 * hf + c + 1) * 128], identity=idf[:])
                    src = bank[6 + hf][:].rearrange("p (c n) -> p c n", c=4)
                    if moe:
                        P.dve([bk[6 + hf]], ["h2Tf"], "tensor_copy", out=h2Tf[:, 4 * hf:4 * hf + 4, t * 128:(t + 1) * 128], in_=src)
                        P.pool(["h2Tf"], [h2k], "tensor_copy", out=h2[:, 4 * hf:4 * hf + 4, t * 128:(t + 1) * 128],
                               in_=h2Tf[:, 4 * hf:4 * hf + 4, t * 128:(t + 1) * 128])
                    else:
                        P.act([bk[6 + hf]], [h2k], "activation", out=h2[:, 4 * hf:4 * hf + 4, t * 128:(t + 1) * 128], in_=src, func=AF.Copy)
                if moe:
                    for c in range(8):
                        P.pe(["h2Tf", "rts"], [bk[3]], "matmul", bank[3][:, 0:8], lhsT=h2Tf[:, c, t * 128:(t + 1) * 128], rhs=rts[:, c, :],
                             start=(c == 0), stop=(c == 7))
                    l_, lk = lg.next()
                    r_, rk = rt.next()
                    q_, qk = rs_.next()
                    P.dve([bk[3]], [lk], "tensor_copy", out=l_[:], in_=bank[3][:, 0:8])
                    P.dve([lk], [qk], "reduce_max", out=q_[:, 0:1], in_=l_[:], axis=AX.X)
                    P.dve([lk, qk], [rk], "tensor_scalar", out=r_[:, 0, :], in0=l_[:], scalar1=q_[:, 0:1], scalar2=None, op0=ALU.is_equal)
                    P.dve([rk, lk], [rk], "scalar_tensor_tensor", out=r_[:, 1, :], in0=r_[:, 0, :], scalar=-1e30, in1=l_[:], op0=ALU.mult, op1=ALU.add)
                    P.dve([rk], [qk], "reduce_max", out=q_[:, 1:2], in_=r_[:, 1, :], axis=AX.X)
                    P.dve([lk, qk], [rk], "tensor_scalar", out=r_[:, 2, :], in0=l_[:], scalar1=q_[:, 1:2], scalar2=None, op0=ALU.is_ge)
                    P.dve([qk], [qk], "tensor_scalar", out=q_[:, 2:3], in0=q_[:, 0:1], scalar1=-1.0, scalar2=None, op0=ALU.mult)
                    P.act([lk, qk], [rk], "activation", out=r_[:, 3, :], in_=l_[:], func=AF.Exp, bias=q_[:, 2:3])
                    P.dve([rk], [rk], "tensor_tensor", out=r_[:, 3, :], in0=r_[:, 3, :], in1=r_[:, 2, :], op=ALU.mult)
                    P.dve([rk], [qk], "reduce_sum", out=q_[:, 3:4], in_=r_[:, 3, :], axis=AX.X)
                    P.dve([qk], [qk], "reciprocal", out=q_[:, 3:4], in_=q_[:, 3:4])
                    P.dve([rk, qk], [cwk], "tensor_scalar", out=cw_[:, t, :], in0=r_[:, 3, :], scalar1=q_[:, 3:4], scalar2=None, op0=ALU.mult)
            for t in range(3):
                ya_, yak = yacc.next()
                yas.append((ya_, yak))
            pending = None
            for e in range(E):
                for gi in range(NG):
                    tiles = []
                    for (srcf, srcb, rot_, shp) in ((wg[e, :, :, gi * 256:(gi + 1) * 256], wgd[e, :, :, gi * 256:(gi + 1) * 256], wgb, 8),
                                                    (wu[e, :, :, gi * 256:(gi + 1) * 256], wud[e, :, :, gi * 256:(gi + 1) * 256], wub, 8),
                                                    (wd[e, :, 2 * gi:2 * gi + 2, :], wdd[e, :, 2 * gi:2 * gi + 2, :], wdb, 2)):
                        wb_, wbk = rot_.next()
                        if s == 0:
                            sg, sk = stage.next()
                            P.load(sg[:].rearrange("p (c n) -> p c n", c=shp), srcf, sk)
                            if ccast % 3 == 0:
                                P.dve([sk], [wbk], "tensor_copy", out=wb_[:], in_=sg[:].rearrange("p (c n) -> p c n", c=shp))
                            elif ccast % 3 == 1:
                                P.pool([sk], [wbk], "tensor_copy", out=wb_[:], in_=sg[:].rearrange("p (c n) -> p c n", c=shp))
                            else:
                                P.act([sk], [wbk], "activation", out=wb_[:], in_=sg[:].rearrange("p (c n) -> p c n", c=shp), func=AF.Copy)
                            ccast += 1
                            P.store(srcb, wb_[:], wbk)
                        else:
                            P.load(wb_[:], srcb, wbk)
                        tiles.append((wb_, wbk))
                    (wg_, wgk), (wu_, wuk), (wd_, wdk) = tiles
                    for j in range(2):
                        for c in range(8):
                            P.pe([wgk, h2k], [bk[6]], "matmul", bank[6][:, 0:ST], lhsT=wg_[:, c, j * 128:(j + 1) * 128], rhs=h2[:, c, :],
                                 start=(c == 0), stop=(c == 7))
                        for c in range(8):
                            P.pe([wuk, h2k], [bk[7]], "matmul", bank[7][:, 0:ST], lhsT=wu_[:, c, j * 128:(j + 1) * 128], rhs=h2[:, c, :],
                                 start=(c == 0), stop=(c == 7))
                        sg_, sgk = sgr.next()
                        P.act([bk[6]], [sgk], "activation", out=sg_[:], in_=bank[6][:, 0:ST], func=AF.Silu)
                        a_, ak = aTr.next()
                        P.dve([sgk, bk[7]], [ak], "tensor_tensor", out=a_[:], in0=sg_[:], in1=bank[7][:, 0:ST], op=ALU.mult)
                        first = (gi == 0 and j == 0)
                        last = (gi == NG - 1 and j == 1)
                        if pending is not None:
                            pending()

                        def down(a_=a_, ak=ak, wd_=wd_, wdk=wdk, j=j, first=first, last=last):
                            for t in range(3):
                                for hf in range(2):
                                    b_ = 2 * t + hf
                                    P.pe([ak, wdk], [bk[b_]], "matmul", bank[b_][:, :], lhsT=a_[:, t * 128:(t + 1) * 128],
                                         rhs=wd_[:, j, hf * 512:(hf + 1) * 512], start=first, stop=last)
                        pending = down
                pending()
                pending = None
                for t in range(3):
                    ya_, yak = yas[t]
                    for hf in range(2):
                        b_ = 2 * t + hf
                        osl = ya_[:, hf * 512:(hf + 1) * 512]
                        if not moe:
                            P.act([bk[b_]], [yak], "activation", out=osl, in_=bank[b_][:, :], func=AF.Copy)
                        elif e == 0:
                            P.dve([bk[b_], cwk], [yak], "tensor_scalar", out=osl, in0=bank[b_][:, :], scalar1=cw_[:, t, e:e + 1], scalar2=None, op0=ALU.mult)
                        else:
                            P.dve([bk[b_], cwk, yak], [yak], "scalar_tensor_tensor", out=osl, in0=bank[b_][:, :], scalar=cw_[:, t, e:e + 1], in1=osl,
                                  op0=ALU.mult, op1=ALU.add)
            for t in range(3):
                g = 3 * s + t
                ya_, yak = yas[t]
                xn_, xnk, v = xns[t]
                P.dve([yak, "G2_%d" % v], [yak], "tensor_tensor", out=ya_[:], in0=ya_[:], in1=G2[v][:], op=ALU.mult)
                P.pool([yak, xnk], [yak], "tensor_tensor", out=ya_[:], in0=ya_[:], in1=xn_[:], op=ALU.add)
                P.store(xo[g * 128:(g + 1) * 128, :], ya_[:], yak)
            if s == 0:
                for eng in ENGS:
                    P.finish(eng)
        P.end()


def phase_p4(P, io):
    xs = io["xs"]
    fg = io["fg"]
    xo = io["xo"]
    if True:
        P.begin()
        g_ = P.sb("g_", [128, D], F32)
        xrot = sb_rot(P, "xt", [128, D], F32, 3)
        orot = sb_rot(P, "ot", [128, D], F32, 3)
        ssr = sb_rot(P, "ss", [128, 2], F32, 3)
        P.load(g_[:], fg[0:1, :].partition_broadcast(128), "g_")
        for g in range(2, TC // 128):
            xt_, xk = xrot.next()
            P.load(xt_[:], xs[g * 128:(g + 1) * 128, :], xk)
            o_, ok = orot.next()
            ss_, sk_ = ssr.next()
            P.act([xk], [ok, sk_], "activation", out=o_[:], in_=xt_[:], func=AF.Square, accum_out=ss_[:, 0:1])
            P.act([sk_], [sk_], "activation", out=ss_[:, 1:2], in_=ss_[:, 0:1], func=AF.Ln, scale=1.0 / D, bias=EPS)
            P.act([sk_], [sk_], "activation", out=ss_[:, 1:2], in_=ss_[:, 1:2], func=AF.Exp, scale=-0.5)
            P.dve([xk, sk_, "g_"], [ok], "scalar_tensor_tensor", out=o_[:], in0=xt_[:], scalar=ss_[:, 1:2], in1=g_[:], op0=ALU.mult, op1=ALU.mult)
            P.store(xo[(g - 2) * 128:(g - 1) * 128, :], o_[:], ok)
        P.end()


def build_fused(depth=DEPTH):
    nc = bass.Bass("TRN2", target_bir_lowering=False)

    def din(name, shape, dt=F32):
        return nc.dram_tensor(name, list(shape), dt, kind="ExternalInput").ap()

    def scratch(name, shape, dt=F32):
        return nc.dram_tensor(name, list(shape), dt).ap()

    xs = din("xs", [TB, D])
    cT = din("cT", [128, 8, 2])
    tabs = din("tabs", [128, 4, TB])
    idn = din("idn", [128, 128])
    ada_w = din("ada_w", [DEPTH, 128, 8, 6 * D])
    ada_b2 = din("ada_b2", [DEPTH, 2, 6 * D])
    ng = din("ng", [DEPTH, 2, 2, D])
    w1 = din("w1", [DEPTH, 128, 8, NW1])
    w2f = din("w2f", [DEPTH, 33, 512])
    lamb = din("lamb", [DEPTH, 128, 4, 32])
    cst = din("cst", [DEPTH, 128, 66])
    sink = din("sink", [DEPTH, 2, 128, 3])
    masks = din("masks", [128, 2, 3, 128])
    tri = din("tri", [64, 2, 65])
    trix = din("trix", [64, 2, 64])
    gmask = din("gmask", [64, 2, 2, 64])
    gc = din("gc", [DEPTH, 64, 192])
    wout = din("wout", [DEPTH, 128, 8, D])
    ffg = din("ffg", [2, 1, 128, 8, D_FF])
    ffu = din("ffu", [2, 1, 128, 8, D_FF])
    ffd = din("ffd", [2, 1, 128, D_FF // 128, D])
    mog = din("mog", [2, NEXP, 128, 8, D_FFE])
    mou = din("mou", [2, NEXP, 128, 8, D_FFE])
    mod_ = din("mod_", [2, NEXP, 128, D_FFE // 128, D])
    router = din("router", [2, 128, 8, 8])
    fg = din("fg", [1, D])
    out = nc.dram_tensor("out", [SEQ, D], F32, kind="ExternalOutput").ap()
    X = [scratch("X%d" % i, [TB, D]) for i in range(2)]
    MODROW = scratch("MODROW", [2, 6, D])
    FEAT = scratch("FEAT", [FEAT_ROWS, TB], BF16)
    TOKB = scratch("TOKB", [TB, 1024], BF16)
    TOKF = scratch("TOKF", [TB, 896])
    MO = scratch("MO", [TB, D])
    WGD = scratch("WGD", [NEXP, 128, 8, D_FFE], BF16)
    WUD = scratch("WUD", [NEXP, 128, 8, D_FFE], BF16)
    WDD = scratch("WDD", [NEXP, 128, D_FFE // 128, D], BF16)
    FGD = scratch("FGD", [1, 128, 8, D_FF], BF16)
    FUD = scratch("FUD", [1, 128, 8, D_FF], BF16)
    FDD = scratch("FDD", [1, 128, D_FF // 128, D], BF16)
    with ExitStack() as st:
        P = Prog(nc, st)
        xin = xs
        for L in range(depth):
            xout = X[L % 2]
            phase_p1(P, dict(xs=xin, cT=cT, ada_w=ada_w[L], ada_b2=ada_b2[L], ng=ng[L], w1=w1[L], w2f=w2f[L], tabs=tabs, idn=idn,
                             modrow=MODROW, feat=FEAT, tokb=TOKB, tokf=TOKF))
            for hh in range(2):
                phase_p2a(P, dict(aqt=FEAT[128 * hh:128 * hh + 128, :], akt=FEAT[256 + 128 * hh:256 + 128 * hh + 128, :],
                                  av=TOKB[:, 128 * hh:128 * hh + 128], lamb=lamb[L], cst=cst[L], idn=idn, mo=MO[:, 128 * hh:128 * hh + 128]))
                phase_p2b(P, dict(bqt=FEAT[512 + 192 * hh:512 + 192 * hh + 192, :], bkt=FEAT[896 + 64 * hh:896 + 64 * hh + 64, :],
                                  bv=TOKB[:, 256 + 64 * hh:256 + 64 * hh + 64], sink=sink[L, hh], masks=masks,
                                  mo=MO[:, 256 + 192 * hh:256 + 192 * hh + 192]))
                phase_p2c(P, dict(cqt=FEAT[1024 + 128 * hh:1024 + 128 * hh + 128, :], ckt=FEAT[1280 + 128 * hh:1280 + 128 * hh + 128, :],
                                  cktok=TOKB[:, 768 + 128 * hh:768 + 128 * hh + 128], cv=TOKB[:, 384 + 192 * hh:384 + 192 * hh + 192],
                                  crs=TOKF[:, 192 * hh:192 * hh + 192],
                                  sp=TOKF[:, 384:896].rearrange("t (d c) -> t d c", d=2)[:, :, 128 * hh:128 * hh + 128],
                                  tri_d=tri, trix_d=trix, mask_d=gmask, gc_d=gc[L], mo=MO[:, 640 + 192 * hh:640 + 192 * hh + 192]))
            j = L // 2
            if L % 2 == 0:
                phase_p3(P, dict(xs=xin, mo=MO, modrow=MODROW, wout=wout[L], router=router[0], wg=ffg[j], wu=ffu[j], wd=ffd[j],
                                 idn=idn, xo=xout, wgd=FGD, wud=FUD, wdd=FDD), 1, D_FF, False)
            else:
                phase_p3(P, dict(xs=xin, mo=MO, modrow=MODROW, wout=wout[L], router=router[j], wg=mog[j], wu=mou[j], wd=mod_[j],
                                 idn=idn, xo=xout, wgd=WGD, wud=WUD, wdd=WDD), NEXP, D_FFE, True)
            xin = xout
        phase_p4(P, dict(xs=xin, fg=fg, xo=out))
    return nc


_PROG = []


def _c(a):
    return np.ascontiguousarray(a)


def kernel(x, c, ctx, c_ctx, norm1_g, norm2_g, ada_w, ada_b, w_in, w_out, a_lambda, a_norm_g,
           b_sink, c_gate_w2, c_gate_b, c_norm_g, ffn_w_gate, ffn_w_up, ffn_w_down,
           moe_router, moe_w_gate, moe_w_up, moe_w_down, final_g):
    f = lambda a: np.asarray(a, np.float32)
    x, c, ctx, c_ctx = f(x), f(c), f(ctx), f(c_ctx)
    if not _PROG:
        _PROG.append(build_fused())
    nc = _PROG[0]
    cols = w1_columns()
    tabs = _c(rope_tables().transpose(1, 0, 2))
    tri, trix, gmask = gla_consts()
    shared = {
        "tabs": tabs, "idn": np.eye(128, dtype=np.float32),
        "ada_w": np.stack([kmajor(f(ada_w[L])) for L in range(DEPTH)]),
        "ada_b2": np.stack([np.stack([f(ada_b[L])] * 2) for L in range(DEPTH)]),
        "ng": np.stack([np.stack([np.stack([f(norm1_g[L]), f(norm2_g[L])])] * 2) for L in range(DEPTH)]),
        "w1": np.stack([kmajor(take_cols(f(w_in[L]), cols)) for L in range(DEPTH)]),
        "w2f": np.stack([w2full(f(c_gate_w2[L]), f(c_gate_b[L])) for L in range(DEPTH)]),
        "lamb": _c(np.broadcast_to(f(a_lambda)[:, None], (DEPTH, 128, 4, 32))),
        "masks": band_masks(), "tri": tri, "trix": trix, "gmask": gmask,
        "gc": _c(np.broadcast_to(np.tile(f(c_norm_g), (1, 2))[:, None, :], (DEPTH, 64, 192))),
        "wout": np.stack([kmajor(f(w_out[L])) for L in range(DEPTH)]),
        "ffg": np.stack([kmajor(f(ffn_w_gate[j]))[None] for j in range(2)]),
        "ffu": np.stack([kmajor(f(ffn_w_up[j]))[None] for j in range(2)]),
        "ffd": np.stack([kmajor(f(ffn_w_down[j]))[None] for j in range(2)]),
        "mog": np.stack([np.stack([kmajor(f(moe_w_gate[j][e])) for e in range(NEXP)]) for j in range(2)]),
        "mou": np.stack([np.stack([kmajor(f(moe_w_up[j][e])) for e in range(NEXP)]) for j in range(2)]),
        "mod_": np.stack([np.stack([kmajor(f(moe_w_down[j][e])) for e in range(NEXP)]) for j in range(2)]),
        "router": np.stack([kmajor(f(moe_router[j])) for j in range(2)]),
        "fg": _c(f(final_g)[None, :]),
    }
    cst = np.zeros((DEPTH, 128, 66), np.float32)
    for L in range(DEPTH):
        lam_init = 0.8 - 0.6 * math.exp(-0.3 * L)
        cst[L, :, :64] = f(a_norm_g[L])[None, :]
        cst[L, :, 64] = lam_init
        cst[L, :, 65] = 1.0 - lam_init
    shared["cst"] = cst
    shared["sink"] = _c(np.broadcast_to(f(b_sink).reshape(DEPTH, 2, 1, 3), (DEPTH, 2, 128, 3)))
    in_maps = []
    for i in range(NCORE):
        b = i // 2
        m = dict(shared)
        m["xs"] = _c(np.concatenate([ctx[b], x[b]], 0))
        m["cT"] = _c(np.stack([c[b].reshape(8, 128).T, c_ctx.reshape(8, 128).T], -1))
        in_maps.append(m)
    res = run_bass_kernel_spmd(nc, in_maps, core_ids=list(range(NCORE)))
    out = np.stack([np.asarray(res.results[2 * b]["out"]) for b in range(BATCH)], 0)
    return np.ascontiguousarray(out.astype(np.float32))
```
